# Optimizing a Trainium2 kernel written in Bass

```python
import math
import jax, jax.numpy as jnp
from jax import lax
import numpy as np


D_MODEL = 1024
BATCH = 4
SEQ = 4096
DEPTH = 2

HY_WIDTH = 512
HY_ORDER = 2
HY_EMB = 33
HY_FFN = 64
HY_SHORT = 3
HY_FAST_DECAY = 0.3
HY_SLOW_DECAY = 1.5
HY_TARGET = 1e-2
HY_FILTER_SCALE = 0.03
DA_HEADS = 4
DA_HEAD_DIM = 64
DA_V_DIM = 2 * DA_HEAD_DIM
DA_QK_WIDTH = DA_HEADS * 2 * DA_HEAD_DIM
DA_V_WIDTH = DA_HEADS * DA_V_DIM
Q_BLOCK = 128
ROPE_THETA = 10000.0
N_BRANCH = 2
IN_WIDTH = (HY_ORDER + 1) * HY_WIDTH + 2 * DA_QK_WIDTH + DA_V_WIDTH + N_BRANCH * D_MODEL
N_EXPERTS = 16
EC_FACTOR = 2
D_EXPERT = 2048
NORM_EPS = 1e-6
SUBLN_EPS = 1e-5

kernel_name = "hybrid_hyena_diffattn_ec_moe_encoder"


def rms_norm(x, g, eps=NORM_EPS):
    xf = x.astype(jnp.float32)
    y = xf * lax.rsqrt(jnp.mean(xf * xf, axis=-1, keepdims=True) + eps) * g.astype(jnp.float32)
    return y.astype(x.dtype)


def rope(x, pos):
    dh = x.shape[-1]
    half = dh // 2
    inv = ROPE_THETA ** (-jnp.arange(half, dtype=jnp.float32) * 2.0 / dh)
    ang = pos[:, None] * inv[None, :]
    cos = jnp.cos(ang)[None, :, None, :]
    sin = jnp.sin(ang)[None, :, None, :]
    xf = x.astype(jnp.float32)
    x1, x2 = xf[..., :half], xf[..., half:]
    out = jnp.concatenate([x1 * cos - x2 * sin, x2 * cos + x1 * sin], axis=-1)
    return out.astype(x.dtype)


def hyena_filters(L, w1, b1, f1, w2, b2, f2, w3):
    t = jnp.linspace(0.0, 1.0, L, dtype=jnp.float32)
    bands = (HY_EMB - 1) // 2
    w = 2.0 * math.pi * jnp.arange(L, dtype=jnp.float32) / L
    fr = jnp.linspace(1e-4, bands - 1, bands, dtype=jnp.float32)
    ang = w[:, None] * fr[None, :]
    z = jnp.concatenate([t[:, None], jnp.cos(ang), -jnp.sin(ang)], axis=-1)
    h = jnp.sin(f1.astype(jnp.float32) * (z @ w1.astype(jnp.float32) + b1.astype(jnp.float32)))
    h = jnp.sin(f2.astype(jnp.float32) * (h @ w2.astype(jnp.float32) + b2.astype(jnp.float32)))
    h = h @ w3.astype(jnp.float32)
    h = h.reshape(L, HY_ORDER, 2, HY_WIDTH).transpose(1, 2, 0, 3)
    min_decay = math.log(HY_TARGET) / HY_FAST_DECAY
    max_decay = math.log(HY_TARGET) / HY_SLOW_DECAY
    deltas = jnp.abs(jnp.linspace(min_decay, max_decay, HY_WIDTH, dtype=jnp.float32))
    decay = jnp.exp(-t[:, None] * deltas[None, :])
    return h * decay[None, None]


def bidir_fftconv(z, h_fwd, h_bwd, d_bias):
    L, C = z.shape[1], z.shape[2]
    h_full = jnp.concatenate([h_fwd, jnp.zeros((1, C), jnp.float32), h_bwd[1:][::-1]], axis=0)
    hf = jnp.fft.rfft(h_full, n=2 * L, axis=0)
    zf = jnp.fft.rfft(z.astype(jnp.float32), n=2 * L, axis=1)
    y = jnp.fft.irfft(zf * hf[None], n=2 * L, axis=1)[:, :L]
    y = y + z.astype(jnp.float32) * d_bias.astype(jnp.float32)
    return y.astype(z.dtype)


def hyena_branch(u, conv_w, conv_b, filters, d_bias):
    up = jnp.pad(u, ((0, 0), (1, 1), (0, 0)))
    uc = up[:, :-2] * conv_w[0] + up[:, 1:-1] * conv_w[1] + up[:, 2:] * conv_w[2] + conv_b
    v, x1, x2 = jnp.split(uc, HY_ORDER + 1, axis=-1)
    z = v
    for o, gate in enumerate((x1, x2)):
        z = gate * bidir_fftconv(z, filters[o, 0], filters[o, 1], d_bias[o])
    return z


def diff_attention(q, k, v, lam, lam_init, subln_g):
    B, S = q.shape[0], q.shape[1]
    nb = S // Q_BLOCK
    scale = DA_HEAD_DIM ** -0.5
    qb = (q * scale).reshape(B, nb, Q_BLOCK, DA_HEADS, 2, DA_HEAD_DIM).transpose(1, 0, 3, 4, 2, 5)
    kh = k.reshape(B, S, DA_HEADS, 2, DA_HEAD_DIM).transpose(0, 2, 3, 1, 4)
    vh = v.transpose(0, 2, 1, 3)

    def block(qblk):
        s = jnp.einsum('bhcqd,bhckd->bhcqk', qblk, kh).astype(jnp.float32)
        p = jax.nn.softmax(s, axis=-1)
        a = p[:, :, 0] - lam * p[:, :, 1]
        return jnp.einsum('bhqk,bhkv->bhqv', a.astype(vh.dtype), vh)

    o = lax.map(block, qb)
    o = o.transpose(1, 0, 3, 2, 4).reshape(B, S, DA_HEADS, DA_V_DIM)
    o = rms_norm(o, subln_g, SUBLN_EPS) * (1.0 - lam_init)
    return o.reshape(B, S, DA_V_WIDTH)


def ec_moe(n, w_r, b_r, w_gate, w_up, w_down):
    B, S, D = n.shape
    cap = EC_FACTOR * S // N_EXPERTS
    logits = (n @ w_r + b_r).astype(jnp.float32)
    aff = jax.nn.softmax(logits, axis=-1)
    g, idx = lax.top_k(aff.transpose(0, 2, 1), cap)
    xg = jax.vmap(lambda xb, ib: xb[ib])(n, idx)
    hdn = jax.nn.silu(jnp.einsum('becd,edf->becf', xg, w_gate)) * jnp.einsum('becd,edf->becf', xg, w_up)
    ye = jnp.einsum('becf,efd->becd', hdn, w_down) * g[..., None].astype(n.dtype)
    flat = (jnp.arange(B, dtype=jnp.int32)[:, None, None] * S + idx).reshape(-1)
    out = jnp.zeros((B * S, D), n.dtype).at[flat].add(ye.reshape(-1, D))
    return out.reshape(B, S, D)


def setup_inputs(seed: int = 0) -> dict:
    key = jax.random.key(seed)
    ks = jax.random.split(key, 32)

    def nrm(k, shape, scale):
        return jax.random.normal(k, shape, jnp.float32) * scale

    C = HY_WIDTH
    E = N_EXPERTS
    F = D_EXPERT
    return {
        "x": nrm(ks[0], (BATCH, SEQ, D_MODEL), 1.0),
        "norm_mix": 1.0 + nrm(ks[1], (DEPTH, D_MODEL), 0.02),
        "w_in": nrm(ks[2], (DEPTH, D_MODEL, IN_WIDTH), D_MODEL ** -0.5),
        "b_in": nrm(ks[3], (DEPTH, IN_WIDTH), 0.02),
        "hy_conv_w": nrm(ks[4], (DEPTH, HY_SHORT, (HY_ORDER + 1) * C), HY_SHORT ** -0.5),
        "hy_conv_b": nrm(ks[5], (DEPTH, (HY_ORDER + 1) * C), 0.02),
        "hy_ffn_w1": nrm(ks[6], (DEPTH, HY_EMB, HY_FFN), HY_EMB ** -0.5),
        "hy_ffn_b1": nrm(ks[7], (DEPTH, HY_FFN), 0.02),
        "hy_ffn_f1": 1.0 + nrm(ks[8], (DEPTH, HY_FFN), 0.02),
        "hy_ffn_w2": nrm(ks[9], (DEPTH, HY_FFN, HY_FFN), HY_FFN ** -0.5),
        "hy_ffn_b2": nrm(ks[10], (DEPTH, HY_FFN), 0.02),
        "hy_ffn_f2": 1.0 + nrm(ks[11], (DEPTH, HY_FFN), 0.02),
        "hy_ffn_w3": nrm(ks[12], (DEPTH, HY_FFN, HY_ORDER * 2 * C), HY_FILTER_SCALE * HY_FFN ** -0.5),
        "hy_bias": nrm(ks[13], (DEPTH, HY_ORDER, C), 0.2),
        "lambda_q1": nrm(ks[14], (DEPTH, DA_HEAD_DIM), 0.1),
        "lambda_k1": nrm(ks[15], (DEPTH, DA_HEAD_DIM), 0.1),
        "lambda_q2": nrm(ks[16], (DEPTH, DA_HEAD_DIM), 0.1),
        "lambda_k2": nrm(ks[17], (DEPTH, DA_HEAD_DIM), 0.1),
        "subln_g": 1.0 + nrm(ks[18], (DEPTH, DA_V_DIM), 0.02),
        "w_up_hyena": nrm(ks[19], (DEPTH, C, D_MODEL), C ** -0.5),
        "w_up_attn": nrm(ks[20], (DEPTH, DA_V_WIDTH, D_MODEL), DA_V_WIDTH ** -0.5),
        "w_out": nrm(ks[21], (DEPTH, D_MODEL, D_MODEL), D_MODEL ** -0.5),
        "norm_ffn": 1.0 + nrm(ks[22], (DEPTH, D_MODEL), 0.02),
        "w_router": nrm(ks[23], (DEPTH, D_MODEL, E), D_MODEL ** -0.5),
        "b_router": nrm(ks[24], (DEPTH, E), 0.01),
        "w_e_gate": nrm(ks[25], (DEPTH, E, D_MODEL, F), D_MODEL ** -0.5),
        "w_e_up": nrm(ks[26], (DEPTH, E, D_MODEL, F), D_MODEL ** -0.5),
        "w_e_down": nrm(ks[27], (DEPTH, E, F, D_MODEL), F ** -0.5),
        "norm_final": 1.0 + nrm(ks[28], (D_MODEL,), 0.02),
    }


def reference(x, norm_mix, w_in, b_in, hy_conv_w, hy_conv_b, hy_ffn_w1, hy_ffn_b1, hy_ffn_f1,
              hy_ffn_w2, hy_ffn_b2, hy_ffn_f2, hy_ffn_w3, hy_bias, lambda_q1, lambda_k1, lambda_q2,
              lambda_k2, subln_g, w_up_hyena, w_up_attn, w_out, norm_ffn, w_router, b_router,
              w_e_gate, w_e_up, w_e_down, norm_final):
    B, S, D = x.shape
    pos = jnp.arange(S, dtype=jnp.float32)
    splits = np.cumsum([(HY_ORDER + 1) * HY_WIDTH, DA_QK_WIDTH, DA_QK_WIDTH, DA_V_WIDTH, D_MODEL]).tolist()
    for l in range(DEPTH):
        n = rms_norm(x, norm_mix[l])
        p = n @ w_in[l] + b_in[l]
        u_hy, q, k, v, g_hy, g_da = jnp.split(p, splits, axis=-1)

        filt = hyena_filters(S, hy_ffn_w1[l], hy_ffn_b1[l], hy_ffn_f1[l], hy_ffn_w2[l],
                             hy_ffn_b2[l], hy_ffn_f2[l], hy_ffn_w3[l])
        y_hy = hyena_branch(u_hy, hy_conv_w[l], hy_conv_b[l], filt, hy_bias[l])

        q = rope(q.reshape(B, S, 2 * DA_HEADS, DA_HEAD_DIM), pos)
        k = rope(k.reshape(B, S, 2 * DA_HEADS, DA_HEAD_DIM), pos)
        v = v.reshape(B, S, DA_HEADS, DA_V_DIM)
        lam_init = 0.8 - 0.6 * math.exp(-0.3 * l)
        lam = (jnp.exp(jnp.sum(lambda_q1[l].astype(jnp.float32) * lambda_k1[l].astype(jnp.float32)))
               - jnp.exp(jnp.sum(lambda_q2[l].astype(jnp.float32) * lambda_k2[l].astype(jnp.float32)))
               + lam_init)
        y_da = diff_attention(q, k, v, lam, lam_init, subln_g[l])

        gh = jax.nn.sigmoid(g_hy.astype(jnp.float32)).astype(x.dtype)
        ga = jax.nn.sigmoid(g_da.astype(jnp.float32)).astype(x.dtype)
        merged = gh * (y_hy @ w_up_hyena[l]) + ga * (y_da @ w_up_attn[l])
        x = x + merged @ w_out[l]

        x = x + ec_moe(rms_norm(x, norm_ffn[l]), w_router[l], b_router[l],
                       w_e_gate[l], w_e_up[l], w_e_down[l])
    return rms_norm(x, norm_final)
```

```python
import numpy as np
import concourse.bass as bass
import concourse.mybir as mybir

F32 = mybir.dt.float32
BF16 = mybir.dt.bfloat16
I32 = mybir.dt.int32
AF = mybir.ActivationFunctionType
ALU = mybir.AluOpType
AX = mybir.AxisListType

COMPUTE = ("tensor", "vector", "scalar", "gpsimd")
NDSEM = 8


class Op:
    __slots__ = ("id", "eng", "fn", "deps", "signal", "semval", "is_dma", "dsem", "dval", "dprev")


class Prog:
    def __init__(self, nc):
        self.nc = nc
        self.ops = []
        self.state = {}

    def _prune(self, ids):
        best = {}
        out = set()
        for i in ids:
            o = self.ops[i]
            if o.is_dma:
                out.add(i)
            else:
                if o.eng not in best or best[o.eng] < i:
                    best[o.eng] = i
        out.update(best.values())
        return out

    def add(self, eng, fn, r=(), w=(), dma=False):
        o = Op()
        o.id = len(self.ops)
        o.eng = eng
        o.fn = fn
        o.is_dma = dma
        o.signal = dma
        deps = set()
        for k in tuple(r) + tuple(w):
            if k not in self.state:
                nm = k if isinstance(k, str) else k[0]
                if not (nm.startswith("D:") or nm.startswith("K:")):
                    self.state[k] = [None, list(getattr(self, "_pend", []))]
            st = self.state.get(k)
            if st is not None and st[0] is not None:
                deps.add(st[0])
        for k in w:
            st = self.state.get(k)
            if st is not None:
                deps.update(st[1])
        o.deps = self._prune(deps)
        self.ops.append(o)
        for k in r:
            st = self.state.setdefault(k, [None, []])
            st[1].append(o.id)
            if len(st[1]) > 24:
                st[1] = list(self._prune(st[1]))
        for k in w:
            self.state[k] = [o.id, []]
        return o

    def phase_barrier(self, newkeys, oldprefix=None):
        pend = set()
        for k, st in list(self.state.items()):
            nm = k if isinstance(k, str) else k[0]
            if nm.startswith("D:") or nm.startswith("K:"):
                continue
            if st[0] is not None:
                pend.add(st[0])
            pend.update(st[1])
            del self.state[k]
        pend = list(self._prune(pend))
        self._pend = pend

    def fresh(self, key):
        self.state[key] = [None, list(getattr(self, "_pend", []))]

    def emit(self, stack):
        nc = self.nc
        engs = ["tensor", "vector", "scalar", "gpsimd", "sync"]
        sems = {e: stack.enter_context(nc.semaphore("s_" + e)) for e in COMPUTE}
        dsems = {e: [stack.enter_context(nc.semaphore("d_%s%d" % (e, i))) for i in range(NDSEM)]
                 for e in engs}
        for o in self.ops:
            for d in o.deps:
                p = self.ops[d]
                if p.is_dma:
                    continue
                if p.eng == "tensor" and o.eng == "tensor" and not o.is_dma:
                    continue
                p.signal = True
        cnt = {e: 0 for e in COMPUTE}
        dcnt = {e: 0 for e in engs}
        dval = {e: [0] * NDSEM for e in engs}
        for o in self.ops:
            if o.is_dma:
                i = dcnt[o.eng] % NDSEM
                dcnt[o.eng] += 1
                o.dsem = dsems[o.eng][i]
                o.dprev = dval[o.eng][i]
                dval[o.eng][i] += 16
                o.dval = dval[o.eng][i]
            elif o.signal:
                cnt[o.eng] += 1
                o.semval = cnt[o.eng]
        self.maxsem = dict(cnt)
        block = stack.enter_context(nc.Block())
        byeng = {e: [o for o in self.ops if o.eng == e] for e in engs}

        def run(engname, eobj):
            waited = {}

            def wait(sem, val):
                key = id(sem)
                if waited.get(key, 0) >= val:
                    return
                waited[key] = val
                eobj.wait_ge(sem, val)

            for o in byeng[engname]:
                for d in sorted(o.deps):
                    p = self.ops[d]
                    if p.is_dma:
                        wait(p.dsem, p.dval)
                    else:
                        if p.eng == "tensor" and engname == "tensor" and not o.is_dma:
                            continue
                        wait(sems[p.eng], p.semval)
                if o.is_dma:
                    if o.dprev > 0:
                        wait(o.dsem, o.dprev)
                    inst = o.fn(eobj)
                    inst.then_inc(o.dsem, 16)
                else:
                    inst = o.fn(eobj)
                    if o.signal:
                        inst.then_inc(sems[engname], 1)

        @block.tensor
        def _(e):
            run("tensor", e)

        @block.vector
        def _(e):
            run("vector", e)

        @block.scalar
        def _(e):
            run("scalar", e)

        @block.gpsimd
        def _(e):
            run("gpsimd", e)

        @block.sync
        def _(e):
            run("sync", e)

    def mm(self, out, lhsT, rhs, start, stop, r, w):
        return self.add("tensor", lambda e: e.matmul(out, lhsT, rhs, start=start, stop=stop), r, w)

    def tr(self, out, in_, ident, r, w):
        return self.add("tensor", lambda e: e.transpose(out, in_, ident), r, w)

    def act(self, out, in_, func, r, w, bias=None, scale=None, accum=None):
        kw = {}
        if bias is not None:
            kw["bias"] = bias
        if scale is not None:
            kw["scale"] = scale
        if accum is not None:
            kw["accum_out"] = accum
        return self.add("scalar", lambda e: e.activation(out, in_, func, **kw), r, w)

    def ts(self, eng, out, in0, s1, s2, op0, op1, r, w):
        if op1 is None:
            return self.add(eng, lambda e: e.tensor_scalar(out, in0, s1, None, op0), r, w)
        return self.add(eng, lambda e: e.tensor_scalar(out, in0, s1, s2, op0, op1), r, w)

    def tt(self, eng, out, in0, in1, op, r, w):
        return self.add(eng, lambda e: e.tensor_tensor(out, in0, in1, op), r, w)

    def stt(self, out, in0, scalar, in1, op0, op1, r, w):
        return self.add("vector", lambda e: e.scalar_tensor_tensor(out, in0, scalar, in1, op0, op1), r, w)

    def copy(self, eng, out, in_, r, w):
        if eng == "scalar":
            return self.add(eng, lambda e: e.copy(out, in_), r, w)
        return self.add(eng, lambda e: e.tensor_copy(out, in_), r, w)

    def dma(self, eng, out, in_, r, w, **kw):
        return self.add(eng, lambda e: e.dma_start(out, in_, **kw), r, w, dma=True)

    def memset(self, eng, ap, val, w):
        return self.add(eng, lambda e: e.memset(ap, val), (), w)


class Arena:
    def __init__(self, P, tensor, words):
        self.P = P
        self.t = tensor
        self.words = words
        self.off = 0
        self.names = []

    def reset(self):
        self.P.phase_barrier(None)
        self.off = 0

    def alloc(self, name, shape, dtype):
        n = int(np.prod(shape[1:]))
        if dtype == BF16:
            assert n % 2 == 0
            w = n // 2
        else:
            w = n
        assert self.off + w <= self.words, (name, self.off, w, self.words)
        ap = self.t[0:shape[0], self.off:self.off + w]
        if dtype != F32:
            ap = ap.bitcast(dtype)
        self.off += w
        if len(shape) > 2:
            names = " ".join("a%d" % i for i in range(len(shape) - 1))
            kw = {"a%d" % i: shape[i + 1] for i in range(len(shape) - 1)}
            ap = ap.rearrange("p (%s) -> p %s" % (names, names), **kw)
        self.P.fresh(name)
        return ap

from contextlib import ExitStack
import math
import ml_dtypes
from concourse.bass_utils import run_bass_kernel_spmd

S = 4096
D = 1024
NT = 32
ARENA = 49152
TWO_PI = 2.0 * math.pi
MAGIC = 12582912.0
PI_LO = 3.1415925
NFFT = 8192


def build(nc, dbg=False, stages=("A", "B0", "B1", "B2", "C", "D", "E", "F"), depth=2):
    def din(name, shape, dt=F32):
        return nc.dram_tensor(name, list(shape), dt, kind="ExternalInput").ap()

    skind = "ExternalOutput" if dbg else "Internal"

    def dscr(name, shape, dt):
        return nc.dram_tensor(name, list(shape), dt, kind=skind).ap()

    I = {}
    I["x"] = din("x", [S, D])
    I["w_in"] = din("w_in", [2, D, 6144])
    I["b_in"] = din("b_in", [2, 128, 48])
    I["b_v"] = din("b_v", [2, 128, 512])
    I["norm_mix"] = din("norm_mix", [2, 128, D])
    I["norm_ffn"] = din("norm_ffn", [2, 128, D])
    I["norm_final"] = din("norm_final", [128, D])
    I["cosT"] = din("cosT", [128, S])
    I["sinT"] = din("sinT", [128, S])
    I["hy_cw"] = din("hy_cw", [2, 128, 36])
    I["hy_cb"] = din("hy_cb", [2, 128, 12])
    I["zT"] = din("zT", [33, S])
    I["hy_w1"] = din("hy_w1", [2, 33, 64])
    I["hy_w2"] = din("hy_w2", [2, 64, 64])
    I["hy_w3"] = din("hy_w3", [2, 64, 2048])
    I["hy_cols"] = din("hy_cols", [2, 64, 4])
    I["decay"] = din("decay", [S, 512])
    I["decayb"] = din("decayb", [S, 512])
    I["hy_bias"] = din("hy_bias", [2, 2, 128, 512])
    I["ctab"] = din("ctab", [16, 128, 16 * 128], BF16)
    I["stab"] = din("stab", [16, 128, 16 * 128], BF16)
    I["twid"] = din("twid", [128, 17 * 3])
    I["alt"] = din("alt", [128, 2], BF16)
    I["altrow"] = din("altrow", [1, 128], BF16)
    I["lamv"] = din("lamv", [2, 128, 256])
    I["subln"] = din("subln", [2, 128, 128])
    I["w_up_hy"] = din("w_up_hy", [2, 512, D])
    I["w_up_da"] = din("w_up_da", [2, 512, D])
    I["w_out"] = din("w_out", [2, D, D])
    I["w_router"] = din("w_router", [2, D, 16])
    I["b_router"] = din("b_router", [2, 128, 16])
    I["w_e_gate"] = din("w_e_gate", [2, 16, D, 2048])
    I["w_e_up"] = din("w_e_up", [2, 16, D, 2048])
    I["w_e_down"] = din("w_e_down", [2, 16, 2048, D])
    I["iota"] = din("iota", [128, 512])
    I["jp"] = din("jp", [128, 64], BF16)
    I["ident_bf"] = din("ident_bf", [128, 128], BF16)
    I["ident_f"] = din("ident_f", [128, 128])
    I["ustrict"] = din("ustrict", [128, 128], BF16)
    I["ones_bf"] = din("ones_bf", [128, 128], BF16)
    out = nc.dram_tensor("out", [S, D], F32, kind="ExternalOutput").ap()

    xres = dscr("xres", [S, D], F32)
    uT = dscr("uT", [1536, S], BF16)
    qkT = dscr("qkT", [1024, S], BF16)
    gT = dscr("gT", [2048, S], BF16)
    Vtm = dscr("Vtm", [S, 512], BF16)
    Hsp = dscr("Hsp", [2, 4, 17 * 128, 512], F32)
    hsd = dscr("hsd", [2, S, 512], BF16)
    yhtm = dscr("yhtm", [S, 512], BF16)
    ctm = dscr("ctm", [3, S, 512], BF16)
    yhT = dscr("yhT", [512, S], BF16)
    ydT = dscr("ydT", [512, S], BF16)
    n2d = dscr("n2d", [S, D], BF16)
    dbgaff = dscr("dbgaff", [128, 32 * 16], F32)
    dbgpos = dscr("dbgpos", [128, 16 * 32], F32)

    st = ExitStack()
    with st:
        arena_t = st.enter_context(nc.sbuf_tensor("arena", [128, ARENA], F32))
        ident = st.enter_context(nc.sbuf_tensor("ident", [128, 128], BF16))
        identf = st.enter_context(nc.sbuf_tensor("identf", [128, 128], F32))
        ustr = st.enter_context(nc.sbuf_tensor("ustr", [128, 128], BF16))
        ones = st.enter_context(nc.sbuf_tensor("ones", [128, 128], BF16))
        alt = st.enter_context(nc.sbuf_tensor("altc", [128, 2], BF16))
        altrow = st.enter_context(nc.sbuf_tensor("altr", [1, 128], BF16))
        posm = st.enter_context(nc.sbuf_tensor("posm", [128, 16, 32], F32))
        affhl = st.enter_context(nc.sbuf_tensor("affhl", [128, 32, 16, 4], BF16))
        lamt = st.enter_context(nc.sbuf_tensor("lamt", [128, 8], F32))
        ps = [st.enter_context(nc.psum_tensor("ps%d" % i, [128, 512], F32)) for i in range(8)]
        PK = ["K:ps%d" % i for i in range(8)]
        P = Prog(nc)
        A = Arena(P, arena_t, ARENA)

        def psb(i):
            return ps[i][:].bitcast(BF16)

        P.dma("sync", ident[:], I["ident_bf"], [], ["K:ident"])
        P.dma("sync", identf[:], I["ident_f"], [], ["K:identf"])
        P.dma("sync", ustr[:], I["ustrict"], [], ["K:ustr"])
        P.dma("sync", ones[:], I["ones_bf"], [], ["K:ones"])
        P.dma("sync", alt[:], I["alt"], [], ["K:alt"])
        P.dma("sync", altrow[:], I["altrow"], [], ["K:altrow"])
        for q in range(4):
            P.dma("sync", xres[q * 1024:(q + 1) * 1024, :], I["x"][q * 1024:(q + 1) * 1024, :], [],
                  [("D:xres", j) for j in range(q * 8, q * 8 + 8)])

        def rms_tile(xt_ap, kx, junk, stt3, ks, n, eps):
            P.act(junk, xt_ap, AF.Square, [kx], [ks + "j", ks], accum=stt3[:, 0:1])
            P.ts("vector", stt3[:, 1:2], stt3[:, 0:1], 1.0 / n, eps, ALU.mult, ALU.add, [ks], [ks])
            P.act(stt3[:, 1:2], stt3[:, 1:2], AF.Sqrt, [ks], [ks])
            P.add("vector", lambda e: e.reciprocal(stt3[:, 2:3], stt3[:, 1:2]), [ks], [ks])

        wcnt = [0]

        def load_w(dst, dkey, src, stg, stgk):
            i = wcnt[0] % len(stg)
            wcnt[0] += 1
            P.dma("sync", stg[i], src, [], [stgk[i]])
            ceng = ("scalar", "vector", "scalar")[wcnt[0] % 3]
            P.copy(ceng, dst, stg[i], [stgk[i]], [dkey])

        def phase_A(l):
            A.reset()
            nT = A.alloc("nT", [128, 8, S], BF16)
            cosT = A.alloc("cosT", [128, S], F32)
            sinT = A.alloc("sinT", [128, S], F32)
            gt = A.alloc("gt", [128, D], F32)
            bcol = A.alloc("bcol", [128, 48], F32)
            bv = A.alloc("bv", [128, 512], F32)
            xt = [A.alloc("xt%d" % i, [128, D], F32) for i in range(2)]
            junk = A.alloc("junk", [128, D], BF16)
            xn = [A.alloc("xn%d" % i, [128, D], BF16) for i in range(2)]
            s3 = [A.alloc("s3%d" % i, [128, 4], F32) for i in range(2)]
            wst = [A.alloc("wst%d" % i, [128, 8, 256], F32) for i in range(2)]
            wstk = ["wst0", "wst1"]
            wt = [A.alloc("wt%d" % i, [128, 8, 512], BF16) for i in range(2)]
            stg = [A.alloc("stg%d" % i, [128, 512], BF16) for i in range(3)]
            tq = [A.alloc("tq%d" % i, [128, 512], F32) for i in range(2)]
            P.dma("sync", gt, I["norm_mix"][l], [], ["gt"])
            P.dma("sync", bcol, I["b_in"][l], [], ["bcol"])
            P.dma("sync", bv, I["b_v"][l], [], ["bv"])
            P.dma("sync", cosT, I["cosT"], [], ["cosT"])
            P.dma("sync", sinT, I["sinT"], [], ["sinT"])
            for j in range(NT):
                b = j % 2
                P.dma("sync", xt[b], xres[j * 128:(j + 1) * 128, :], [("D:xres", j)], ["xt%d" % b])
                rms_tile(xt[b], "xt%d" % b, junk, s3[b], "s3%d" % b, D, 1e-6)
                P.stt(xn[b], xt[b], s3[b][:, 2:3], gt, ALU.mult, ALU.mult, ["xt%d" % b, "s3%d" % b, "gt"], ["xn%d" % b])
                pb = psb(6 + b)
                for c in range(8):
                    P.tr(pb[:, c * 128:(c + 1) * 128], xn[b][:, c * 128:(c + 1) * 128], ident[:],
                         ["xn%d" % b, "K:ident"], [PK[6 + b]])
                P.copy("scalar", nT[:, :, j * 128:(j + 1) * 128], pb.rearrange("p (c t) -> p c t", c=8),
                       [PK[6 + b]], [("nT", j // 4)])
            wsrc = I["w_in"][l].rearrange("(c p) f -> p c f", p=128)
            gcnt = [0]

            def load_group(g):
                b = gcnt[0] % 2
                gcnt[0] += 1
                for h in range(2):
                    load_w(wt[b][:, :, h * 256:(h + 1) * 256], ("wt%d" % b, h),
                           wsrc[:, :, g * 512 + h * 256: g * 512 + (h + 1) * 256], wst, wstk)
                return b

            def wkeys(b):
                return [("wt%d" % b, 0), ("wt%d" % b, 1)]

            bankc = [0]
            stc = [0]

            def proj_fm(b, fc, tg):
                bk = bankc[0] % 4
                bankc[0] += 1
                for kc in range(8):
                    P.mm(ps[bk][:], wt[b][:, kc, fc * 128:(fc + 1) * 128], nT[:, kc, tg * 512:(tg + 1) * 512],
                         kc == 0, kc == 7, wkeys(b) + [("nT", tg)], [PK[bk]])
                return bk

            for g in [0, 1, 2, 7, 8, 9, 10]:
                b = load_group(g)
                for fc in range(4):
                    ch = g * 4 + fc
                    for tg in range(8):
                        bk = proj_fm(b, fc, tg)
                        si = stc[0] % 3
                        stc[0] += 1
                        func = AF.Identity if g < 3 else AF.Sigmoid
                        P.act(stg[si], ps[bk][:], func, [PK[bk], "bcol"], ["stg%d" % si], bias=bcol[:, ch:ch + 1])
                        if g < 3:
                            dst = uT[ch * 128:(ch + 1) * 128, tg * 512:(tg + 1) * 512]
                            dk = ("D:uT", ch, tg)
                        else:
                            gc = ch - 28
                            dst = gT[gc * 128:(gc + 1) * 128, tg * 512:(tg + 1) * 512]
                            dk = ("D:gT", gc, tg)
                        P.dma("gpsimd", dst, stg[si], ["stg%d" % si], [dk])
            for (ga, gb2, rbase) in [(3, 5, 0), (4, 6, 4)]:
                ba = load_group(ga)
                bb = load_group(gb2)
                for fc in range(4):
                    cha = ga * 4 + fc
                    chb = gb2 * 4 + fc
                    for tg in range(8):
                        bk1 = proj_fm(ba, fc, tg)
                        bk2 = proj_fm(bb, fc, tg)
                        sl = slice(tg * 512, (tg + 1) * 512)
                        P.stt(tq[0], ps[bk1][:], bcol[:, cha:cha + 1], cosT[:, sl], ALU.add, ALU.mult,
                              [PK[bk1], "bcol", "cosT"], ["tq0"])
                        P.stt(tq[1], ps[bk2][:], bcol[:, chb:chb + 1], sinT[:, sl], ALU.add, ALU.mult,
                              [PK[bk2], "bcol", "sinT"], ["tq1"])
                        si = stc[0] % 3
                        stc[0] += 1
                        P.tt("vector", stg[si], tq[0], tq[1], ALU.add, ["tq0", "tq1"], ["stg%d" % si])
                        rc = rbase + fc
                        P.dma("gpsimd", qkT[rc * 128:(rc + 1) * 128, sl], stg[si], ["stg%d" % si], [("D:qkT", rc, tg)])
            b = load_group(11)
            for j in range(NT):
                bk = bankc[0] % 4
                bankc[0] += 1
                for kc in range(8):
                    P.mm(ps[bk][:], nT[:, kc, j * 128:(j + 1) * 128], wt[b][:, kc, :], kc == 0, kc == 7,
                         wkeys(b) + [("nT", j // 4)], [PK[bk]])
                si = stc[0] % 3
                stc[0] += 1
                P.tt("vector", stg[si], ps[bk][:], bv, ALU.add, [PK[bk], "bv"], ["stg%d" % si])
                P.dma("gpsimd", Vtm[j * 128:(j + 1) * 128, :], stg[si], ["stg%d" % si], [("D:Vtm", j)])

        def sin_layer(dst, lhsT, rhs_fn, bcolap, fcolap, kr, tmp, M):
            for tg in range(8):
                bk = tg % 2
                P.mm(ps[bk][0:M, :], lhsT, rhs_fn(tg), True, True, kr, [PK[bk]])
                a, b_, c = tmp
                P.ts("vector", a, ps[bk][0:M, :], bcolap, fcolap, ALU.add, ALU.mult, [PK[bk], "fcols"], ["sa"])
                P.ts("vector", b_, a, 1.0 / TWO_PI, MAGIC, ALU.mult, ALU.add, ["sa"], ["sb"])
                P.ts("vector", b_, b_, MAGIC, None, ALU.subtract, None, ["sb"], ["sb"])
                P.stt(c, b_, -TWO_PI, a, ALU.mult, ALU.add, ["sa", "sb"], ["sc"])
                P.ts("vector", c, c, PI_LO, -PI_LO, ALU.min, ALU.max, ["sc"], ["sc"])
                P.act(dst[:, tg * 512:(tg + 1) * 512], c, AF.Sin, ["sc"], [dst_key[0]])

        dst_key = [None]

        def table_loads(cb, sb, b, idx):
            P.dma("sync", cb[b], I["ctab"][idx].rearrange("p (u k) -> p u k", k=128), [], ["cb%d" % b])
            P.dma("sync", sb[b], I["stab"][idx].rearrange("p (u k) -> p u k", k=128), [], ["sb%d" % b])

        def eo_view(dram2d, v):
            return dram2d.rearrange("(uc p two) c -> two p uc c", p=128, two=2)[v]

        def fwd_chunk(kc, ze, zo, zk, cb, sb, T, tw, need=("X1r", "X1i", "X2r", "X2i")):
            b = kc % 2
            M = 128 if kc < 16 else 1
            if kc < 16:
                table_loads(cb, sb, b, kc)
                for (bank, tab, tk, zz) in ((0, cb, "cb", ze), (1, sb, "sb", ze), (2, cb, "cb", zo), (3, sb, "sb", zo)):
                    for uc in range(16):
                        P.mm(ps[bank][:], tab[b][:, uc, :], zz[:, uc, :], uc == 0, uc == 15, ["%s%d" % (tk, b)] + zk, [PK[bank]])
            else:
                for (bank, zz) in ((0, ze), (2, zo)):
                    for uc in range(16):
                        P.mm(ps[bank][0:1, :], alt[:, 0:1], zz[:, uc, :], uc == 0, uc == 15, ["K:alt"] + zk, [PK[bank]])
            Er, Ei, Or, Oi = ps[0][0:M, :], ps[1][0:M, :], ps[2][0:M, :], ps[3][0:M, :]
            twr = tw[0:M, kc * 3:kc * 3 + 1]
            tws = tw[0:M, kc * 3 + 1:kc * 3 + 2]
            twn = tw[0:M, kc * 3 + 2:kc * 3 + 3]
            wor, woi, t1, t2 = T["wor"][0:M, :], T["woi"][0:M, :], T["t1"][0:M, :], T["t2"][0:M, :]
            if kc < 16:
                P.ts("vector", t1, Oi, tws, None, ALU.mult, None, [PK[3], "tw"], ["t1"])
                P.stt(wor, Or, twr, t1, ALU.mult, ALU.add, [PK[2], "tw", "t1"], ["wor"])
                P.ts("vector", t2, Or, twn, None, ALU.mult, None, [PK[2], "tw"], ["t2"])
                P.stt(woi, Oi, twr, t2, ALU.mult, ALU.add, [PK[3], "tw", "t2"], ["woi"])
                P.tt("vector", T["x1r"][0:M, :], Er, wor, ALU.add, [PK[0], "wor"], ["x1r"])
                P.tt("vector", T["x2r"][0:M, :], Er, wor, ALU.subtract, [PK[0], "wor"], ["x2r"])
                P.tt("vector", T["x1i"][0:M, :], Ei, woi, ALU.add, [PK[1], "woi"], ["x1i"])
                P.tt("vector", T["x2i"][0:M, :], woi, Ei, ALU.subtract, ["woi", PK[1]], ["x2i"])
            else:
                P.copy("vector", T["x1r"][0:1, :], Er, [PK[0]], ["x1r"])
                P.copy("vector", T["x2r"][0:1, :], Er, [PK[0]], ["x2r"])
                P.ts("vector", T["x1i"][0:1, :], Or, -1.0, None, ALU.mult, None, [PK[2]], ["x1i"])
                P.ts("vector", T["x2i"][0:1, :], Or, -1.0, None, ALU.mult, None, [PK[2]], ["x2i"])
            return M

        def phase_B0(l):
            A.reset()
            zT = A.alloc("zT", [33, S], F32)
            w1 = A.alloc("w1", [33, 64], F32)
            w2 = A.alloc("w2", [64, 64], F32)
            w3 = A.alloc("w3", [64, 2048], F32)
            fcols = A.alloc("fcols", [64, 4], F32)
            h1 = A.alloc("h1T", [64, S], F32)
            h2 = A.alloc("h2T", [64, S], F32)
            tmp = [A.alloc(n, [64, 512], F32) for n in ("sa", "sb", "sc")]
            dec = [A.alloc("dec%d" % i, [128, 512], F32) for i in range(2)]
            decb = [A.alloc("decb%d" % i, [128, 512], F32) for i in range(2)]
            hf = A.alloc("hf", [128, 512], F32)
            hb = A.alloc("hb", [128, 512], F32)
            hso = [A.alloc("hso%d" % i, [128, 512], BF16) for i in range(2)]
            hdo = [A.alloc("hdo%d" % i, [128, 512], BF16) for i in range(2)]
            ze = A.alloc("ze", [128, 16, 512], BF16)
            zo = A.alloc("zo", [128, 16, 512], BF16)
            cb = [A.alloc("cb%d" % i, [128, 16, 128], BF16) for i in range(2)]
            sb = [A.alloc("sb%d" % i, [128, 16, 128], BF16) for i in range(2)]
            tw = A.alloc("tw", [128, 51], F32)
            T = {n: A.alloc(n, [128, 512], F32) for n in ("wor", "woi", "t1", "t2", "x1r", "x1i", "x2r", "x2i")}
            P.dma("sync", zT, I["zT"], [], ["zT"])
            P.dma("sync", w1, I["hy_w1"][l], [], ["w1"])
            P.dma("sync", w2, I["hy_w2"][l], [], ["w2"])
            P.dma("sync", w3, I["hy_w3"][l], [], ["w3"])
            P.dma("sync", fcols, I["hy_cols"][l], [], ["fcols"])
            P.dma("sync", tw, I["twid"], [], ["tw"])
            dst_key[0] = "h1T"
            sin_layer(h1, w1, lambda tg: zT[:, tg * 512:(tg + 1) * 512], fcols[:, 0:1], fcols[:, 1:2],
                      ["w1", "zT"], tmp, 64)
            dst_key[0] = "h2T"
            sin_layer(h2, w2, lambda tg: h1[:, tg * 512:(tg + 1) * 512], fcols[:, 2:3], fcols[:, 3:4],
                      ["w2", "h1T"], tmp, 64)
            for o in range(2):
                for jt in range(NT):
                    b = jt % 2
                    P.dma("sync", dec[b], I["decay"][jt * 128:(jt + 1) * 128, :], [], ["dec%d" % b])
                    P.dma("sync", decb[b], I["decayb"][jt * 128:(jt + 1) * 128, :], [], ["decb%d" % b])
                    P.mm(ps[4][:], h2[:, jt * 128:(jt + 1) * 128], w3[:, o * 1024:o * 1024 + 512], True, True,
                         ["h2T", "w3"], [PK[4]])
                    P.mm(ps[5][:], h2[:, jt * 128:(jt + 1) * 128], w3[:, o * 1024 + 512:o * 1024 + 1024], True, True,
                         ["h2T", "w3"], [PK[5]])
                    P.tt("vector", hf, ps[4][:], dec[b], ALU.mult, [PK[4], "dec%d" % b], ["hf"])
                    P.tt("vector", hb, ps[5][:], decb[b], ALU.mult, [PK[5], "decb%d" % b], ["hb"])
                    P.tt("vector", hso[b], hf, hb, ALU.add, ["hf", "hb"], ["hso%d" % b])
                    P.tt("gpsimd", hdo[b], hf, hb, ALU.subtract, ["hf", "hb"], ["hdo%d" % b])
                    P.dma("gpsimd", hsd[0, jt * 128:(jt + 1) * 128, :], hso[b], ["hso%d" % b], [("D:hsd", 0, jt)])
                    P.dma("gpsimd", hsd[1, jt * 128:(jt + 1) * 128, :], hdo[b], ["hdo%d" % b], [("D:hsd", 1, jt)])
                for sig in range(2):
                    hk_ = [("D:hsd", sig, jt) for jt in range(NT)]
                    P.dma("sync", ze, eo_view(hsd[sig], 0), hk_, ["ze"])
                    P.dma("sync", zo, eo_view(hsd[sig], 1), hk_, ["zo"])
                    for kc in range(17):
                        M = fwd_chunk(kc, ze, zo, ["ze", "zo"], cb, sb, T, tw)
                        if sig == 0:
                            P.dma("gpsimd", Hsp[o, 0, kc * 128:kc * 128 + M, :], T["x1r"][0:M, :], ["x1r"], [("D:Hsp", o, 0, kc)])
                            P.dma("gpsimd", Hsp[o, 2, kc * 128:kc * 128 + M, :], T["x2r"][0:M, :], ["x2r"], [("D:Hsp", o, 2, kc)])
                        else:
                            P.dma("gpsimd", Hsp[o, 1, kc * 128:kc * 128 + M, :], T["x1i"][0:M, :], ["x1i"], [("D:Hsp", o, 1, kc)])
                            P.dma("gpsimd", Hsp[o, 3, kc * 128:kc * 128 + M, :], T["x2i"][0:M, :], ["x2i"], [("D:Hsp", o, 3, kc)])

        def phase_B1(l):
            A.reset()
            cw = A.alloc("cw", [128, 36], F32)
            cbi = A.alloc("cbi", [128, 12], F32)
            ub = [A.alloc("ub%d" % i, [128, S], BF16) for i in range(2)]
            acc = [A.alloc("acc%d" % i, [128, S], F32) for i in range(2)]
            ucb = [A.alloc("ucb%d" % i, [128, S], BF16) for i in range(2)]
            ttm = [A.alloc("ttm%d" % i, [128, 32, 128], BF16) for i in range(2)]
            P.dma("sync", cw, I["hy_cw"][l], [], ["cw"])
            P.dma("sync", cbi, I["hy_cb"][l], [], ["cbi"])
            for ch in range(12):
                b = ch % 2
                P.dma("sync", ub[b], uT[ch * 128:(ch + 1) * 128, :], [("D:uT", ch, tg) for tg in range(8)], ["ub%d" % b])
                P.act(acc[b], ub[b], AF.Identity, ["ub%d" % b, "cw", "cbi"], ["acc%d" % b],
                      bias=cbi[:, ch:ch + 1], scale=cw[:, ch * 3 + 1:ch * 3 + 2])
                P.stt(acc[b][:, 1:S], ub[b][:, 0:S - 1], cw[:, ch * 3:ch * 3 + 1], acc[b][:, 1:S], ALU.mult, ALU.add,
                      ["ub%d" % b, "cw", "acc%d" % b], ["acc%d" % b])
                P.stt(ucb[b][:, 0:S - 1], ub[b][:, 1:S], cw[:, ch * 3 + 2:ch * 3 + 3], acc[b][:, 0:S - 1], ALU.mult, ALU.add,
                      ["ub%d" % b, "cw", "acc%d" % b], ["ucb%d" % b])
                P.copy("vector", ucb[b][:, S - 1:S], acc[b][:, S - 1:S], ["acc%d" % b, "ucb%d" % b], ["ucb%d" % b])
                for t8 in range(4):
                    bk = 4 + (t8 % 2)
                    pb = psb(bk)
                    for i in range(8):
                        tc = t8 * 8 + i
                        P.tr(pb[:, i * 128:(i + 1) * 128], ucb[b][:, tc * 128:(tc + 1) * 128], ident[:],
                             ["ucb%d" % b, "K:ident"], [PK[bk]])
                    P.copy("scalar" if t8 % 2 == 0 else "vector", ttm[b][:, t8 * 8:(t8 + 1) * 8, :],
                           pb.rearrange("p (c t) -> p c t", c=8), [PK[bk]], ["ttm%d" % b])
                which = ch // 4
                cc = ch % 4
                dst = ctm[which].rearrange("(tc p) c -> p tc c", p=128)[:, :, cc * 128:(cc + 1) * 128]
                P.dma("gpsimd", dst, ttm[b], ["ttm%d" % b], [("D:ctm", which, cc)])

        def phase_B2(l):
            A.reset()
            ze = [A.alloc("ze%d" % i, [128, 16, 512], BF16) for i in range(2)]
            zo = [A.alloc("zo%d" % i, [128, 16, 512], BF16) for i in range(2)]
            AA = {n: A.alloc(n, [128, 16, 512], BF16) for n in ("a0r", "a0i", "a1r", "a1i")}
            any_ = A.alloc("any", [1, 2, 512], BF16)
            cb = [A.alloc("cb%d" % i, [128, 16, 128], BF16) for i in range(2)]
            sb = [A.alloc("sb%d" % i, [128, 16, 128], BF16) for i in range(2)]
            tw = A.alloc("tw", [128, 51], F32)
            T = {n: A.alloc(n, [128, 512], F32) for n in ("wor", "woi", "t1", "t2", "x1r", "x1i", "x2r", "x2i",
                                                            "y1r", "y1i", "y2r", "y2i", "u1", "u2")}
            Hh = [A.alloc("hh%d" % i, [128, 512], F32) for i in range(4)]
            dbt = A.alloc("dbt", [128, 512], F32)
            gte = [A.alloc("gte%d" % i, [128, 512], BF16) for i in range(2)]
            zot = [A.alloc("zot%d" % i, [128, 512], BF16) for i in range(2)]
            P.dma("sync", tw, I["twid"], [], ["tw"])
            ck = [("D:ctm", 0, cc) for cc in range(4)]
            P.dma("sync", ze[0], eo_view(ctm[0], 0), ck, ["ze0"])
            P.dma("sync", zo[0], eo_view(ctm[0], 1), ck, ["zo0"])
            for o in range(2):
                zin = (ze[o], zo[o])
                zk = ["ze%d" % o, "zo%d" % o]
                P.dma("sync", dbt, I["hy_bias"][l, o], [], ["dbt"])
                for kc in range(17):
                    M = fwd_chunk(kc, zin[0], zin[1], zk, cb, sb, T, tw)
                    for q in range(4):
                        P.dma("sync", Hh[q][0:M, :], Hsp[o, q, kc * 128:kc * 128 + M, :], [("D:Hsp", o, q, kc)], ["hh%d" % q])
                    R = lambda n: T[n][0:M, :]
                    H1r, H1i, H2r, H2i = (Hh[q][0:M, :] for q in range(4))
                    for (xr, xi, hr, hi, yr, yi, e1, e2) in (("x1r", "x1i", H1r, H1i, "y1r", "y1i", "vector", "gpsimd"),
                                                             ("x2r", "x2i", H2r, H2i, "y2r", "y2i", "gpsimd", "vector")):
                        hq = ["hh0", "hh1"] if yr == "y1r" else ["hh2", "hh3"]
                        P.tt(e1, R("u1"), R(xr), hr, ALU.mult, [xr] + hq, ["u1"])
                        P.tt(e1, R("u2"), R(xi), hi, ALU.mult, [xi] + hq, ["u2"])
                        P.tt(e1, R(yr), R("u1"), R("u2"), ALU.subtract, ["u1", "u2"], [yr])
                        P.tt(e2, R("t1"), R(xr), hi, ALU.mult, [xr] + hq, ["t1"])
                        P.tt(e2, R("t2"), R(xi), hr, ALU.mult, [xi] + hq, ["t2"])
                        P.tt(e2, R(yi), R("t1"), R("t2"), ALU.add, ["t1", "t2"], [yi])
                    twr = tw[0:M, kc * 3:kc * 3 + 1]
                    tws = tw[0:M, kc * 3 + 1:kc * 3 + 2]
                    twn = tw[0:M, kc * 3 + 2:kc * 3 + 3]
                    if kc < 16:
                        P.tt("vector", AA["a0r"][:, kc, :], R("y1r"), R("y2r"), ALU.add, ["y1r", "y2r"], [("a0r", kc)])
                        P.tt("gpsimd", AA["a0i"][:, kc, :], R("y1i"), R("y2i"), ALU.subtract, ["y1i", "y2i"], [("a0i", kc)])
                        P.tt("vector", R("u1"), R("y1r"), R("y2r"), ALU.subtract, ["y1r", "y2r"], ["u1"])
                        P.tt("gpsimd", R("u2"), R("y1i"), R("y2i"), ALU.add, ["y1i", "y2i"], ["u2"])
                        P.ts("vector", R("t1"), R("u2"), twn, None, ALU.mult, None, ["u2", "tw"], ["t1"])
                        P.stt(AA["a1r"][:, kc, :], R("u1"), twr, R("t1"), ALU.mult, ALU.add, ["u1", "tw", "t1"], [("a1r", kc)])
                        P.ts("vector", R("t2"), R("u1"), tws, None, ALU.mult, None, ["u1", "tw"], ["t2"])
                        P.stt(AA["a1i"][:, kc, :], R("u2"), twr, R("t2"), ALU.mult, ALU.add, ["u2", "tw", "t2"], [("a1i", kc)])
                        if kc == 0:
                            for n in ("a0r", "a1r"):
                                P.ts("vector", AA[n][0:1, 0, :], AA[n][0:1, 0, :], 0.5, None, ALU.mult, None, [(n, 0)], [(n, 0)])
                    else:
                        P.copy("vector", any_[0:1, 0, :], R("y1r"), ["y1r"], ["any"])
                        P.ts("vector", any_[0:1, 1, :], R("y1i"), -1.0, None, ALU.mult, None, ["y1i", "any"], ["any"])
                akeys = {n: [(n, kc) for kc in range(16)] for n in AA}
                for v in range(2):
                    ar, ai = ("a0r", "a0i") if v == 0 else ("a1r", "a1i")
                    zv = zin[v]
                    for uc in range(16):
                        b = uc % 2
                        table_loads(cb, sb, b, uc)
                        gsrc = ctm[1 + o].rearrange("(uc p two) c -> two p uc c", p=128, two=2)[v][:, uc, :]
                        P.dma("sync", gte[b], gsrc, [("D:ctm", 1 + o, cc) for cc in range(4)], ["gte%d" % b])
                        bk = 4 + b
                        for kc in range(16):
                            P.mm(ps[bk][:], cb[b][:, kc, :], AA[ar][:, kc, :], kc == 0, False, ["cb%d" % b] + akeys[ar], [PK[bk]])
                        for kc in range(16):
                            P.mm(ps[bk][:], sb[b][:, kc, :], AA[ai][:, kc, :], False, False, ["sb%d" % b] + akeys[ai], [PK[bk]])
                        P.mm(ps[bk][:], altrow[0:1, :], any_[0:1, v, :], False, True, ["K:altrow", "any"], [PK[bk]])
                        P.tt("gpsimd", T["u1"], dbt, zv[:, uc, :], ALU.mult, ["dbt", zk[v]], ["u1"])
                        P.stt(T["u2"], ps[bk][:], 2.0 / NFFT, T["u1"], ALU.mult, ALU.add, [PK[bk], "u1"], ["u2"])
                        if o == 0:
                            dstz = (ze[1], zo[1])[v]
                            P.tt("vector", dstz[:, uc, :], T["u2"], gte[b], ALU.mult, ["u2", "gte%d" % b], [("ze1", "zo1")[v]])
                        else:
                            P.tt("vector", zot[b], T["u2"], gte[b], ALU.mult, ["u2", "gte%d" % b], ["zot%d" % b])
                            dsty = yhtm.rearrange("(uc p two) c -> two p uc c", p=128, two=2)[v][:, uc, :]
                            P.dma("gpsimd", dsty, zot[b], ["zot%d" % b], [("D:yhtm", v, uc)])
            ytk = [("D:yhtm", v, uc) for v in range(2) for uc in range(16)]
            for tc in range(32):
                b = tc % 2
                P.dma("sync", gte[b], yhtm[tc * 128:(tc + 1) * 128, :], ytk, ["gte%d" % b])
                pb = psb(6 + b)
                for cc in range(4):
                    P.tr(pb[:, cc * 128:(cc + 1) * 128], gte[b][:, cc * 128:(cc + 1) * 128], ident[:],
                         ["gte%d" % b, "K:ident"], [PK[6 + b]])
                ysv = zot[b].rearrange("p (c t) -> p c t", c=4)
                P.copy("scalar", ysv, pb[:, 0:512].rearrange("p (c t) -> p c t", c=4), [PK[6 + b]], ["zot%d" % b])
                dst = yhT.rearrange("(cc p) t -> p cc t", p=128)[:, :, tc * 128:(tc + 1) * 128]
                P.dma("gpsimd", dst, ysv, ["zot%d" % b], [("D:yhT", tc)])

        def phase_C(l):
            A.reset()
            lam_init = 0.8 - 0.6 * math.exp(-0.3 * l)
            lv = A.alloc("lv", [128, 256], F32)
            lj = A.alloc("lj", [128, 64], F32)
            gs = A.alloc("gs", [128, 128], F32)
            QT = [A.alloc("QT%d" % i, [128, S], BF16) for i in range(2)]
            KT = [A.alloc("KT%d" % i, [128, S], BF16) for i in range(2)]
            Vh = [A.alloc("Vh%d" % i, [128, 32, 130], BF16) for i in range(2)]
            Et = [A.alloc("Et%d" % i, [128, 512], BF16) for i in range(3)]
            oc = [[A.alloc("oc%d_%d" % (c, q), [128, 128], F32) for q in range(4)] for c in range(2)]
            rc = A.alloc("rc", [128, 8], F32)
            od = A.alloc("od", [128, 128], F32)
            oj = A.alloc("oj", [128, 128], BF16)
            s3 = A.alloc("s3a", [128, 4], F32)
            yb = A.alloc("yb", [128, 128], BF16)
            ydst = [A.alloc("ydst%d" % i, [128, 512], BF16) for i in range(2)]
            P.dma("sync", lv, I["lamv"][l], [], ["lv"])
            P.dma("sync", gs, I["subln"][l], [], ["gs"])
            P.tt("vector", lj, lv[:, 0:64], lv[:, 64:128], ALU.mult, ["lv"], ["lj"])
            P.add("vector", lambda e: e.tensor_reduce(lamt[:, 0:1], lj, AX.X, ALU.add), ["lj"], ["K:lamt"])
            P.tt("vector", lj, lv[:, 128:192], lv[:, 192:256], ALU.mult, ["lv", "lj"], ["lj"])
            P.add("vector", lambda e: e.tensor_reduce(lamt[:, 1:2], lj, AX.X, ALU.add), ["lj", "K:lamt"], ["K:lamt"])
            P.act(lamt[:, 3:5], lamt[:, 0:2], AF.Exp, ["K:lamt"], ["K:lamt"])
            P.tt("vector", lamt[:, 5:6], lamt[:, 4:5], lamt[:, 3:4], ALU.subtract, ["K:lamt"], ["K:lamt"])
            P.ts("vector", lamt[:, 2:3], lamt[:, 5:6], -lam_init, None, ALU.add, None, ["K:lamt"], ["K:lamt"])
            P.ts("vector", gs, gs, 1.0 - lam_init, None, ALU.mult, None, ["gs"], ["gs"])
            scale = 64 ** -0.5
            ecnt = [0]
            for h in range(4):
                hb_ = h % 2
                P.dma("sync", QT[hb_], qkT[h * 128:(h + 1) * 128, :], [("D:qkT", h, tg) for tg in range(8)], ["QT%d" % hb_])
                P.dma("sync", KT[hb_], qkT[512 + h * 128:512 + (h + 1) * 128, :], [("D:qkT", 4 + h, tg) for tg in range(8)],
                      ["KT%d" % hb_])
                P.dma("sync", Vh[hb_][:, :, 0:128], Vtm.rearrange("(kt p) c -> p kt c", p=128)[:, :, h * 128:(h + 1) * 128],
                      [("D:Vtm", j) for j in range(NT)], [("Vh%d" % hb_, 0)])
                P.memset("gpsimd", Vh[hb_][:, :, 128:129], 1.0, [("Vh%d" % hb_, 1)])
                vk = [("Vh%d" % hb_, 0), ("Vh%d" % hb_, 1)]
                for qg in range(8):
                    qs_ = slice(qg * 512, (qg + 1) * 512)
                    for c in range(2):
                        pr = slice(c * 64, (c + 1) * 64)
                        def score(kt):
                            P.mm(ps[kt % 2][:], KT[hb_][pr, kt * 128:(kt + 1) * 128], QT[hb_][pr, qs_], True, True,
                                 ["KT%d" % hb_, "QT%d" % hb_], [PK[kt % 2]])
                        score(0)
                        for kt in range(32):
                            sb_ = kt % 2
                            if kt + 1 < 32:
                                score(kt + 1)
                            ei = ecnt[0] % 3
                            ecnt[0] += 1
                            P.act(Et[ei], ps[sb_][:], AF.Exp, [PK[sb_]], ["Et%d" % ei], scale=scale)
                            for q4 in range(4):
                                P.mm(ps[2 + q4][:, 0:129], Et[ei][:, q4 * 128:(q4 + 1) * 128], Vh[hb_][:, kt, 0:129],
                                     kt == 0, kt == 31, ["Et%d" % ei] + vk, [PK[2 + q4]])
                        for q4 in range(4):
                            P.add("vector", (lambda q4=q4, c=c: lambda e: e.reciprocal(rc[:, c * 4 + q4:c * 4 + q4 + 1],
                                                                                       ps[2 + q4][:, 128:129]))(),
                                  [PK[2 + q4]], [("rc", c, q4)])
                            P.ts("vector", oc[c][q4], ps[2 + q4][:, 0:128], rc[:, c * 4 + q4:c * 4 + q4 + 1], None, ALU.mult, None,
                                 [PK[2 + q4], ("rc", c, q4)], ["oc%d_%d" % (c, q4)])
                    yi = qg % 2
                    pb = psb(6)
                    for q4 in range(4):
                        P.stt(od, oc[1][q4], lamt[:, 2:3], oc[0][q4], ALU.mult, ALU.add,
                              ["oc1_%d" % q4, "oc0_%d" % q4, "K:lamt"], ["od"])
                        rms_tile(od, "od", oj, s3, "s3a", 128, 1e-5)
                        P.stt(yb, od, s3[:, 2:3], gs, ALU.mult, ALU.mult, ["od", "s3a", "gs"], ["yb"])
                        P.tr(pb[:, q4 * 128:(q4 + 1) * 128], yb, ident[:], ["yb", "K:ident"], [PK[6]])
                    P.copy("scalar", ydst[yi], pb[:, 0:512], [PK[6]], ["ydst%d" % yi])
                    P.dma("gpsimd", ydT[h * 128:(h + 1) * 128, qs_], ydst[yi], ["ydst%d" % yi], [("D:ydT", h, qg)])

        def phase_D(l):
            A.reset()
            wuh = A.alloc("wuh", [128, 4, D], BF16)
            wua = A.alloc("wua", [128, 4, D], BF16)
            wo = A.alloc("wo", [128, 8, D], BF16)
            wst = [A.alloc("wst%d" % i, [128, 4, 512], F32) for i in range(2)]
            wstk = ["wst0", "wst1"]
            yh = [A.alloc("yh%d" % i, [128, 4, 512], BF16) for i in range(2)]
            yd = [A.alloc("yd%d" % i, [128, 4, 512], BF16) for i in range(2)]
            gg = [A.alloc("gg%d" % i, [128, 16, 512], BF16) for i in range(2)]
            mT = [A.alloc("mT%d" % i, [128, 8, 512], BF16) for i in range(2)]
            ta = A.alloc("ta", [128, 512], F32)
            tb = A.alloc("tb", [128, 512], F32)
            xt = [A.alloc("xt%d" % i, [128, D], F32) for i in range(2)]
            xo = [A.alloc("xo%d" % i, [128, D], F32) for i in range(2)]
            for (dst, nm, src) in ((wuh, "wuh", I["w_up_hy"][l]), (wua, "wua", I["w_up_da"][l])):
                sv = src.rearrange("(c p) f -> p c f", p=128)
                for h in range(2):
                    load_w(dst[:, :, h * 512:(h + 1) * 512], (nm, h), sv[:, :, h * 512:(h + 1) * 512], wst, wstk)
            sv = I["w_out"][l].rearrange("(c p) f -> p c f", p=128)
            for c2 in range(2):
                for h in range(2):
                    load_w(wo[:, c2 * 4:(c2 + 1) * 4, h * 512:(h + 1) * 512], ("wo", c2, h),
                           sv[:, c2 * 4:(c2 + 1) * 4, h * 512:(h + 1) * 512], wst, wstk)
            wok = [("wo", c2, h) for c2 in range(2) for h in range(2)]
            xcnt = [0]
            for tg in range(8):
                b = tg % 2
                sl = slice(tg * 512, (tg + 1) * 512)
                P.dma("sync", yh[b], yhT.rearrange("(cc p) t -> p cc t", p=128)[:, :, sl],
                      [("D:yhT", tc) for tc in range(tg * 4, tg * 4 + 4)], ["yh%d" % b])
                P.dma("sync", yd[b], ydT.rearrange("(cc p) t -> p cc t", p=128)[:, :, sl],
                      [("D:ydT", h, tg) for h in range(4)], ["yd%d" % b])
                P.dma("sync", gg[b], gT.rearrange("(cc p) t -> p cc t", p=128)[:, :, sl],
                      [("D:gT", gc, tg) for gc in range(16)], ["gg%d" % b])
                for dm in range(8):
                    for cc in range(4):
                        P.mm(ps[0][:], wuh[:, cc, dm * 128:(dm + 1) * 128], yh[b][:, cc, :], cc == 0, cc == 3,
                             [("wuh", 0), ("wuh", 1), "yh%d" % b], [PK[0]])
                    for cc in range(4):
                        P.mm(ps[1][:], wua[:, cc, dm * 128:(dm + 1) * 128], yd[b][:, cc, :], cc == 0, cc == 3,
                             [("wua", 0), ("wua", 1), "yd%d" % b], [PK[1]])
                    P.tt("vector", ta, ps[0][:], gg[b][:, dm, :], ALU.mult, [PK[0], "gg%d" % b], ["ta"])
                    P.tt("vector", tb, ps[1][:], gg[b][:, 8 + dm, :], ALU.mult, [PK[1], "gg%d" % b], ["tb"])
                    P.tt("gpsimd", mT[b][:, dm, :], ta, tb, ALU.add, ["ta", "tb"], [("mT%d" % b, dm)])
                mk = [("mT%d" % b, dm) for dm in range(8)]
                for tt_ in range(4):
                    j = tg * 4 + tt_
                    xb = xcnt[0] % 2
                    xcnt[0] += 1
                    P.dma("sync", xt[xb], xres[j * 128:(j + 1) * 128, :], [("D:xres", j)], ["xt%d" % xb])
                    for og in range(2):
                        bk = 2 + og
                        for dm in range(8):
                            P.mm(ps[bk][:], mT[b][:, dm, tt_ * 128:(tt_ + 1) * 128], wo[:, dm, og * 512:(og + 1) * 512],
                                 dm == 0, dm == 7, mk + wok, [PK[bk]])
                        P.tt("vector", xo[xb][:, og * 512:(og + 1) * 512], ps[bk][:], xt[xb][:, og * 512:(og + 1) * 512], ALU.add,
                             [PK[bk], "xt%d" % xb], [("xo%d" % xb, og)])
                    P.dma("gpsimd", xres[j * 128:(j + 1) * 128, :], xo[xb], [("xo%d" % xb, 0), ("xo%d" % xb, 1)], [("D:xres", j)])

        def phase_E1(l):
            A.reset()
            gt = A.alloc("gt", [128, D], F32)
            wr = A.alloc("wr", [128, 8, 16], F32)
            br = A.alloc("br", [128, 16], F32)
            xt = [A.alloc("xt%d" % i, [128, D], F32) for i in range(2)]
            junk = A.alloc("junk", [128, D], BF16)
            xn = [A.alloc("xn%d" % i, [128, D], F32) for i in range(2)]
            nb = [A.alloc("nb%d" % i, [128, D], BF16) for i in range(2)]
            xT = [A.alloc("xT%d" % i, [128, 8, 128], F32) for i in range(2)]
            s3 = [A.alloc("s3%d" % i, [128, 4], F32) for i in range(2)]
            lg = A.alloc("lg", [128, 32, 16], F32)
            mx = A.alloc("mx", [128, 32], F32)
            ex = A.alloc("ex", [128, 32, 16], F32)
            aff = A.alloc("aff", [128, 32, 16], F32)
            lo = A.alloc("lo", [128, 16], F32)
            mid = A.alloc("mid", [128, 16], F32)
            cmp_ = A.alloc("cmp", [128, 32, 16], F32)
            cnt = A.alloc("cnt", [128, 16], F32)
            onesm = A.alloc("onesm", [128, 128], F32)
            ge = A.alloc("ge", [128, 16], F32)
            mask = A.alloc("mask", [128, 16, 32], F32)
            maskb = A.alloc("maskb", [128, 16, 32], BF16)
            csum = A.alloc("csum", [128, 16, 32], F32)
            onesf = A.alloc("onesf", [128, 512], F32)
            base = A.alloc("base", [128, 16], F32)
            mcum = A.alloc("mcum", [128, 16, 32], F32)
            mcumb = A.alloc("mcumb", [128, 16, 32], BF16)
            ptmp = A.alloc("ptmp", [128, 16, 32], F32)
            ahi = A.alloc("ahi", [128, 32, 16], BF16)
            P.dma("sync", gt, I["norm_ffn"][l], [], ["gt"])
            P.dma("sync", wr, I["w_router"][l].rearrange("(c p) e -> p c e", p=128), [], ["wr"])
            P.dma("sync", br, I["b_router"][l], [], ["br"])
            for j in range(NT):
                b = j % 2
                P.dma("sync", xt[b], xres[j * 128:(j + 1) * 128, :], [("D:xres", j)], ["xt%d" % b])
                rms_tile(xt[b], "xt%d" % b, junk, s3[b], "s3%d" % b, D, 1e-6)
                P.stt(xn[b], xt[b], s3[b][:, 2:3], gt, ALU.mult, ALU.mult, ["xt%d" % b, "s3%d" % b, "gt"], ["xn%d" % b])
                P.copy("gpsimd", nb[b], xn[b], ["xn%d" % b], ["nb%d" % b])
                P.dma("gpsimd", n2d[j * 128:(j + 1) * 128, :], nb[b], ["nb%d" % b], [("D:n2d", j)])
                for c in range(8):
                    bk = 4 + 2 * b + c // 4
                    P.tr(ps[bk][:, (c % 4) * 128:(c % 4 + 1) * 128], xn[b][:, c * 128:(c + 1) * 128], identf[:],
                         ["xn%d" % b, "K:identf"], [PK[bk]])
                P.copy("scalar", xT[b][:, 0:4, :], ps[4 + 2 * b][:].rearrange("p (c t) -> p c t", c=4), [PK[4 + 2 * b]],
                       [("xT%d" % b, 0)])
                P.copy("vector", xT[b][:, 4:8, :], ps[5 + 2 * b][:].rearrange("p (c t) -> p c t", c=4), [PK[5 + 2 * b]],
                       [("xT%d" % b, 1)])
                for c in range(8):
                    P.mm(ps[b][:, 0:16], xT[b][:, c, :], wr[:, c, :], c == 0, c == 7,
                         [("xT%d" % b, 0), ("xT%d" % b, 1), "wr"], [PK[b]])
                P.tt("vector", lg[:, j, :], ps[b][:, 0:16], br, ALU.add, [PK[b], "br"], ["lg"])
            P.add("vector", lambda e: e.tensor_reduce(mx, lg, AX.X, ALU.max), ["lg"], ["mx"])
            P.tt("vector", ex, lg, mx.unsqueeze(2).broadcast_to([128, 32, 16]), ALU.subtract, ["lg", "mx"], ["ex"])
            P.act(ex, ex, AF.Exp, ["ex"], ["ex"])
            P.add("vector", lambda e: e.tensor_reduce(mx, ex, AX.X, ALU.add), ["ex"], ["mx"])
            P.add("vector", lambda e: e.reciprocal(mx, mx), ["mx"], ["mx"])
            P.tt("vector", aff, ex, mx.unsqueeze(2).broadcast_to([128, 32, 16]), ALU.mult, ["ex", "mx"], ["aff"])
            P.memset("vector", lo, 0.0, ["lo"])
            P.memset("vector", onesf, 1.0, ["onesf"])
            P.memset("vector", onesm, 1.0, ["onesm"])
            for it in range(28):
                hstep = 0.5 ** (it + 1)
                P.ts("vector", mid, lo, hstep, None, ALU.add, None, ["lo"], ["mid"])
                P.tt("vector", cmp_, aff, mid.unsqueeze(1).broadcast_to([128, 32, 16]), ALU.is_ge, ["aff", "mid"], ["cmp"])
                P.add("vector", lambda e: e.tensor_reduce(cnt, cmp_.rearrange("p j e -> p e j"), AX.X, ALU.add), ["cmp"], ["cnt"])
                P.mm(ps[2][:, 0:16], onesm, cnt, True, True, ["onesm", "cnt"], [PK[2]])
                P.ts("vector", ge, ps[2][:, 0:16], 511.5, hstep, ALU.is_ge, ALU.mult, [PK[2]], ["ge"])
                P.tt("vector", lo, lo, ge, ALU.add, ["lo", "ge"], ["lo"])
            P.tt("vector", mask, aff.rearrange("p j e -> p e j"), lo.unsqueeze(2).broadcast_to([128, 16, 32]), ALU.is_ge,
                 ["aff", "lo"], ["mask"])
            P.copy("vector", maskb, mask, ["mask"], ["maskb"])
            mflat = mask.rearrange("p e j -> p (e j)")
            cflat = csum.rearrange("p e j -> p (e j)")
            P.add("vector", lambda e: e.tensor_tensor_scan(cflat, onesf, mflat, 0.0, ALU.mult, ALU.add), ["mask", "onesf"], ["csum"])
            P.memset("vector", base[:, 0:1], 0.0, [("base", 0)])
            P.copy("vector", base[:, 1:16], csum[:, 0:15, 31], ["csum"], [("base", 1)])
            P.tt("vector", mcum, csum, mask, ALU.subtract, ["csum", "mask"], ["mcum"])
            P.tt("vector", mcum, mcum, base.unsqueeze(2).broadcast_to([128, 16, 32]), ALU.subtract,
                 ["mcum", ("base", 0), ("base", 1)], ["mcum"])
            P.copy("vector", mcumb, mcum, ["mcum"], ["mcumb"])
            P.mm(ps[3][:], ustr[:], maskb.rearrange("p e j -> p (e j)"), True, False, ["K:ustr", "maskb"], [PK[3]])
            P.mm(ps[3][:], ones[:], mcumb.rearrange("p e j -> p (e j)"), False, True, ["K:ones", "mcumb"], [PK[3]])
            pf = ptmp.rearrange("p e j -> p (e j)")
            P.ts("vector", pf, ps[3][:], 1.0, None, ALU.add, None, [PK[3]], ["ptmp"])
            P.tt("vector", pf, pf, mflat, ALU.mult, ["ptmp", "mask"], ["ptmp"])
            P.ts("vector", posm[:].rearrange("p e j -> p (e j)"), pf, -1.0, None, ALU.add, None, ["ptmp"], ["K:posm"])
            jpt = A.alloc("jpt", [128, 32, 2], BF16)
            P.dma("sync", jpt, I["jp"].rearrange("p (j two) -> p j two", two=2), [], ["jpt"])
            P.copy("vector", affhl[:, :, :, 2:4], jpt.unsqueeze(2).broadcast_to([128, 32, 16, 2]), ["jpt", "K:affhl"], ["K:affhl"])
            P.copy("vector", ahi, aff, ["aff"], ["ahi"])
            P.copy("vector", affhl[:, :, :, 0], ahi, ["ahi"], ["K:affhl"])
            P.tt("vector", affhl[:, :, :, 1], aff, ahi, ALU.subtract, ["aff", "ahi", "K:affhl"], ["K:affhl"])
            if dbg:
                P.dma("gpsimd", dbgaff, aff.rearrange("p j e -> p (j e)"), ["aff"], ["D:dbgaff"])
                P.dma("gpsimd", dbgpos, posm[:].rearrange("p e j -> p (e j)"), ["K:posm"], ["D:dbgpos"])

        def phase_E2(l):
            A.reset()
            Sel = A.alloc("Sel", [128, 32, 512], BF16)
            xgt = A.alloc("xgt", [128, 4, D], BF16)
            xg = A.alloc("xg", [128, 8, 512], BF16)
            hT = A.alloc("hT", [128, 16, 512], BF16)
            yy = [A.alloc("yy%d" % i, [128, 4, D], F32) for i in range(2)]
            wsl = [A.alloc("wsl%d" % i, [128, 2048], BF16) for i in range(6)]
            wst = [A.alloc("wst%d" % i, [128, 2048], F32) for i in range(3)]
            wstk = ["wst0", "wst1", "wst2"]
            iot = A.alloc("iot", [128, 512], F32)
            g16 = A.alloc("g16", [128, 16], F32)
            gsl = A.alloc("gsl", [128, 4], F32)
            idxf = A.alloc("idxf", [128, 4], F32)
            idxi = [A.alloc("idxi%d" % i, [128, 4], I32) for i in range(2)]
            sg = [A.alloc("sg%d" % i, [128, 512], F32) for i in range(2)]
            P.dma("sync", iot, I["iota"], [], ["iot"])
            n2k = [("D:n2d", j) for j in range(NT)]
            xk = [("D:xres", j) for j in range(NT)]
            wc = [0]

            def wload(src3, shape3):
                i = wc[0] % 6
                wc[0] += 1
                names = {"a": shape3[0], "b": shape3[1]}
                dstv = wsl[i].rearrange("p (a b) -> p a b", **names)
                si = wcnt[0] % 3
                stv = [wst[k].rearrange("p (a b) -> p a b", **names) for k in range(3)]
                load_w(dstv, "wsl%d" % i, src3, stv, wstk)
                return dstv, "wsl%d" % i

            for e_ in range(16):
                eb = e_ % 2
                for j in range(32):
                    P.ts("vector", Sel[:, j, :], iot, posm[:, e_, j:j + 1], None, ALU.is_equal, None,
                         ["iot", "K:posm"], [("Sel", j // 8)])
                selk = [("Sel", q) for q in range(4)]
                for sc in range(4):
                    for j in range(32):
                        P.mm(ps[7][:, sc * 4:sc * 4 + 4], Sel[:, j, sc * 128:(sc + 1) * 128], affhl[:, j, e_, :], j == 0, j == 31,
                             selk + ["K:affhl"], [PK[7]])
                P.copy("vector", g16, ps[7][:, 0:16], [PK[7]], ["g16"])
                g4 = g16.rearrange("p (s f) -> p s f", f=4)
                P.tt("vector", gsl, g4[:, :, 0], g4[:, :, 1], ALU.add, ["g16"], ["gsl"])
                P.stt(idxf, g4[:, :, 2], 128.0, g4[:, :, 3], ALU.mult, ALU.add, ["g16"], ["idxf"])
                P.copy("vector", idxi[eb], idxf, ["idxf"], ["idxi%d" % eb])
                for sc in range(4):
                    P.add("gpsimd", (lambda sc=sc, eb=eb: lambda e: e.indirect_dma_start(
                        out=xgt[:, sc, :], out_offset=None, in_=n2d,
                        in_offset=bass.IndirectOffsetOnAxis(ap=idxi[eb][:, sc:sc + 1], axis=0)))(),
                        ["idxi%d" % eb] + n2k, [("xgt", sc)], dma=True)
                for dc2 in range(4):
                    bk = 2 + (dc2 % 2)
                    pb = psb(bk)
                    for h in range(2):
                        dc = dc2 * 2 + h
                        for sc in range(4):
                            P.tr(pb[:, h * 512 + sc * 128:h * 512 + (sc + 1) * 128], xgt[:, sc, dc * 128:(dc + 1) * 128], ident[:],
                                 [("xgt", sc), "K:ident"], [PK[bk]])
                    P.copy("scalar" if dc2 % 2 == 0 else "vector", xg[:, dc2 * 2:dc2 * 2 + 2, :],
                           pb.rearrange("p (h s) -> p h s", h=2), [PK[bk]], [("xg", dc2)])
                xgk = [("xg", q) for q in range(4)]
                gsrc = I["w_e_gate"][l, e_].rearrange("(c p) f -> p c f", p=128)
                usrc = I["w_e_up"][l, e_].rearrange("(c p) f -> p c f", p=128)
                for fg in range(8):
                    wg, wgk = wload(gsrc[:, :, fg * 256:(fg + 1) * 256], (8, 256))
                    wu, wuk = wload(usrc[:, :, fg * 256:(fg + 1) * 256], (8, 256))
                    for fc in range(2):
                        for dc in range(8):
                            P.mm(ps[4][:], wg[:, dc, fc * 128:(fc + 1) * 128], xg[:, dc, :], dc == 0, dc == 7, [wgk] + xgk, [PK[4]])
                        for dc in range(8):
                            P.mm(ps[5][:], wu[:, dc, fc * 128:(fc + 1) * 128], xg[:, dc, :], dc == 0, dc == 7, [wuk] + xgk, [PK[5]])
                        si = fc
                        P.act(sg[si], ps[4][:], AF.Silu, [PK[4]], ["sg%d" % si])
                        P.tt("vector", hT[:, fg * 2 + fc, :], sg[si], ps[5][:], ALU.mult, ["sg%d" % si, PK[5]], [("hT", fg * 2 + fc)])
                hk = [("hT", f) for f in range(16)]
                dsrc = I["w_e_down"][l, e_].rearrange("(c p) d -> p c d", p=128)
                for dq in range(8):
                    wd, wdk = wload(dsrc[:, :, dq * 128:(dq + 1) * 128], (16, 128))
                    bk = 6 if dq % 2 == 0 else 1
                    for sc in range(4):
                        for fc in range(16):
                            P.mm(ps[bk][:, sc * 128:(sc + 1) * 128], hT[:, fc, sc * 128:(sc + 1) * 128], wd[:, fc, :], fc == 0, fc == 15,
                                 hk + [wdk], [PK[bk]])
                    for sc in range(4):
                        P.ts("vector", yy[eb][:, sc, dq * 128:(dq + 1) * 128], ps[bk][:, sc * 128:(sc + 1) * 128], gsl[:, sc:sc + 1], None,
                             ALU.mult, None, [PK[bk], "gsl"], [("yy%d" % eb, sc)])
                for sc in range(4):
                    P.add("gpsimd", (lambda sc=sc, eb=eb: lambda e: e.indirect_dma_start(
                        out=xres, out_offset=bass.IndirectOffsetOnAxis(ap=idxi[eb][:, sc:sc + 1], axis=0),
                        in_=yy[eb][:, sc, :], in_offset=None, compute_op=ALU.add))(),
                        ["idxi%d" % eb, ("yy%d" % eb, sc)] + xk, xk, dma=True)

        def phase_F():
            A.reset()
            gt = A.alloc("gt", [128, D], F32)
            xt = [A.alloc("xt%d" % i, [128, D], F32) for i in range(2)]
            junk = A.alloc("junk", [128, D], BF16)
            xo = [A.alloc("xo%d" % i, [128, D], F32) for i in range(2)]
            s3 = [A.alloc("s3%d" % i, [128, 4], F32) for i in range(2)]
            P.dma("sync", gt, I["norm_final"], [], ["gt"])
            for j in range(NT):
                b = j % 2
                P.dma("sync", xt[b], xres[j * 128:(j + 1) * 128, :], [("D:xres", j)], ["xt%d" % b])
                rms_tile(xt[b], "xt%d" % b, junk, s3[b], "s3%d" % b, D, 1e-6)
                P.stt(xo[b], xt[b], s3[b][:, 2:3], gt, ALU.mult, ALU.mult, ["xt%d" % b, "s3%d" % b, "gt"], ["xo%d" % b])
                P.dma("gpsimd", out[j * 128:(j + 1) * 128, :], xo[b], ["xo%d" % b], [("D:out", j)])

        for l in range(depth):
            if "A" in stages:
                phase_A(l)
            if "B0" in stages:
                phase_B0(l)
            if "B1" in stages:
                phase_B1(l)
            if "B2" in stages:
                phase_B2(l)
            if "C" in stages:
                phase_C(l)
            if "D" in stages:
                phase_D(l)
            if "E" in stages:
                phase_E1(l)
                phase_E2(l)
        if "F" in stages:
            phase_F()
        fin = [k for k in P.state if (k if isinstance(k, str) else k[0]).startswith("D:")]
        P.add("sync", lambda e: e.nop(), fin, [])
        P.add("gpsimd", lambda e: e.nop(), fin, [])
        P.emit(st)
        print("ops", len(P.ops), "maxsem", P.maxsem, flush=True)
    return nc


_CONST = {}


def _consts():
    if _CONST:
        return _CONST
    bf = ml_dtypes.bfloat16
    half = 32
    inv = (10000.0 ** (-np.arange(half, dtype=np.float32) * 2.0 / 64)).astype(np.float32)
    pos = np.arange(S, dtype=np.float32)
    ang = pos[:, None] * inv[None, :]
    cos = np.cos(ang).astype(np.float32).T
    sin = np.sin(ang).astype(np.float32).T
    cosT = np.zeros((128, S), np.float32)
    sinT = np.zeros((128, S), np.float32)
    for p in range(128):
        i = p % 32
        cosT[p] = cos[i]
        sinT[p] = -sin[i] if (p % 64) < 32 else sin[i]
    _CONST["cosT"] = cosT
    _CONST["sinT"] = sinT
    L = S
    t = np.linspace(0.0, 1.0, L, dtype=np.float32)
    bands = 16
    w = (2.0 * np.float32(math.pi) * np.arange(L, dtype=np.float32) / L).astype(np.float32)
    fr = np.linspace(1e-4, bands - 1, bands, dtype=np.float32)
    a = w[:, None] * fr[None, :]
    z = np.concatenate([t[:, None], np.cos(a), -np.sin(a)], axis=-1).astype(np.float32)
    _CONST["zT"] = np.ascontiguousarray(z.T)
    mind = math.log(1e-2) / 0.3
    maxd = math.log(1e-2) / 1.5
    deltas = np.abs(np.linspace(mind, maxd, 512, dtype=np.float32))
    decay = np.exp(-t[:, None] * deltas[None, :]).astype(np.float32)
    _CONST["decay"] = decay
    db = decay.copy()
    db[0] = 0.0
    _CONST["decayb"] = db
    n = np.arange(2048, dtype=np.int64)
    prod = (n[:, None] * n[None, :]) % 4096
    angd = prod.astype(np.float64) * (2.0 * math.pi / 4096)
    for nm, fn in (("ctab", np.cos), ("stab", lambda v: -np.sin(v))):
        m = fn(angd).astype(np.float32)
        m = m.reshape(16, 128, 16, 128)
        m = np.ascontiguousarray(m.transpose(2, 1, 0, 3)).reshape(16, 128, 16 * 128)
        _CONST[nm] = m.astype(bf)
    kk = np.arange(17 * 128, dtype=np.float64)
    ph = kk * (2.0 * math.pi / NFFT)
    tw = np.stack([np.cos(ph), np.sin(ph), -np.sin(ph)], axis=-1).astype(np.float32)
    _CONST["twid"] = np.ascontiguousarray(tw.reshape(17, 128, 3).transpose(1, 0, 2)).reshape(128, 51)
    altv = np.where(np.arange(128) % 2 == 0, 1.0, -1.0).astype(np.float32)
    _CONST["alt"] = np.stack([altv, altv], axis=1).astype(bf)
    _CONST["altrow"] = altv[None, :].astype(bf)
    _CONST["iota"] = np.broadcast_to(np.arange(512, dtype=np.float32)[None, :], (128, 512)).copy()
    jp = np.zeros((128, 32, 2), np.float32)
    jp[:, :, 0] = np.arange(32, dtype=np.float32)[None, :]
    jp[:, :, 1] = np.arange(128, dtype=np.float32)[:, None]
    _CONST["jp"] = jp.reshape(128, 64).astype(bf)
    _CONST["ident_bf"] = np.eye(128, dtype=np.float32).astype(bf)
    _CONST["ident_f"] = np.eye(128, dtype=np.float32)
    _CONST["ustrict"] = np.triu(np.ones((128, 128), np.float32), 1).astype(bf)
    _CONST["ones_bf"] = np.ones((128, 128), np.float32).astype(bf)
    return _CONST


def _rep(v, n=128):
    return np.ascontiguousarray(np.broadcast_to(v[..., None, :], v.shape[:-1] + (n, v.shape[-1])))


def prep_shared(inp):
    c = dict(_consts())
    w_in = np.asarray(inp["w_in"])
    b_in = np.asarray(inp["b_in"])
    sw = np.concatenate([np.arange(h * 64 + 32, h * 64 + 64).tolist() + np.arange(h * 64, h * 64 + 32).tolist()
                         for h in range(8)]).astype(np.int64)
    u0, q0, k0, v0, gh0, ga0 = 0, 1536, 2048, 2560, 3072, 4096
    cols = np.concatenate([np.arange(u0, u0 + 1536), np.arange(q0, q0 + 512), np.arange(k0, k0 + 512),
                           q0 + sw, k0 + sw, np.arange(gh0, gh0 + 1024), np.arange(ga0, ga0 + 1024),
                           np.arange(v0, v0 + 512)])
    c["w_in"] = np.ascontiguousarray(w_in[:, :, cols])
    bext = b_in[:, cols]
    c["b_in"] = np.ascontiguousarray(bext.reshape(2, 48, 128).transpose(0, 2, 1))
    c["b_v"] = _rep(b_in[:, v0:v0 + 512])
    c["norm_mix"] = _rep(np.asarray(inp["norm_mix"]))
    c["norm_ffn"] = _rep(np.asarray(inp["norm_ffn"]))
    c["norm_final"] = _rep(np.asarray(inp["norm_final"]))
    cw = np.asarray(inp["hy_conv_w"])
    c["hy_cw"] = np.ascontiguousarray(cw.reshape(2, 3, 12, 128).transpose(0, 3, 2, 1)).reshape(2, 128, 36)
    c["hy_cb"] = np.ascontiguousarray(np.asarray(inp["hy_conv_b"]).reshape(2, 12, 128).transpose(0, 2, 1))
    c["hy_w1"] = np.asarray(inp["hy_ffn_w1"])
    c["hy_w2"] = np.asarray(inp["hy_ffn_w2"])
    c["hy_w3"] = np.asarray(inp["hy_ffn_w3"])
    c["hy_cols"] = np.ascontiguousarray(np.stack([np.asarray(inp["hy_ffn_b1"]), np.asarray(inp["hy_ffn_f1"]),
                                                  np.asarray(inp["hy_ffn_b2"]), np.asarray(inp["hy_ffn_f2"])], axis=-1))
    c["hy_bias"] = _rep(np.asarray(inp["hy_bias"]))
    lam = np.concatenate([np.asarray(inp["lambda_q1"]), np.asarray(inp["lambda_k1"]),
                          np.asarray(inp["lambda_q2"]), np.asarray(inp["lambda_k2"])], axis=-1)
    c["lamv"] = _rep(lam)
    c["subln"] = _rep(np.asarray(inp["subln_g"]))
    c["w_up_hy"] = np.asarray(inp["w_up_hyena"])
    c["w_up_da"] = np.asarray(inp["w_up_attn"])
    c["w_out"] = np.asarray(inp["w_out"])
    c["w_router"] = np.asarray(inp["w_router"])
    c["b_router"] = _rep(np.asarray(inp["b_router"]))
    c["w_e_gate"] = np.asarray(inp["w_e_gate"])
    c["w_e_up"] = np.asarray(inp["w_e_up"])
    c["w_e_down"] = np.asarray(inp["w_e_down"])
    return {k: np.ascontiguousarray(v) for k, v in c.items()}


def kernel(**inputs):
    x = np.asarray(inputs["x"], dtype=np.float32)
    shared = prep_shared(inputs)
    nc = build(bass.Bass("TRN2", target_bir_lowering=False))
    in_maps = []
    for core in range(8):
        m = dict(shared)
        m["x"] = np.ascontiguousarray(x[core % 4])
        in_maps.append(m)
    res = run_bass_kernel_spmd(nc, in_maps, core_ids=list(range(8)))
    return np.stack([np.asarray(res.results[b]["out"], dtype=np.float32) for b in range(4)], axis=0)
```

```python
import numpy as np
import concourse.bass as bass
import concourse.mybir as mybir

F32 = mybir.dt.float32
BF16 = mybir.dt.bfloat16
I32 = mybir.dt.int32
AF = mybir.ActivationFunctionType
ALU = mybir.AluOpType
AX = mybir.AxisListType

COMPUTE = ("tensor", "vector", "scalar", "gpsimd")
NDSEM = 8


class Op:
    __slots__ = ("id", "eng", "fn", "deps", "signal", "semval", "is_dma", "dsem", "dval", "dprev")


class Prog:
    def __init__(self, nc):
        self.nc = nc
        self.ops = []
        self.state = {}

    def _prune(self, ids):
        best = {}
        out = set()
        for i in ids:
            o = self.ops[i]
            if o.is_dma:
                out.add(i)
            else:
                if o.eng not in best or best[o.eng] < i:
                    best[o.eng] = i
        out.update(best.values())
        return out

    def add(self, eng, fn, r=(), w=(), dma=False):
        o = Op()
        o.id = len(self.ops)
        o.eng = eng
        o.fn = fn
        o.is_dma = dma
        o.signal = dma
        deps = set()
        for k in tuple(r) + tuple(w):
            if k not in self.state:
                nm = k if isinstance(k, str) else k[0]
                if not (nm.startswith("D:") or nm.startswith("K:")):
                    self.state[k] = [None, list(getattr(self, "_pend", []))]
            st = self.state.get(k)
            if st is not None and st[0] is not None:
                deps.add(st[0])
        for k in w:
            st = self.state.get(k)
            if st is not None:
                deps.update(st[1])
        o.deps = self._prune(deps)
        self.ops.append(o)
        for k in r:
            st = self.state.setdefault(k, [None, []])
            st[1].append(o.id)
            if len(st[1]) > 24:
                st[1] = list(self._prune(st[1]))
        for k in w:
            self.state[k] = [o.id, []]
        return o

    def phase_barrier(self, newkeys, oldprefix=None):
        pend = set()
        for k, st in list(self.state.items()):
            nm = k if isinstance(k, str) else k[0]
            if nm.startswith("D:") or nm.startswith("K:"):
                continue
            if st[0] is not None:
                pend.add(st[0])
            pend.update(st[1])
            del self.state[k]
        pend = list(self._prune(pend))
        self._pend = pend

    def fresh(self, key):
        self.state[key] = [None, list(getattr(self, "_pend", []))]

    def emit(self, stack):
        nc = self.nc
        engs = ["tensor", "vector", "scalar", "gpsimd", "sync"]
        sems = {e: stack.enter_context(nc.semaphore("s_" + e)) for e in COMPUTE}
        dsems = {e: [stack.enter_context(nc.semaphore("d_%s%d" % (e, i))) for i in range(NDSEM)]
                 for e in engs}
        for o in self.ops:
            for d in o.deps:
                p = self.ops[d]
                if p.is_dma:
                    continue
                if p.eng == "tensor" and o.eng == "tensor" and not o.is_dma:
                    continue
                p.signal = True
        cnt = {e: 0 for e in COMPUTE}
        dcnt = {e: 0 for e in engs}
        dval = {e: [0] * NDSEM for e in engs}
        for o in self.ops:
            if o.is_dma:
                i = dcnt[o.eng] % NDSEM
                dcnt[o.eng] += 1
                o.dsem = dsems[o.eng][i]
                o.dprev = dval[o.eng][i]
                dval[o.eng][i] += 16
                o.dval = dval[o.eng][i]
            elif o.signal:
                cnt[o.eng] += 1
                o.semval = cnt[o.eng]
        self.maxsem = dict(cnt)
        block = stack.enter_context(nc.Block())
        byeng = {e: [o for o in self.ops if o.eng == e] for e in engs}

        def run(engname, eobj):
            waited = {}

            def wait(sem, val):
                key = id(sem)
                if waited.get(key, 0) >= val:
                    return
                waited[key] = val
                eobj.wait_ge(sem, val)

            for o in byeng[engname]:
                for d in sorted(o.deps):
                    p = self.ops[d]
                    if p.is_dma:
                        wait(p.dsem, p.dval)
                    else:
                        if p.eng == "tensor" and engname == "tensor" and not o.is_dma:
                            continue
                        wait(sems[p.eng], p.semval)
                if o.is_dma:
                    if o.dprev > 0:
                        wait(o.dsem, o.dprev)
                    inst = o.fn(eobj)
                    inst.then_inc(o.dsem, 16)
                else:
                    inst = o.fn(eobj)
                    if o.signal:
                        inst.then_inc(sems[engname], 1)

        @block.tensor
        def _(e):
            run("tensor", e)

        @block.vector
        def _(e):
            run("vector", e)

        @block.scalar
        def _(e):
            run("scalar", e)

        @block.gpsimd
        def _(e):
            run("gpsimd", e)

        @block.sync
        def _(e):
            run("sync", e)

    def mm(self, out, lhsT, rhs, start, stop, r, w):
        return self.add("tensor", lambda e: e.matmul(out, lhsT, rhs, start=start, stop=stop), r, w)

    def tr(self, out, in_, ident, r, w):
        return self.add("tensor", lambda e: e.transpose(out, in_, ident), r, w)

    def act(self, out, in_, func, r, w, bias=None, scale=None, accum=None):
        kw = {}
        if bias is not None:
            kw["bias"] = bias
        if scale is not None:
            kw["scale"] = scale
        if accum is not None:
            kw["accum_out"] = accum
        return self.add("scalar", lambda e: e.activation(out, in_, func, **kw), r, w)

    def ts(self, eng, out, in0, s1, s2, op0, op1, r, w):
        if op1 is None:
            return self.add(eng, lambda e: e.tensor_scalar(out, in0, s1, None, op0), r, w)
        return self.add(eng, lambda e: e.tensor_scalar(out, in0, s1, s2, op0, op1), r, w)

    def tt(self, eng, out, in0, in1, op, r, w):
        return self.add(eng, lambda e: e.tensor_tensor(out, in0, in1, op), r, w)

    def stt(self, out, in0, scalar, in1, op0, op1, r, w):
        return self.add("vector", lambda e: e.scalar_tensor_tensor(out, in0, scalar, in1, op0, op1), r, w)

    def copy(self, eng, out, in_, r, w):
        if eng == "scalar":
            return self.add(eng, lambda e: e.copy(out, in_), r, w)
        return self.add(eng, lambda e: e.tensor_copy(out, in_), r, w)

    def dma(self, eng, out, in_, r, w, **kw):
        return self.add(eng, lambda e: e.dma_start(out, in_, **kw), r, w, dma=True)

    def memset(self, eng, ap, val, w):
        return self.add(eng, lambda e: e.memset(ap, val), (), w)


class Arena:
    def __init__(self, P, tensor, words):
        self.P = P
        self.t = tensor
        self.words = words
        self.off = 0
        self.names = []

    def reset(self):
        self.P.phase_barrier(None)
        self.off = 0

    def alloc(self, name, shape, dtype):
        n = int(np.prod(shape[1:]))
        if dtype == BF16:
            assert n % 2 == 0
            w = n // 2
        else:
            w = n
        assert self.off + w <= self.words, (name, self.off, w, self.words)
        ap = self.t[0:shape[0], self.off:self.off + w]
        if dtype != F32:
            ap = ap.bitcast(dtype)
        self.off += w
        if len(shape) > 2:
            names = " ".join("a%d" % i for i in range(len(shape) - 1))
            kw = {"a%d" % i: shape[i + 1] for i in range(len(shape) - 1)}
            ap = ap.rearrange("p (%s) -> p %s" % (names, names), **kw)
        self.P.fresh(name)
        return ap

from contextlib import ExitStack
import math
import ml_dtypes
from concourse.bass_utils import run_bass_kernel_spmd

S = 4096
D = 1024
NT = 32
ARENA = 49152
TWO_PI = 2.0 * math.pi
MAGIC = 12582912.0
PI_LO = 3.1415925
NFFT = 8192


def build(nc, dbg=False, stages=("A", "B0", "B1", "B2", "C", "D", "E", "F"), depth=2):
    def din(name, shape, dt=F32):
        return nc.dram_tensor(name, list(shape), dt, kind="ExternalInput").ap()

    skind = "ExternalOutput" if dbg else "Internal"

    def dscr(name, shape, dt):
        return nc.dram_tensor(name, list(shape), dt, kind=skind).ap()

    I = {}
    I["x"] = din("x", [S, D])
    I["w_in"] = din("w_in", [2, D, 6144])
    I["b_in"] = din("b_in", [2, 128, 48])
    I["b_v"] = din("b_v", [2, 128, 512])
    I["norm_mix"] = din("norm_mix", [2, 128, D])
    I["norm_ffn"] = din("norm_ffn", [2, 128, D])
    I["norm_final"] = din("norm_final", [128, D])
    I["cosT"] = din("cosT", [128, S])
    I["sinT"] = din("sinT", [128, S])
    I["hy_cw"] = din("hy_cw", [2, 128, 36])
    I["hy_cb"] = din("hy_cb", [2, 128, 12])
    I["zT"] = din("zT", [33, S])
    I["hy_w1"] = din("hy_w1", [2, 33, 64])
    I["hy_w2"] = din("hy_w2", [2, 64, 64])
    I["hy_w3"] = din("hy_w3", [2, 64, 2048])
    I["hy_cols"] = din("hy_cols", [2, 64, 4])
    I["decay"] = din("decay", [S, 512])
    I["decayb"] = din("decayb", [S, 512])
    I["hy_bias"] = din("hy_bias", [2, 2, 128, 512])
    I["ctab"] = din("ctab", [16, 128, 16 * 128], BF16)
    I["stab"] = din("stab", [16, 128, 16 * 128], BF16)
    I["twid"] = din("twid", [128, 17 * 3])
    I["alt"] = din("alt", [128, 2], BF16)
    I["altrow"] = din("altrow", [1, 128], BF16)
    I["lamv"] = din("lamv", [2, 128, 256])
    I["subln"] = din("subln", [2, 128, 128])
    I["w_up_hy"] = din("w_up_hy", [2, 512, D])
    I["w_up_da"] = din("w_up_da", [2, 512, D])
    I["w_out"] = din("w_out", [2, D, D])
    I["w_router"] = din("w_router", [2, D, 16])
    I["b_router"] = din("b_router", [2, 128, 16])
    I["w_e_gate"] = din("w_e_gate", [2, 16, D, 2048])
    I["w_e_up"] = din("w_e_up", [2, 16, D, 2048])
    I["w_e_down"] = din("w_e_down", [2, 16, 2048, D])
    I["iota"] = din("iota", [128, 512])
    I["jp"] = din("jp", [128, 64], BF16)
    I["ident_bf"] = din("ident_bf", [128, 128], BF16)
    I["ident_f"] = din("ident_f", [128, 128])
    I["ustrict"] = din("ustrict", [128, 128], BF16)
    I["ones_bf"] = din("ones_bf", [128, 128], BF16)
    out = nc.dram_tensor("out", [S, D], F32, kind="ExternalOutput").ap()

    xres = dscr("xres", [S, D], F32)
    uT = dscr("uT", [1536, S], BF16)
    qkT = dscr("qkT", [1024, S], BF16)
    gT = dscr("gT", [2048, S], BF16)
    Vtm = dscr("Vtm", [S, 512], BF16)
    Hsp = dscr("Hsp", [2, 4, 17 * 128, 512], F32)
    hsd = dscr("hsd", [2, S, 512], BF16)
    yhtm = dscr("yhtm", [S, 512], BF16)
    ctm = dscr("ctm", [3, S, 512], BF16)
    yhT = dscr("yhT", [512, S], BF16)
    ydT = dscr("ydT", [512, S], BF16)
    n2d = dscr("n2d", [S, D], BF16)
    dbgaff = dscr("dbgaff", [128, 32 * 16], F32)
    dbgpos = dscr("dbgpos", [128, 16 * 32], F32)

    st = ExitStack()
    with st:
        arena_t = st.enter_context(nc.sbuf_tensor("arena", [128, ARENA], F32))
        ident = st.enter_context(nc.sbuf_tensor("ident", [128, 128], BF16))
        identf = st.enter_context(nc.sbuf_tensor("identf", [128, 128], F32))
        ustr = st.enter_context(nc.sbuf_tensor("ustr", [128, 128], BF16))
        ones = st.enter_context(nc.sbuf_tensor("ones", [128, 128], BF16))
        alt = st.enter_context(nc.sbuf_tensor("altc", [128, 2], BF16))
        altrow = st.enter_context(nc.sbuf_tensor("altr", [1, 128], BF16))
        posm = st.enter_context(nc.sbuf_tensor("posm", [128, 16, 32], F32))
        affhl = st.enter_context(nc.sbuf_tensor("affhl", [128, 32, 16, 4], BF16))
        lamt = st.enter_context(nc.sbuf_tensor("lamt", [128, 8], F32))
        ps = [st.enter_context(nc.psum_tensor("ps%d" % i, [128, 512], F32)) for i in range(8)]
        PK = ["K:ps%d" % i for i in range(8)]
        P = Prog(nc)
        A = Arena(P, arena_t, ARENA)

        def psb(i):
            return ps[i][:].bitcast(BF16)

        P.dma("sync", ident[:], I["ident_bf"], [], ["K:ident"])
        P.dma("sync", identf[:], I["ident_f"], [], ["K:identf"])
        P.dma("sync", ustr[:], I["ustrict"], [], ["K:ustr"])
        P.dma("sync", ones[:], I["ones_bf"], [], ["K:ones"])
        P.dma("sync", alt[:], I["alt"], [], ["K:alt"])
        P.dma("sync", altrow[:], I["altrow"], [], ["K:altrow"])
        for q in range(4):
            P.dma("sync", xres[q * 1024:(q + 1) * 1024, :], I["x"][q * 1024:(q + 1) * 1024, :], [],
                  [("D:xres", j) for j in range(q * 8, q * 8 + 8)])

        def rms_tile(xt_ap, kx, junk, stt3, ks, n, eps):
            P.act(junk, xt_ap, AF.Square, [kx], [ks + "j", ks], accum=stt3[:, 0:1])
            P.ts("vector", stt3[:, 1:2], stt3[:, 0:1], 1.0 / n, eps, ALU.mult, ALU.add, [ks], [ks])
            P.act(stt3[:, 1:2], stt3[:, 1:2], AF.Sqrt, [ks], [ks])
            P.add("vector", lambda e: e.reciprocal(stt3[:, 2:3], stt3[:, 1:2]), [ks], [ks])

        wcnt = [0]

        def load_w(dst, dkey, src, stg, stgk):
            i = wcnt[0] % len(stg)
            wcnt[0] += 1
            P.dma("sync", stg[i], src, [], [stgk[i]])
            ceng = ("scalar", "vector", "scalar")[wcnt[0] % 3]
            P.copy(ceng, dst, stg[i], [stgk[i]], [dkey])

        def phase_A(l):
            A.reset()
            nT = A.alloc("nT", [128, 8, S], BF16)
            cosT = A.alloc("cosT", [128, S], F32)
            sinT = A.alloc("sinT", [128, S], F32)
            gt = A.alloc("gt", [128, D], F32)
            bcol = A.alloc("bcol", [128, 48], F32)
            bv = A.alloc("bv", [128, 512], F32)
            xt = [A.alloc("xt%d" % i, [128, D], F32) for i in range(2)]
            junk = A.alloc("junk", [128, D], BF16)
            xn = [A.alloc("xn%d" % i, [128, D], BF16) for i in range(2)]
            s3 = [A.alloc("s3%d" % i, [128, 4], F32) for i in range(2)]
            wst = [A.alloc("wst%d" % i, [128, 8, 256], F32) for i in range(2)]
            wstk = ["wst0", "wst1"]
            wt = [A.alloc("wt%d" % i, [128, 8, 512], BF16) for i in range(2)]
            stg = [A.alloc("stg%d" % i, [128, 512], BF16) for i in range(3)]
            tq = [A.alloc("tq%d" % i, [128, 512], F32) for i in range(2)]
            P.dma("sync", gt, I["norm_mix"][l], [], ["gt"])
            P.dma("sync", bcol, I["b_in"][l], [], ["bcol"])
            P.dma("sync", bv, I["b_v"][l], [], ["bv"])
            P.dma("sync", cosT, I["cosT"], [], ["cosT"])
            P.dma("sync", sinT, I["sinT"], [], ["sinT"])
            for j in range(NT):
                b = j % 2
                P.dma("sync", xt[b], xres[j * 128:(j + 1) * 128, :], [("D:xres", j)], ["xt%d" % b])
                rms_tile(xt[b], "xt%d" % b, junk, s3[b], "s3%d" % b, D, 1e-6)
                P.stt(xn[b], xt[b], s3[b][:, 2:3], gt, ALU.mult, ALU.mult, ["xt%d" % b, "s3%d" % b, "gt"], ["xn%d" % b])
                pb = psb(6 + b)
                for c in range(8):
                    P.tr(pb[:, c * 128:(c + 1) * 128], xn[b][:, c * 128:(c + 1) * 128], ident[:],
                         ["xn%d" % b, "K:ident"], [PK[6 + b]])
                P.copy("scalar", nT[:, :, j * 128:(j + 1) * 128], pb.rearrange("p (c t) -> p c t", c=8),
                       [PK[6 + b]], [("nT", j // 4)])
            wsrc = I["w_in"][l].rearrange("(c p) f -> p c f", p=128)
            gcnt = [0]

            def load_group(g):
                b = gcnt[0] % 2
                gcnt[0] += 1
                for h in range(2):
                    load_w(wt[b][:, :, h * 256:(h + 1) * 256], ("wt%d" % b, h),
                           wsrc[:, :, g * 512 + h * 256: g * 512 + (h + 1) * 256], wst, wstk)
                return b

            def wkeys(b):
                return [("wt%d" % b, 0), ("wt%d" % b, 1)]

            bankc = [0]
            stc = [0]

            def proj_fm(b, fc, tg):
                bk = bankc[0] % 4
                bankc[0] += 1
                for kc in range(8):
                    P.mm(ps[bk][:], wt[b][:, kc, fc * 128:(fc + 1) * 128], nT[:, kc, tg * 512:(tg + 1) * 512],
                         kc == 0, kc == 7, wkeys(b) + [("nT", tg)], [PK[bk]])
                return bk

            for g in [0, 1, 2, 7, 8, 9, 10]:
                b = load_group(g)
                for fc in range(4):
                    ch = g * 4 + fc
                    for tg in range(8):
                        bk = proj_fm(b, fc, tg)
                        si = stc[0] % 3
                        stc[0] += 1
                        func = AF.Identity if g < 3 else AF.Sigmoid
                        P.act(stg[si], ps[bk][:], func, [PK[bk], "bcol"], ["stg%d" % si], bias=bcol[:, ch:ch + 1])
                        if g < 3:
                            dst = uT[ch * 128:(ch + 1) * 128, tg * 512:(tg + 1) * 512]
                            dk = ("D:uT", ch, tg)
                        else:
                            gc = ch - 28
                            dst = gT[gc * 128:(gc + 1) * 128, tg * 512:(tg + 1) * 512]
                            dk = ("D:gT", gc, tg)
                        P.dma("gpsimd", dst, stg[si], ["stg%d" % si], [dk])
            for (ga, gb2, rbase) in [(3, 5, 0), (4, 6, 4)]:
                ba = load_group(ga)
                bb = load_group(gb2)
                for fc in range(4):
                    cha = ga * 4 + fc
                    chb = gb2 * 4 + fc
                    for tg in range(8):
                        bk1 = proj_fm(ba, fc, tg)
                        bk2 = proj_fm(bb, fc, tg)
                        sl = slice(tg * 512, (tg + 1) * 512)
                        P.stt(tq[0], ps[bk1][:], bcol[:, cha:cha + 1], cosT[:, sl], ALU.add, ALU.mult,
                              [PK[bk1], "bcol", "cosT"], ["tq0"])
                        P.stt(tq[1], ps[bk2][:], bcol[:, chb:chb + 1], sinT[:, sl], ALU.add, ALU.mult,
                              [PK[bk2], "bcol", "sinT"], ["tq1"])
                        si = stc[0] % 3
                        stc[0] += 1
                        P.tt("vector", stg[si], tq[0], tq[1], ALU.add, ["tq0", "tq1"], ["stg%d" % si])
                        rc = rbase + fc
                        P.dma("gpsimd", qkT[rc * 128:(rc + 1) * 128, sl], stg[si], ["stg%d" % si], [("D:qkT", rc, tg)])
            b = load_group(11)
            for j in range(NT):
                bk = bankc[0] % 4
                bankc[0] += 1
                for kc in range(8):
                    P.mm(ps[bk][:], nT[:, kc, j * 128:(j + 1) * 128], wt[b][:, kc, :], kc == 0, kc == 7,
                         wkeys(b) + [("nT", j // 4)], [PK[bk]])
                si = stc[0] % 3
                stc[0] += 1
                P.tt("vector", stg[si], ps[bk][:], bv, ALU.add, [PK[bk], "bv"], ["stg%d" % si])
                P.dma("gpsimd", Vtm[j * 128:(j + 1) * 128, :], stg[si], ["stg%d" % si], [("D:Vtm", j)])

        def sin_layer(dst, lhsT, rhs_fn, bcolap, fcolap, kr, tmp, M):
            for tg in range(8):
                bk = tg % 2
                P.mm(ps[bk][0:M, :], lhsT, rhs_fn(tg), True, True, kr, [PK[bk]])
                a, b_, c = tmp
                P.ts("vector", a, ps[bk][0:M, :], bcolap, fcolap, ALU.add, ALU.mult, [PK[bk], "fcols"], ["sa"])
                P.ts("vector", b_, a, 1.0 / TWO_PI, MAGIC, ALU.mult, ALU.add, ["sa"], ["sb"])
                P.ts("vector", b_, b_, MAGIC, None, ALU.subtract, None, ["sb"], ["sb"])
                P.stt(c, b_, -TWO_PI, a, ALU.mult, ALU.add, ["sa", "sb"], ["sc"])
                P.ts("vector", c, c, PI_LO, -PI_LO, ALU.min, ALU.max, ["sc"], ["sc"])
                P.act(dst[:, tg * 512:(tg + 1) * 512], c, AF.Sin, ["sc"], [dst_key[0]])

        dst_key = [None]

        def table_loads(cb, sb, b, idx):
            P.dma("sync", cb[b], I["ctab"][idx].rearrange("p (u k) -> p u k", k=128), [], ["cb%d" % b])
            P.dma("sync", sb[b], I["stab"][idx].rearrange("p (u k) -> p u k", k=128), [], ["sb%d" % b])

        def eo_view(dram2d, v):
            return dram2d.rearrange("(uc p two) c -> two p uc c", p=128, two=2)[v]

        def smul(out, in_, col, r, w):
            P.ts("vector", out, in_, col, None, ALU.mult, None, r, w)

        def fwd_chunk(kc, ze, zo, zk, cb, sb, T, tw, need=("X1r", "X1i", "X2r", "X2i")):
            b = kc % 2
            M = 128 if kc < 16 else 1
            B0_ = 4 * b
            if kc < 16:
                table_loads(cb, sb, b, kc)
                for (bank, tab, tk, zz) in ((B0_, cb, "cb", ze), (B0_ + 1, sb, "sb", ze), (B0_ + 2, cb, "cb", zo), (B0_ + 3, sb, "sb", zo)):
                    for uc in range(16):
                        P.mm(ps[bank][:], tab[b][:, uc, :], zz[:, uc, :], uc == 0, uc == 15, ["%s%d" % (tk, b)] + zk, [PK[bank]])
            else:
                for (bank, zz) in ((B0_, ze), (B0_ + 2, zo)):
                    for uc in range(16):
                        P.mm(ps[bank][0:1, :], alt[:, 0:1], zz[:, uc, :], uc == 0, uc == 15, ["K:alt"] + zk, [PK[bank]])
            Er, Ei, Or, Oi = (ps[B0_ + q][0:M, :] for q in range(4))
            kE, kEi, kO, kOi = (PK[B0_ + q] for q in range(4))
            twr = tw[0:M, kc * 3:kc * 3 + 1]
            tws = tw[0:M, kc * 3 + 1:kc * 3 + 2]
            twn = tw[0:M, kc * 3 + 2:kc * 3 + 3]
            wor, woi, t1, t2 = T["wor"][0:M, :], T["woi"][0:M, :], T["t1"][0:M, :], T["t2"][0:M, :]
            if kc < 16:
                smul(t1, Oi, tws, [kOi, "tw"], ["t1"])
                P.stt(wor, Or, twr, t1, ALU.mult, ALU.add, [kO, "tw", "t1"], ["wor"])
                smul(t2, Or, twn, [kO, "tw"], ["t2"])
                P.stt(woi, Oi, twr, t2, ALU.mult, ALU.add, [kOi, "tw", "t2"], ["woi"])
                P.tt("vector", T["x1r"][0:M, :], Er, wor, ALU.add, [kE, "wor"], ["x1r"])
                P.tt("vector", T["x2r"][0:M, :], Er, wor, ALU.subtract, [kE, "wor"], ["x2r"])
                P.tt("vector", T["x1i"][0:M, :], Ei, woi, ALU.add, [kEi, "woi"], ["x1i"])
                P.tt("vector", T["x2i"][0:M, :], woi, Ei, ALU.subtract, ["woi", kEi], ["x2i"])
            else:
                P.copy("vector", T["x1r"][0:1, :], Er, [kE], ["x1r"])
                P.copy("vector", T["x2r"][0:1, :], Er, [kE], ["x2r"])
                P.ts("vector", T["x1i"][0:1, :], Or, -1.0, None, ALU.mult, None, [kO], ["x1i"])
                P.ts("vector", T["x2i"][0:1, :], Or, -1.0, None, ALU.mult, None, [kO], ["x2i"])
            return M

        def phase_B0(l):
            A.reset()
            zT = A.alloc("zT", [33, S], F32)
            w1 = A.alloc("w1", [33, 64], F32)
            w2 = A.alloc("w2", [64, 64], F32)
            w3 = A.alloc("w3", [64, 2048], F32)
            fcols = A.alloc("fcols", [64, 4], F32)
            h1 = A.alloc("h1T", [64, S], F32)
            h2 = A.alloc("h2T", [64, S], F32)
            tmp = [A.alloc(n, [64, 512], F32) for n in ("sa", "sb", "sc")]
            dec = [A.alloc("dec%d" % i, [128, 512], F32) for i in range(2)]
            decb = [A.alloc("decb%d" % i, [128, 512], F32) for i in range(2)]
            hf = A.alloc("hf", [128, 512], F32)
            hb = A.alloc("hb", [128, 512], F32)
            hso = [A.alloc("hso%d" % i, [128, 512], BF16) for i in range(2)]
            hdo = [A.alloc("hdo%d" % i, [128, 512], BF16) for i in range(2)]
            ze = A.alloc("ze", [128, 16, 512], BF16)
            zo = A.alloc("zo", [128, 16, 512], BF16)
            cb = [A.alloc("cb%d" % i, [128, 16, 128], BF16) for i in range(2)]
            sb = [A.alloc("sb%d" % i, [128, 16, 128], BF16) for i in range(2)]
            tw = A.alloc("tw", [128, 51], F32)
            T = {n: A.alloc(n, [128, 512], F32) for n in ("wor", "woi", "t1", "t2", "x1r", "x1i", "x2r", "x2i")}
            P.dma("sync", zT, I["zT"], [], ["zT"])
            P.dma("sync", w1, I["hy_w1"][l], [], ["w1"])
            P.dma("sync", w2, I["hy_w2"][l], [], ["w2"])
            P.dma("sync", w3, I["hy_w3"][l], [], ["w3"])
            P.dma("sync", fcols, I["hy_cols"][l], [], ["fcols"])
            P.dma("sync", tw, I["twid"], [], ["tw"])
            dst_key[0] = "h1T"
            sin_layer(h1, w1, lambda tg: zT[:, tg * 512:(tg + 1) * 512], fcols[:, 0:1], fcols[:, 1:2],
                      ["w1", "zT"], tmp, 64)
            dst_key[0] = "h2T"
            sin_layer(h2, w2, lambda tg: h1[:, tg * 512:(tg + 1) * 512], fcols[:, 2:3], fcols[:, 3:4],
                      ["w2", "h1T"], tmp, 64)
            for o in range(2):
                for jt in range(NT):
                    b = jt % 2
                    P.dma("sync", dec[b], I["decay"][jt * 128:(jt + 1) * 128, :], [], ["dec%d" % b])
                    P.dma("sync", decb[b], I["decayb"][jt * 128:(jt + 1) * 128, :], [], ["decb%d" % b])
                    P.mm(ps[4][:], h2[:, jt * 128:(jt + 1) * 128], w3[:, o * 1024:o * 1024 + 512], True, True,
                         ["h2T", "w3"], [PK[4]])
                    P.mm(ps[5][:], h2[:, jt * 128:(jt + 1) * 128], w3[:, o * 1024 + 512:o * 1024 + 1024], True, True,
                         ["h2T", "w3"], [PK[5]])
                    P.tt("vector", hf, ps[4][:], dec[b], ALU.mult, [PK[4], "dec%d" % b], ["hf"])
                    P.tt("vector", hb, ps[5][:], decb[b], ALU.mult, [PK[5], "decb%d" % b], ["hb"])
                    P.tt("vector", hso[b], hf, hb, ALU.add, ["hf", "hb"], ["hso%d" % b])
                    P.tt("vector", hdo[b], hf, hb, ALU.subtract, ["hf", "hb"], ["hdo%d" % b])
                    P.dma("gpsimd", hsd[0, jt * 128:(jt + 1) * 128, :], hso[b], ["hso%d" % b], [("D:hsd", 0, jt)])
                    P.dma("gpsimd", hsd[1, jt * 128:(jt + 1) * 128, :], hdo[b], ["hdo%d" % b], [("D:hsd", 1, jt)])
                for sig in range(2):
                    hk_ = [("D:hsd", sig, jt) for jt in range(NT)]
                    P.dma("sync", ze, eo_view(hsd[sig], 0), hk_, ["ze"])
                    P.dma("sync", zo, eo_view(hsd[sig], 1), hk_, ["zo"])
                    for kc in range(17):
                        M = fwd_chunk(kc, ze, zo, ["ze", "zo"], cb, sb, T, tw)
                        if sig == 0:
                            P.dma("gpsimd", Hsp[o, 0, kc * 128:kc * 128 + M, :], T["x1r"][0:M, :], ["x1r"], [("D:Hsp", o, 0, kc)])
                            P.dma("gpsimd", Hsp[o, 2, kc * 128:kc * 128 + M, :], T["x2r"][0:M, :], ["x2r"], [("D:Hsp", o, 2, kc)])
                        else:
                            P.dma("gpsimd", Hsp[o, 1, kc * 128:kc * 128 + M, :], T["x1i"][0:M, :], ["x1i"], [("D:Hsp", o, 1, kc)])
                            P.dma("gpsimd", Hsp[o, 3, kc * 128:kc * 128 + M, :], T["x2i"][0:M, :], ["x2i"], [("D:Hsp", o, 3, kc)])

        def phase_B1(l):
            A.reset()
            cw = A.alloc("cw", [128, 36], F32)
            cbi = A.alloc("cbi", [128, 12], F32)
            ub = [A.alloc("ub%d" % i, [128, S], BF16) for i in range(2)]
            acc = [A.alloc("acc%d" % i, [128, S], F32) for i in range(2)]
            ucb = [A.alloc("ucb%d" % i, [128, S], BF16) for i in range(2)]
            ttm = [A.alloc("ttm%d" % i, [128, 32, 128], BF16) for i in range(2)]
            P.dma("sync", cw, I["hy_cw"][l], [], ["cw"])
            P.dma("sync", cbi, I["hy_cb"][l], [], ["cbi"])
            for ch in range(12):
                b = ch % 2
                P.dma("sync", ub[b], uT[ch * 128:(ch + 1) * 128, :], [("D:uT", ch, tg) for tg in range(8)], ["ub%d" % b])
                P.act(acc[b], ub[b], AF.Identity, ["ub%d" % b, "cw", "cbi"], ["acc%d" % b],
                      bias=cbi[:, ch:ch + 1], scale=cw[:, ch * 3 + 1:ch * 3 + 2])
                P.stt(acc[b][:, 1:S], ub[b][:, 0:S - 1], cw[:, ch * 3:ch * 3 + 1], acc[b][:, 1:S], ALU.mult, ALU.add,
                      ["ub%d" % b, "cw", "acc%d" % b], ["acc%d" % b])
                P.stt(ucb[b][:, 0:S - 1], ub[b][:, 1:S], cw[:, ch * 3 + 2:ch * 3 + 3], acc[b][:, 0:S - 1], ALU.mult, ALU.add,
                      ["ub%d" % b, "cw", "acc%d" % b], ["ucb%d" % b])
                P.copy("vector", ucb[b][:, S - 1:S], acc[b][:, S - 1:S], ["acc%d" % b, "ucb%d" % b], ["ucb%d" % b])
                for t8 in range(4):
                    bk = 4 + (t8 % 2)
                    pb = psb(bk)
                    for i in range(8):
                        tc = t8 * 8 + i
                        P.tr(pb[:, i * 128:(i + 1) * 128], ucb[b][:, tc * 128:(tc + 1) * 128], ident[:],
                             ["ucb%d" % b, "K:ident"], [PK[bk]])
                    P.copy("scalar" if t8 % 2 == 0 else "vector", ttm[b][:, t8 * 8:(t8 + 1) * 8, :],
                           pb.rearrange("p (c t) -> p c t", c=8), [PK[bk]], ["ttm%d" % b])
                which = ch // 4
                cc = ch % 4
                dst = ctm[which].rearrange("(tc p) c -> p tc c", p=128)[:, :, cc * 128:(cc + 1) * 128]
                P.dma("gpsimd", dst, ttm[b], ["ttm%d" % b], [("D:ctm", which, cc)])

        def phase_B2(l):
            A.reset()
            ze = [A.alloc("ze%d" % i, [128, 16, 512], BF16) for i in range(2)]
            zo = [A.alloc("zo%d" % i, [128, 16, 512], BF16) for i in range(2)]
            AA = {n: A.alloc(n, [128, 16, 512], BF16) for n in ("a0r", "a0i", "a1r", "a1i")}
            any_ = A.alloc("any", [1, 2, 512], BF16)
            cb = [A.alloc("cb%d" % i, [128, 16, 128], BF16) for i in range(2)]
            sb = [A.alloc("sb%d" % i, [128, 16, 128], BF16) for i in range(2)]
            tw = A.alloc("tw", [128, 51], F32)
            T = {n: A.alloc(n, [128, 512], F32) for n in ("wor", "woi", "t1", "t2", "x1r", "x1i", "x2r", "x2i",
                                                            "y1r", "y1i", "y2r", "y2i", "u1", "u2")}
            Hh = [A.alloc("hh%d" % i, [128, 512], F32) for i in range(4)]
            dbt = A.alloc("dbt", [128, 512], F32)
            gte = [A.alloc("gte%d" % i, [128, 512], BF16) for i in range(2)]
            zot = [A.alloc("zot%d" % i, [128, 512], BF16) for i in range(2)]
            P.dma("sync", tw, I["twid"], [], ["tw"])
            ck = [("D:ctm", 0, cc) for cc in range(4)]
            P.dma("sync", ze[0], eo_view(ctm[0], 0), ck, ["ze0"])
            P.dma("sync", zo[0], eo_view(ctm[0], 1), ck, ["zo0"])
            for o in range(2):
                zin = (ze[o], zo[o])
                zk = ["ze%d" % o, "zo%d" % o]
                P.dma("sync", dbt, I["hy_bias"][l, o], [], ["dbt"])
                for kc in range(17):
                    M = fwd_chunk(kc, zin[0], zin[1], zk, cb, sb, T, tw)
                    for q in range(4):
                        P.dma("sync", Hh[q][0:M, :], Hsp[o, q, kc * 128:kc * 128 + M, :], [("D:Hsp", o, q, kc)], ["hh%d" % q])
                    R = lambda n: T[n][0:M, :]
                    H1r, H1i, H2r, H2i = (Hh[q][0:M, :] for q in range(4))
                    for (xr, xi, hr, hi, yr, yi, e1, e2) in (("x1r", "x1i", H1r, H1i, "y1r", "y1i", "vector", "vector"),
                                                             ("x2r", "x2i", H2r, H2i, "y2r", "y2i", "vector", "vector")):
                        hq = ["hh0", "hh1"] if yr == "y1r" else ["hh2", "hh3"]
                        P.tt(e1, R("u1"), R(xr), hr, ALU.mult, [xr] + hq, ["u1"])
                        P.tt(e1, R("u2"), R(xi), hi, ALU.mult, [xi] + hq, ["u2"])
                        P.tt(e1, R(yr), R("u1"), R("u2"), ALU.subtract, ["u1", "u2"], [yr])
                        P.tt(e2, R("t1"), R(xr), hi, ALU.mult, [xr] + hq, ["t1"])
                        P.tt(e2, R("t2"), R(xi), hr, ALU.mult, [xi] + hq, ["t2"])
                        P.tt(e2, R(yi), R("t1"), R("t2"), ALU.add, ["t1", "t2"], [yi])
                    twr = tw[0:M, kc * 3:kc * 3 + 1]
                    tws = tw[0:M, kc * 3 + 1:kc * 3 + 2]
                    twn = tw[0:M, kc * 3 + 2:kc * 3 + 3]
                    if kc < 16:
                        P.tt("vector", AA["a0r"][:, kc, :], R("y1r"), R("y2r"), ALU.add, ["y1r", "y2r"], [("a0r", kc)])
                        P.tt("vector", AA["a0i"][:, kc, :], R("y1i"), R("y2i"), ALU.subtract, ["y1i", "y2i"], [("a0i", kc)])
                        P.tt("vector", R("u1"), R("y1r"), R("y2r"), ALU.subtract, ["y1r", "y2r"], ["u1"])
                        P.tt("vector", R("u2"), R("y1i"), R("y2i"), ALU.add, ["y1i", "y2i"], ["u2"])
                        smul(R("t1"), R("u2"), twn, ["u2", "tw"], ["t1"])
                        P.stt(AA["a1r"][:, kc, :], R("u1"), twr, R("t1"), ALU.mult, ALU.add, ["u1", "tw", "t1"], [("a1r", kc)])
                        smul(R("t2"), R("u1"), tws, ["u1", "tw"], ["t2"])
                        P.stt(AA["a1i"][:, kc, :], R("u2"), twr, R("t2"), ALU.mult, ALU.add, ["u2", "tw", "t2"], [("a1i", kc)])
                        if kc == 0:
                            for n in ("a0r", "a1r"):
                                P.ts("vector", AA[n][0:1, 0, :], AA[n][0:1, 0, :], 0.5, None, ALU.mult, None, [(n, 0)], [(n, 0)])
                    else:
                        P.copy("vector", any_[0:1, 0, :], R("y1r"), ["y1r"], ["any"])
                        P.ts("vector", any_[0:1, 1, :], R("y1i"), -1.0, None, ALU.mult, None, ["y1i", "any"], ["any"])
                akeys = {n: [(n, kc) for kc in range(16)] for n in AA}
                for v in range(2):
                    ar, ai = ("a0r", "a0i") if v == 0 else ("a1r", "a1i")
                    zv = zin[v]
                    for uc in range(16):
                        b = uc % 2
                        table_loads(cb, sb, b, uc)
                        gsrc = ctm[1 + o].rearrange("(uc p two) c -> two p uc c", p=128, two=2)[v][:, uc, :]
                        P.dma("sync", gte[b], gsrc, [("D:ctm", 1 + o, cc) for cc in range(4)], ["gte%d" % b])
                        bk = 4 + b
                        for kc in range(16):
                            P.mm(ps[bk][:], cb[b][:, kc, :], AA[ar][:, kc, :], kc == 0, False, ["cb%d" % b] + akeys[ar], [PK[bk]])
                        for kc in range(16):
                            P.mm(ps[bk][:], sb[b][:, kc, :], AA[ai][:, kc, :], False, False, ["sb%d" % b] + akeys[ai], [PK[bk]])
                        P.mm(ps[bk][:], altrow[0:1, :], any_[0:1, v, :], False, True, ["K:altrow", "any"], [PK[bk]])
                        P.tt("vector", T["u1"], dbt, zv[:, uc, :], ALU.mult, ["dbt", zk[v]], ["u1"])
                        P.stt(T["u2"], ps[bk][:], 2.0 / NFFT, T["u1"], ALU.mult, ALU.add, [PK[bk], "u1"], ["u2"])
                        if o == 0:
                            dstz = (ze[1], zo[1])[v]
                            P.tt("vector", dstz[:, uc, :], T["u2"], gte[b], ALU.mult, ["u2", "gte%d" % b], [("ze1", "zo1")[v]])
                        else:
                            P.tt("vector", zot[b], T["u2"], gte[b], ALU.mult, ["u2", "gte%d" % b], ["zot%d" % b])
                            dsty = yhtm.rearrange("(uc p two) c -> two p uc c", p=128, two=2)[v][:, uc, :]
                            P.dma("gpsimd", dsty, zot[b], ["zot%d" % b], [("D:yhtm", v, uc)])
            ytk = [("D:yhtm", v, uc) for v in range(2) for uc in range(16)]
            for tc in range(32):
                b = tc % 2
                P.dma("sync", gte[b], yhtm[tc * 128:(tc + 1) * 128, :], ytk, ["gte%d" % b])
                pb = psb(6 + b)
                for cc in range(4):
                    P.tr(pb[:, cc * 128:(cc + 1) * 128], gte[b][:, cc * 128:(cc + 1) * 128], ident[:],
                         ["gte%d" % b, "K:ident"], [PK[6 + b]])
                ysv = zot[b].rearrange("p (c t) -> p c t", c=4)
                P.copy("scalar", ysv, pb[:, 0:512].rearrange("p (c t) -> p c t", c=4), [PK[6 + b]], ["zot%d" % b])
                dst = yhT.rearrange("(cc p) t -> p cc t", p=128)[:, :, tc * 128:(tc + 1) * 128]
                P.dma("gpsimd", dst, ysv, ["zot%d" % b], [("D:yhT", tc)])

        def phase_C(l):
            A.reset()
            lam_init = 0.8 - 0.6 * math.exp(-0.3 * l)
            lv = A.alloc("lv", [128, 256], F32)
            lj = A.alloc("lj", [128, 64], F32)
            gs = A.alloc("gs", [128, 128], F32)
            QT = [A.alloc("QT%d" % i, [128, S], BF16) for i in range(2)]
            KT = [A.alloc("KT%d" % i, [128, S], BF16) for i in range(2)]
            Vh = [A.alloc("Vh%d" % i, [128, 32, 130], BF16) for i in range(2)]
            Et = [A.alloc("Et%d" % i, [128, 512], BF16) for i in range(3)]
            oc = [[A.alloc("oc%d_%d" % (c, q), [128, 128], F32) for q in range(4)] for c in range(2)]
            rc = A.alloc("rc", [128, 8], F32)
            od = A.alloc("od", [128, 128], F32)
            oj = A.alloc("oj", [128, 128], BF16)
            s3 = A.alloc("s3a", [128, 4], F32)
            yb = A.alloc("yb", [128, 128], BF16)
            ydst = [A.alloc("ydst%d" % i, [128, 512], BF16) for i in range(2)]
            P.dma("sync", lv, I["lamv"][l], [], ["lv"])
            P.dma("sync", gs, I["subln"][l], [], ["gs"])
            P.tt("vector", lj, lv[:, 0:64], lv[:, 64:128], ALU.mult, ["lv"], ["lj"])
            P.add("vector", lambda e: e.tensor_reduce(lamt[:, 0:1], lj, AX.X, ALU.add), ["lj"], ["K:lamt"])
            P.tt("vector", lj, lv[:, 128:192], lv[:, 192:256], ALU.mult, ["lv", "lj"], ["lj"])
            P.add("vector", lambda e: e.tensor_reduce(lamt[:, 1:2], lj, AX.X, ALU.add), ["lj", "K:lamt"], ["K:lamt"])
            P.act(lamt[:, 3:5], lamt[:, 0:2], AF.Exp, ["K:lamt"], ["K:lamt"])
            P.tt("vector", lamt[:, 5:6], lamt[:, 4:5], lamt[:, 3:4], ALU.subtract, ["K:lamt"], ["K:lamt"])
            P.ts("vector", lamt[:, 2:3], lamt[:, 5:6], -lam_init, None, ALU.add, None, ["K:lamt"], ["K:lamt"])
            P.ts("vector", gs, gs, 1.0 - lam_init, None, ALU.mult, None, ["gs"], ["gs"])
            scale = 64 ** -0.5
            ecnt = [0]
            for h in range(4):
                hb_ = h % 2
                P.dma("sync", QT[hb_], qkT[h * 128:(h + 1) * 128, :], [("D:qkT", h, tg) for tg in range(8)], ["QT%d" % hb_])
                P.dma("sync", KT[hb_], qkT[512 + h * 128:512 + (h + 1) * 128, :], [("D:qkT", 4 + h, tg) for tg in range(8)],
                      ["KT%d" % hb_])
                P.dma("sync", Vh[hb_][:, :, 0:128], Vtm.rearrange("(kt p) c -> p kt c", p=128)[:, :, h * 128:(h + 1) * 128],
                      [("D:Vtm", j) for j in range(NT)], [("Vh%d" % hb_, 0)])
                P.memset("gpsimd", Vh[hb_][:, :, 128:129], 1.0, [("Vh%d" % hb_, 1)])
                vk = [("Vh%d" % hb_, 0), ("Vh%d" % hb_, 1)]
                for qg in range(8):
                    qs_ = slice(qg * 512, (qg + 1) * 512)
                    for c in range(2):
                        pr = slice(c * 64, (c + 1) * 64)
                        def score(kt):
                            P.mm(ps[kt % 2][:], KT[hb_][pr, kt * 128:(kt + 1) * 128], QT[hb_][pr, qs_], True, True,
                                 ["KT%d" % hb_, "QT%d" % hb_], [PK[kt % 2]])
                        score(0)
                        for kt in range(32):
                            sb_ = kt % 2
                            if kt + 1 < 32:
                                score(kt + 1)
                            ei = ecnt[0] % 3
                            ecnt[0] += 1
                            P.act(Et[ei], ps[sb_][:], AF.Exp, [PK[sb_]], ["Et%d" % ei], scale=scale)
                            for q4 in range(4):
                                P.mm(ps[2 + q4][:, 0:129], Et[ei][:, q4 * 128:(q4 + 1) * 128], Vh[hb_][:, kt, 0:129],
                                     kt == 0, kt == 31, ["Et%d" % ei] + vk, [PK[2 + q4]])
                        for q4 in range(4):
                            P.add("vector", (lambda q4=q4, c=c: lambda e: e.reciprocal(rc[:, c * 4 + q4:c * 4 + q4 + 1],
                                                                                       ps[2 + q4][:, 128:129]))(),
                                  [PK[2 + q4]], [("rc", c, q4)])
                            P.ts("vector", oc[c][q4], ps[2 + q4][:, 0:128], rc[:, c * 4 + q4:c * 4 + q4 + 1], None, ALU.mult, None,
                                 [PK[2 + q4], ("rc", c, q4)], ["oc%d_%d" % (c, q4)])
                    yi = qg % 2
                    pb = psb(6)
                    for q4 in range(4):
                        P.stt(od, oc[1][q4], lamt[:, 2:3], oc[0][q4], ALU.mult, ALU.add,
                              ["oc1_%d" % q4, "oc0_%d" % q4, "K:lamt"], ["od"])
                        rms_tile(od, "od", oj, s3, "s3a", 128, 1e-5)
                        P.stt(yb, od, s3[:, 2:3], gs, ALU.mult, ALU.mult, ["od", "s3a", "gs"], ["yb"])
                        P.tr(pb[:, q4 * 128:(q4 + 1) * 128], yb, ident[:], ["yb", "K:ident"], [PK[6]])
                    P.copy("scalar", ydst[yi], pb[:, 0:512], [PK[6]], ["ydst%d" % yi])
                    P.dma("gpsimd", ydT[h * 128:(h + 1) * 128, qs_], ydst[yi], ["ydst%d" % yi], [("D:ydT", h, qg)])

        def phase_D(l):
            A.reset()
            wuh = A.alloc("wuh", [128, 4, D], BF16)
            wua = A.alloc("wua", [128, 4, D], BF16)
            wo = A.alloc("wo", [128, 8, D], BF16)
            wst = [A.alloc("wst%d" % i, [128, 4, 512], F32) for i in range(2)]
            wstk = ["wst0", "wst1"]
            yh = [A.alloc("yh%d" % i, [128, 4, 512], BF16) for i in range(2)]
            yd = [A.alloc("yd%d" % i, [128, 4, 512], BF16) for i in range(2)]
            gg = [A.alloc("gg%d" % i, [128, 16, 512], BF16) for i in range(2)]
            mT = [A.alloc("mT%d" % i, [128, 8, 512], BF16) for i in range(2)]
            ta = A.alloc("ta", [128, 512], F32)
            tb = A.alloc("tb", [128, 512], F32)
            xt = [A.alloc("xt%d" % i, [128, D], F32) for i in range(2)]
            xo = [A.alloc("xo%d" % i, [128, D], F32) for i in range(2)]
            for (dst, nm, src) in ((wuh, "wuh", I["w_up_hy"][l]), (wua, "wua", I["w_up_da"][l])):
                sv = src.rearrange("(c p) f -> p c f", p=128)
                for h in range(2):
                    load_w(dst[:, :, h * 512:(h + 1) * 512], (nm, h), sv[:, :, h * 512:(h + 1) * 512], wst, wstk)
            sv = I["w_out"][l].rearrange("(c p) f -> p c f", p=128)
            for c2 in range(2):
                for h in range(2):
                    load_w(wo[:, c2 * 4:(c2 + 1) * 4, h * 512:(h + 1) * 512], ("wo", c2, h),
                           sv[:, c2 * 4:(c2 + 1) * 4, h * 512:(h + 1) * 512], wst, wstk)
            wok = [("wo", c2, h) for c2 in range(2) for h in range(2)]
            xcnt = [0]
            for tg in range(8):
                b = tg % 2
                sl = slice(tg * 512, (tg + 1) * 512)
                P.dma("sync", yh[b], yhT.rearrange("(cc p) t -> p cc t", p=128)[:, :, sl],
                      [("D:yhT", tc) for tc in range(tg * 4, tg * 4 + 4)], ["yh%d" % b])
                P.dma("sync", yd[b], ydT.rearrange("(cc p) t -> p cc t", p=128)[:, :, sl],
                      [("D:ydT", h, tg) for h in range(4)], ["yd%d" % b])
                P.dma("sync", gg[b], gT.rearrange("(cc p) t -> p cc t", p=128)[:, :, sl],
                      [("D:gT", gc, tg) for gc in range(16)], ["gg%d" % b])
                for dm in range(8):
                    for cc in range(4):
                        P.mm(ps[0][:], wuh[:, cc, dm * 128:(dm + 1) * 128], yh[b][:, cc, :], cc == 0, cc == 3,
                             [("wuh", 0), ("wuh", 1), "yh%d" % b], [PK[0]])
                    for cc in range(4):
                        P.mm(ps[1][:], wua[:, cc, dm * 128:(dm + 1) * 128], yd[b][:, cc, :], cc == 0, cc == 3,
                             [("wua", 0), ("wua", 1), "yd%d" % b], [PK[1]])
                    P.tt("vector", ta, ps[0][:], gg[b][:, dm, :], ALU.mult, [PK[0], "gg%d" % b], ["ta"])
                    P.tt("vector", tb, ps[1][:], gg[b][:, 8 + dm, :], ALU.mult, [PK[1], "gg%d" % b], ["tb"])
                    P.tt("vector", mT[b][:, dm, :], ta, tb, ALU.add, ["ta", "tb"], [("mT%d" % b, dm)])
                mk = [("mT%d" % b, dm) for dm in range(8)]
                for tt_ in range(4):
                    j = tg * 4 + tt_
                    xb = xcnt[0] % 2
                    xcnt[0] += 1
                    P.dma("sync", xt[xb], xres[j * 128:(j + 1) * 128, :], [("D:xres", j)], ["xt%d" % xb])
                    for og in range(2):
                        bk = 2 + og
                        for dm in range(8):
                            P.mm(ps[bk][:], mT[b][:, dm, tt_ * 128:(tt_ + 1) * 128], wo[:, dm, og * 512:(og + 1) * 512],
                                 dm == 0, dm == 7, mk + wok, [PK[bk]])
                        P.tt("vector", xo[xb][:, og * 512:(og + 1) * 512], ps[bk][:], xt[xb][:, og * 512:(og + 1) * 512], ALU.add,
                             [PK[bk], "xt%d" % xb], [("xo%d" % xb, og)])
                    P.dma("gpsimd", xres[j * 128:(j + 1) * 128, :], xo[xb], [("xo%d" % xb, 0), ("xo%d" % xb, 1)], [("D:xres", j)])

        def phase_E1(l):
            A.reset()
            gt = A.alloc("gt", [128, D], F32)
            wr = A.alloc("wr", [128, 8, 16], F32)
            br = A.alloc("br", [128, 16], F32)
            xt = [A.alloc("xt%d" % i, [128, D], F32) for i in range(2)]
            junk = A.alloc("junk", [128, D], BF16)
            xn = [A.alloc("xn%d" % i, [128, D], F32) for i in range(2)]
            nb = [A.alloc("nb%d" % i, [128, D], BF16) for i in range(2)]
            xT = [A.alloc("xT%d" % i, [128, 8, 128], F32) for i in range(2)]
            s3 = [A.alloc("s3%d" % i, [128, 4], F32) for i in range(2)]
            lg = A.alloc("lg", [128, 32, 16], F32)
            mx = A.alloc("mx", [128, 32], F32)
            ex = A.alloc("ex", [128, 32, 16], F32)
            aff = A.alloc("aff", [128, 32, 16], F32)
            lo = A.alloc("lo", [128, 16], F32)
            mid = A.alloc("mid", [128, 16], F32)
            cmp_ = A.alloc("cmp", [128, 32, 16], F32)
            cnt = A.alloc("cnt", [128, 16], F32)
            onesm = A.alloc("onesm", [128, 128], F32)
            ge = A.alloc("ge", [128, 16], F32)
            mask = A.alloc("mask", [128, 16, 32], F32)
            maskb = A.alloc("maskb", [128, 16, 32], BF16)
            csum = A.alloc("csum", [128, 16, 32], F32)
            onesf = A.alloc("onesf", [128, 512], F32)
            base = A.alloc("base", [128, 16], F32)
            mcum = A.alloc("mcum", [128, 16, 32], F32)
            mcumb = A.alloc("mcumb", [128, 16, 32], BF16)
            ptmp = A.alloc("ptmp", [128, 16, 32], F32)
            ahi = A.alloc("ahi", [128, 32, 16], BF16)
            P.dma("sync", gt, I["norm_ffn"][l], [], ["gt"])
            P.dma("sync", wr, I["w_router"][l].rearrange("(c p) e -> p c e", p=128), [], ["wr"])
            P.dma("sync", br, I["b_router"][l], [], ["br"])
            for j in range(NT):
                b = j % 2
                P.dma("sync", xt[b], xres[j * 128:(j + 1) * 128, :], [("D:xres", j)], ["xt%d" % b])
                rms_tile(xt[b], "xt%d" % b, junk, s3[b], "s3%d" % b, D, 1e-6)
                P.stt(xn[b], xt[b], s3[b][:, 2:3], gt, ALU.mult, ALU.mult, ["xt%d" % b, "s3%d" % b, "gt"], ["xn%d" % b])
                P.copy("scalar", nb[b], xn[b], ["xn%d" % b], ["nb%d" % b])
                P.dma("gpsimd", n2d[j * 128:(j + 1) * 128, :], nb[b], ["nb%d" % b], [("D:n2d", j)])
                for c in range(8):
                    bk = 4 + 2 * b + c // 4
                    P.tr(ps[bk][:, (c % 4) * 128:(c % 4 + 1) * 128], xn[b][:, c * 128:(c + 1) * 128], identf[:],
                         ["xn%d" % b, "K:identf"], [PK[bk]])
                P.copy("scalar", xT[b][:, 0:4, :], ps[4 + 2 * b][:].rearrange("p (c t) -> p c t", c=4), [PK[4 + 2 * b]],
                       [("xT%d" % b, 0)])
                P.copy("vector", xT[b][:, 4:8, :], ps[5 + 2 * b][:].rearrange("p (c t) -> p c t", c=4), [PK[5 + 2 * b]],
                       [("xT%d" % b, 1)])
                for c in range(8):
                    P.mm(ps[b][:, 0:16], xT[b][:, c, :], wr[:, c, :], c == 0, c == 7,
                         [("xT%d" % b, 0), ("xT%d" % b, 1), "wr"], [PK[b]])
                P.tt("vector", lg[:, j, :], ps[b][:, 0:16], br, ALU.add, [PK[b], "br"], ["lg"])
            P.add("vector", lambda e: e.tensor_reduce(mx, lg, AX.X, ALU.max), ["lg"], ["mx"])
            P.tt("vector", ex, lg, mx.unsqueeze(2).broadcast_to([128, 32, 16]), ALU.subtract, ["lg", "mx"], ["ex"])
            P.act(ex, ex, AF.Exp, ["ex"], ["ex"])
            P.add("vector", lambda e: e.tensor_reduce(mx, ex, AX.X, ALU.add), ["ex"], ["mx"])
            P.add("vector", lambda e: e.reciprocal(mx, mx), ["mx"], ["mx"])
            P.tt("vector", aff, ex, mx.unsqueeze(2).broadcast_to([128, 32, 16]), ALU.mult, ["ex", "mx"], ["aff"])
            P.memset("vector", lo, 0.0, ["lo"])
            P.memset("vector", onesf, 1.0, ["onesf"])
            P.memset("vector", onesm, 1.0, ["onesm"])
            for it in range(28):
                hstep = 0.5 ** (it + 1)
                P.ts("vector", mid, lo, hstep, None, ALU.add, None, ["lo"], ["mid"])
                P.tt("vector", cmp_, aff, mid.unsqueeze(1).broadcast_to([128, 32, 16]), ALU.is_ge, ["aff", "mid"], ["cmp"])
                P.add("vector", lambda e: e.tensor_reduce(cnt, cmp_.rearrange("p j e -> p e j"), AX.X, ALU.add), ["cmp"], ["cnt"])
                P.mm(ps[2][:, 0:16], onesm, cnt, True, True, ["onesm", "cnt"], [PK[2]])
                P.ts("vector", ge, ps[2][:, 0:16], 511.5, hstep, ALU.is_ge, ALU.mult, [PK[2]], ["ge"])
                P.tt("vector", lo, lo, ge, ALU.add, ["lo", "ge"], ["lo"])
            P.tt("vector", mask, aff.rearrange("p j e -> p e j"), lo.unsqueeze(2).broadcast_to([128, 16, 32]), ALU.is_ge,
                 ["aff", "lo"], ["mask"])
            P.copy("vector", maskb, mask, ["mask"], ["maskb"])
            mflat = mask.rearrange("p e j -> p (e j)")
            cflat = csum.rearrange("p e j -> p (e j)")
            P.add("vector", lambda e: e.tensor_tensor_scan(cflat, onesf, mflat, 0.0, ALU.mult, ALU.add), ["mask", "onesf"], ["csum"])
            P.memset("vector", base[:, 0:1], 0.0, [("base", 0)])
            P.copy("vector", base[:, 1:16], csum[:, 0:15, 31], ["csum"], [("base", 1)])
            P.tt("vector", mcum, csum, mask, ALU.subtract, ["csum", "mask"], ["mcum"])
            P.tt("vector", mcum, mcum, base.unsqueeze(2).broadcast_to([128, 16, 32]), ALU.subtract,
                 ["mcum", ("base", 0), ("base", 1)], ["mcum"])
            P.copy("vector", mcumb, mcum, ["mcum"], ["mcumb"])
            P.mm(ps[3][:], ustr[:], maskb.rearrange("p e j -> p (e j)"), True, False, ["K:ustr", "maskb"], [PK[3]])
            P.mm(ps[3][:], ones[:], mcumb.rearrange("p e j -> p (e j)"), False, True, ["K:ones", "mcumb"], [PK[3]])
            pf = ptmp.rearrange("p e j -> p (e j)")
            P.ts("vector", pf, ps[3][:], 1.0, None, ALU.add, None, [PK[3]], ["ptmp"])
            P.tt("vector", pf, pf, mflat, ALU.mult, ["ptmp", "mask"], ["ptmp"])
            P.ts("vector", posm[:].rearrange("p e j -> p (e j)"), pf, -1.0, None, ALU.add, None, ["ptmp"], ["K:posm"])
            jpt = A.alloc("jpt", [128, 32, 2], BF16)
            P.dma("sync", jpt, I["jp"].rearrange("p (j two) -> p j two", two=2), [], ["jpt"])
            P.copy("vector", affhl[:, :, :, 2:4], jpt.unsqueeze(2).broadcast_to([128, 32, 16, 2]), ["jpt", "K:affhl"], ["K:affhl"])
            P.copy("vector", ahi, aff, ["aff"], ["ahi"])
            P.copy("vector", affhl[:, :, :, 0], ahi, ["ahi"], ["K:affhl"])
            P.tt("vector", affhl[:, :, :, 1], aff, ahi, ALU.subtract, ["aff", "ahi", "K:affhl"], ["K:affhl"])
            if dbg:
                P.dma("gpsimd", dbgaff, aff.rearrange("p j e -> p (j e)"), ["aff"], ["D:dbgaff"])
                P.dma("gpsimd", dbgpos, posm[:].rearrange("p e j -> p (e j)"), ["K:posm"], ["D:dbgpos"])

        def phase_E2(l):
            A.reset()
            Sel = A.alloc("Sel", [128, 32, 512], BF16)
            xgt = A.alloc("xgt", [128, 4, D], BF16)
            xg = A.alloc("xg", [128, 8, 512], BF16)
            hT = A.alloc("hT", [128, 16, 512], BF16)
            yy = [A.alloc("yy%d" % i, [128, 4, D], F32) for i in range(2)]
            wsl = [A.alloc("wsl%d" % i, [128, 2048], BF16) for i in range(6)]
            wst = [A.alloc("wst%d" % i, [128, 2048], F32) for i in range(3)]
            wstk = ["wst0", "wst1", "wst2"]
            iot = A.alloc("iot", [128, 512], F32)
            g16 = A.alloc("g16", [128, 16], F32)
            gsl = A.alloc("gsl", [128, 4], F32)
            idxf = A.alloc("idxf", [128, 4], F32)
            idxi = [A.alloc("idxi%d" % i, [128, 4], I32) for i in range(2)]
            sg = [A.alloc("sg%d" % i, [128, 512], F32) for i in range(2)]
            P.dma("sync", iot, I["iota"], [], ["iot"])
            n2k = [("D:n2d", j) for j in range(NT)]
            xk = [("D:xres", j) for j in range(NT)]
            wc = [0]

            def wload(src3, shape3):
                i = wc[0] % 6
                wc[0] += 1
                names = {"a": shape3[0], "b": shape3[1]}
                dstv = wsl[i].rearrange("p (a b) -> p a b", **names)
                si = wcnt[0] % 3
                stv = [wst[k].rearrange("p (a b) -> p a b", **names) for k in range(3)]
                load_w(dstv, "wsl%d" % i, src3, stv, wstk)
                return dstv, "wsl%d" % i

            for e_ in range(16):
                eb = e_ % 2
                for j in range(32):
                    P.ts("vector", Sel[:, j, :], iot, posm[:, e_, j:j + 1], None, ALU.is_equal, None,
                         ["iot", "K:posm"], [("Sel", j // 8)])
                selk = [("Sel", q) for q in range(4)]
                for sc in range(4):
                    for j in range(32):
                        P.mm(ps[7][:, sc * 4:sc * 4 + 4], Sel[:, j, sc * 128:(sc + 1) * 128], affhl[:, j, e_, :], j == 0, j == 31,
                             selk + ["K:affhl"], [PK[7]])
                P.copy("vector", g16, ps[7][:, 0:16], [PK[7]], ["g16"])
                g4 = g16.rearrange("p (s f) -> p s f", f=4)
                P.tt("vector", gsl, g4[:, :, 0], g4[:, :, 1], ALU.add, ["g16"], ["gsl"])
                P.stt(idxf, g4[:, :, 2], 128.0, g4[:, :, 3], ALU.mult, ALU.add, ["g16"], ["idxf"])
                P.copy("vector", idxi[eb], idxf, ["idxf"], ["idxi%d" % eb])
                for sc in range(4):
                    P.add("gpsimd", (lambda sc=sc, eb=eb: lambda e: e.indirect_dma_start(
                        out=xgt[:, sc, :], out_offset=None, in_=n2d,
                        in_offset=bass.IndirectOffsetOnAxis(ap=idxi[eb][:, sc:sc + 1], axis=0)))(),
                        ["idxi%d" % eb] + n2k, [("xgt", sc)], dma=True)
                for dc2 in range(4):
                    bk = 2 + (dc2 % 2)
                    pb = psb(bk)
                    for h in range(2):
                        dc = dc2 * 2 + h
                        for sc in range(4):
                            P.tr(pb[:, h * 512 + sc * 128:h * 512 + (sc + 1) * 128], xgt[:, sc, dc * 128:(dc + 1) * 128], ident[:],
                                 [("xgt", sc), "K:ident"], [PK[bk]])
                    P.copy("scalar" if dc2 % 2 == 0 else "vector", xg[:, dc2 * 2:dc2 * 2 + 2, :],
                           pb.rearrange("p (h s) -> p h s", h=2), [PK[bk]], [("xg", dc2)])
                xgk = [("xg", q) for q in range(4)]
                gsrc = I["w_e_gate"][l, e_].rearrange("(c p) f -> p c f", p=128)
                usrc = I["w_e_up"][l, e_].rearrange("(c p) f -> p c f", p=128)
                for fg in range(8):
                    wg, wgk = wload(gsrc[:, :, fg * 256:(fg + 1) * 256], (8, 256))
                    wu, wuk = wload(usrc[:, :, fg * 256:(fg + 1) * 256], (8, 256))
                    for fc in range(2):
                        for dc in range(8):
                            P.mm(ps[4][:], wg[:, dc, fc * 128:(fc + 1) * 128], xg[:, dc, :], dc == 0, dc == 7, [wgk] + xgk, [PK[4]])
                        for dc in range(8):
                            P.mm(ps[5][:], wu[:, dc, fc * 128:(fc + 1) * 128], xg[:, dc, :], dc == 0, dc == 7, [wuk] + xgk, [PK[5]])
                        si = fc
                        P.act(sg[si], ps[4][:], AF.Silu, [PK[4]], ["sg%d" % si])
                        P.tt("vector", hT[:, fg * 2 + fc, :], sg[si], ps[5][:], ALU.mult, ["sg%d" % si, PK[5]], [("hT", fg * 2 + fc)])
                hk = [("hT", f) for f in range(16)]
                dsrc = I["w_e_down"][l, e_].rearrange("(c p) d -> p c d", p=128)
                for dq in range(8):
                    wd, wdk = wload(dsrc[:, :, dq * 128:(dq + 1) * 128], (16, 128))
                    bk = 6 if dq % 2 == 0 else 1
                    for sc in range(4):
                        for fc in range(16):
                            P.mm(ps[bk][:, sc * 128:(sc + 1) * 128], hT[:, fc, sc * 128:(sc + 1) * 128], wd[:, fc, :], fc == 0, fc == 15,
                                 hk + [wdk], [PK[bk]])
                    for sc in range(4):
                        P.ts("vector", yy[eb][:, sc, dq * 128:(dq + 1) * 128], ps[bk][:, sc * 128:(sc + 1) * 128], gsl[:, sc:sc + 1], None,
                             ALU.mult, None, [PK[bk], "gsl"], [("yy%d" % eb, sc)])
                for sc in range(4):
                    P.add("gpsimd", (lambda sc=sc, eb=eb: lambda e: e.indirect_dma_start(
                        out=xres, out_offset=bass.IndirectOffsetOnAxis(ap=idxi[eb][:, sc:sc + 1], axis=0),
                        in_=yy[eb][:, sc, :], in_offset=None, compute_op=ALU.add))(),
                        ["idxi%d" % eb, ("yy%d" % eb, sc)] + xk, xk, dma=True)

        def phase_F():
            A.reset()
            gt = A.alloc("gt", [128, D], F32)
            xt = [A.alloc("xt%d" % i, [128, D], F32) for i in range(2)]
            junk = A.alloc("junk", [128, D], BF16)
            xo = [A.alloc("xo%d" % i, [128, D], F32) for i in range(2)]
            s3 = [A.alloc("s3%d" % i, [128, 4], F32) for i in range(2)]
            P.dma("sync", gt, I["norm_final"], [], ["gt"])
            for j in range(NT):
                b = j % 2
                P.dma("sync", xt[b], xres[j * 128:(j + 1) * 128, :], [("D:xres", j)], ["xt%d" % b])
                rms_tile(xt[b], "xt%d" % b, junk, s3[b], "s3%d" % b, D, 1e-6)
                P.stt(xo[b], xt[b], s3[b][:, 2:3], gt, ALU.mult, ALU.mult, ["xt%d" % b, "s3%d" % b, "gt"], ["xo%d" % b])
                P.dma("gpsimd", out[j * 128:(j + 1) * 128, :], xo[b], ["xo%d" % b], [("D:out", j)])

        for l in range(depth):
            if "A" in stages:
                phase_A(l)
            if "B0" in stages:
                phase_B0(l)
            if "B1" in stages:
                phase_B1(l)
            if "B2" in stages:
                phase_B2(l)
            if "C" in stages:
                phase_C(l)
            if "D" in stages:
                phase_D(l)
            if "E" in stages:
                phase_E1(l)
                phase_E2(l)
        if "F" in stages:
            phase_F()
        fin = [k for k in P.state if (k if isinstance(k, str) else k[0]).startswith("D:")]
        P.add("sync", lambda e: e.nop(), fin, [])
        P.add("gpsimd", lambda e: e.nop(), fin, [])
        P.emit(st)
        print("ops", len(P.ops), "maxsem", P.maxsem, flush=True)
    return nc


_CONST = {}


def _consts():
    if _CONST:
        return _CONST
    bf = ml_dtypes.bfloat16
    half = 32
    inv = (10000.0 ** (-np.arange(half, dtype=np.float32) * 2.0 / 64)).astype(np.float32)
    pos = np.arange(S, dtype=np.float32)
    ang = pos[:, None] * inv[None, :]
    cos = np.cos(ang).astype(np.float32).T
    sin = np.sin(ang).astype(np.float32).T
    cosT = np.zeros((128, S), np.float32)
    sinT = np.zeros((128, S), np.float32)
    for p in range(128):
        i = p % 32
        cosT[p] = cos[i]
        sinT[p] = -sin[i] if (p % 64) < 32 else sin[i]
    _CONST["cosT"] = cosT
    _CONST["sinT"] = sinT
    L = S
    t = np.linspace(0.0, 1.0, L, dtype=np.float32)
    bands = 16
    w = (2.0 * np.float32(math.pi) * np.arange(L, dtype=np.float32) / L).astype(np.float32)
    fr = np.linspace(1e-4, bands - 1, bands, dtype=np.float32)
    a = w[:, None] * fr[None, :]
    z = np.concatenate([t[:, None], np.cos(a), -np.sin(a)], axis=-1).astype(np.float32)
    _CONST["zT"] = np.ascontiguousarray(z.T)
    mind = math.log(1e-2) / 0.3
    maxd = math.log(1e-2) / 1.5
    deltas = np.abs(np.linspace(mind, maxd, 512, dtype=np.float32))
    decay = np.exp(-t[:, None] * deltas[None, :]).astype(np.float32)
    _CONST["decay"] = decay
    db = decay.copy()
    db[0] = 0.0
    _CONST["decayb"] = db
    n = np.arange(2048, dtype=np.int64)
    prod = (n[:, None] * n[None, :]) % 4096
    angd = prod.astype(np.float64) * (2.0 * math.pi / 4096)
    for nm, fn in (("ctab", np.cos), ("stab", lambda v: -np.sin(v))):
        m = fn(angd).astype(np.float32)
        m = m.reshape(16, 128, 16, 128)
        m = np.ascontiguousarray(m.transpose(2, 1, 0, 3)).reshape(16, 128, 16 * 128)
        _CONST[nm] = m.astype(bf)
    kk = np.arange(17 * 128, dtype=np.float64)
    ph = kk * (2.0 * math.pi / NFFT)
    tw = np.stack([np.cos(ph), np.sin(ph), -np.sin(ph)], axis=-1).astype(np.float32)
    _CONST["twid"] = np.ascontiguousarray(tw.reshape(17, 128, 3).transpose(1, 0, 2)).reshape(128, 51)
    altv = np.where(np.arange(128) % 2 == 0, 1.0, -1.0).astype(np.float32)
    _CONST["alt"] = np.stack([altv, altv], axis=1).astype(bf)
    _CONST["altrow"] = altv[None, :].astype(bf)
    _CONST["iota"] = np.broadcast_to(np.arange(512, dtype=np.float32)[None, :], (128, 512)).copy()
    jp = np.zeros((128, 32, 2), np.float32)
    jp[:, :, 0] = np.arange(32, dtype=np.float32)[None, :]
    jp[:, :, 1] = np.arange(128, dtype=np.float32)[:, None]
    _CONST["jp"] = jp.reshape(128, 64).astype(bf)
    _CONST["ident_bf"] = np.eye(128, dtype=np.float32).astype(bf)
    _CONST["ident_f"] = np.eye(128, dtype=np.float32)
    _CONST["ustrict"] = np.triu(np.ones((128, 128), np.float32), 1).astype(bf)
    _CONST["ones_bf"] = np.ones((128, 128), np.float32).astype(bf)
    return _CONST


def _rep(v, n=128):
    return np.ascontiguousarray(np.broadcast_to(v[..., None, :], v.shape[:-1] + (n, v.shape[-1])))


def prep_shared(inp):
    c = dict(_consts())
    w_in = np.asarray(inp["w_in"])
    b_in = np.asarray(inp["b_in"])
    sw = np.concatenate([np.arange(h * 64 + 32, h * 64 + 64).tolist() + np.arange(h * 64, h * 64 + 32).tolist()
                         for h in range(8)]).astype(np.int64)
    u0, q0, k0, v0, gh0, ga0 = 0, 1536, 2048, 2560, 3072, 4096
    cols = np.concatenate([np.arange(u0, u0 + 1536), np.arange(q0, q0 + 512), np.arange(k0, k0 + 512),
                           q0 + sw, k0 + sw, np.arange(gh0, gh0 + 1024), np.arange(ga0, ga0 + 1024),
                           np.arange(v0, v0 + 512)])
    c["w_in"] = np.ascontiguousarray(w_in[:, :, cols])
    bext = b_in[:, cols]
    c["b_in"] = np.ascontiguousarray(bext.reshape(2, 48, 128).transpose(0, 2, 1))
    c["b_v"] = _rep(b_in[:, v0:v0 + 512])
    c["norm_mix"] = _rep(np.asarray(inp["norm_mix"]))
    c["norm_ffn"] = _rep(np.asarray(inp["norm_ffn"]))
    c["norm_final"] = _rep(np.asarray(inp["norm_final"]))
    cw = np.asarray(inp["hy_conv_w"])
    c["hy_cw"] = np.ascontiguousarray(cw.reshape(2, 3, 12, 128).transpose(0, 3, 2, 1)).reshape(2, 128, 36)
    c["hy_cb"] = np.ascontiguousarray(np.asarray(inp["hy_conv_b"]).reshape(2, 12, 128).transpose(0, 2, 1))
    c["hy_w1"] = np.asarray(inp["hy_ffn_w1"])
    c["hy_w2"] = np.asarray(inp["hy_ffn_w2"])
    c["hy_w3"] = np.asarray(inp["hy_ffn_w3"])
    c["hy_cols"] = np.ascontiguousarray(np.stack([np.asarray(inp["hy_ffn_b1"]), np.asarray(inp["hy_ffn_f1"]),
                                                  np.asarray(inp["hy_ffn_b2"]), np.asarray(inp["hy_ffn_f2"])], axis=-1))
    c["hy_bias"] = _rep(np.asarray(inp["hy_bias"]))
    lam = np.concatenate([np.asarray(inp["lambda_q1"]), np.asarray(inp["lambda_k1"]),
                          np.asarray(inp["lambda_q2"]), np.asarray(inp["lambda_k2"])], axis=-1)
    c["lamv"] = _rep(lam)
    c["subln"] = _rep(np.asarray(inp["subln_g"]))
    c["w_up_hy"] = np.asarray(inp["w_up_hyena"])
    c["w_up_da"] = np.asarray(inp["w_up_attn"])
    c["w_out"] = np.asarray(inp["w_out"])
    c["w_router"] = np.asarray(inp["w_router"])
    c["b_router"] = _rep(np.asarray(inp["b_router"]))
    c["w_e_gate"] = np.asarray(inp["w_e_gate"])
    c["w_e_up"] = np.asarray(inp["w_e_up"])
    c["w_e_down"] = np.asarray(inp["w_e_down"])
    return {k: np.ascontiguousarray(v) for k, v in c.items()}


def kernel(**inputs):
    x = np.asarray(inputs["x"], dtype=np.float32)
    shared = prep_shared(inputs)
    nc = build(bass.Bass("TRN2", target_bir_lowering=False))
    in_maps = []
    for core in range(8):
        m = dict(shared)
        m["x"] = np.ascontiguousarray(x[core % 4])
        in_maps.append(m)
    res = run_bass_kernel_spmd(nc, in_maps, core_ids=list(range(8)))
    return np.stack([np.asarray(res.results[b]["out"], dtype=np.float32) for b in range(4)], axis=0)
```

```python
import numpy as np
import concourse.bass as bass
import concourse.mybir as mybir

F32 = mybir.dt.float32
BF16 = mybir.dt.bfloat16
I32 = mybir.dt.int32
AF = mybir.ActivationFunctionType
ALU = mybir.AluOpType
AX = mybir.AxisListType

COMPUTE = ("tensor", "vector", "scalar", "gpsimd")
NDSEM = 8


class Op:
    __slots__ = ("id", "eng", "fn", "deps", "signal", "semval", "is_dma", "dsem", "dval", "dprev")


class Prog:
    def __init__(self, nc):
        self.nc = nc
        self.ops = []
        self.state = {}

    def _prune(self, ids):
        best = {}
        out = set()
        for i in ids:
            o = self.ops[i]
            if o.is_dma:
                out.add(i)
            else:
                if o.eng not in best or best[o.eng] < i:
                    best[o.eng] = i
        out.update(best.values())
        return out

    def add(self, eng, fn, r=(), w=(), dma=False):
        o = Op()
        o.id = len(self.ops)
        o.eng = eng
        o.fn = fn
        o.is_dma = dma
        o.signal = dma
        deps = set()
        for k in tuple(r) + tuple(w):
            if k not in self.state:
                nm = k if isinstance(k, str) else k[0]
                if not (nm.startswith("D:") or nm.startswith("K:")):
                    self.state[k] = [None, list(getattr(self, "_pend", []))]
            st = self.state.get(k)
            if st is not None and st[0] is not None:
                deps.add(st[0])
        for k in w:
            st = self.state.get(k)
            if st is not None:
                deps.update(st[1])
        o.deps = self._prune(deps)
        self.ops.append(o)
        for k in r:
            st = self.state.setdefault(k, [None, []])
            st[1].append(o.id)
            if len(st[1]) > 24:
                st[1] = list(self._prune(st[1]))
        for k in w:
            self.state[k] = [o.id, []]
        return o

    def phase_barrier(self, newkeys, oldprefix=None):
        pend = set()
        for k, st in list(self.state.items()):
            nm = k if isinstance(k, str) else k[0]
            if nm.startswith("D:") or nm.startswith("K:"):
                continue
            if st[0] is not None:
                pend.add(st[0])
            pend.update(st[1])
            del self.state[k]
        pend = list(self._prune(pend))
        self._pend = pend

    def fresh(self, key):
        self.state[key] = [None, list(getattr(self, "_pend", []))]

    def emit(self, stack):
        nc = self.nc
        engs = ["tensor", "vector", "scalar", "gpsimd", "sync"]
        sems = {e: stack.enter_context(nc.semaphore("s_" + e)) for e in COMPUTE}
        dsems = {e: [stack.enter_context(nc.semaphore("d_%s%d" % (e, i))) for i in range(NDSEM)]
                 for e in engs}
        for o in self.ops:
            for d in o.deps:
                p = self.ops[d]
                if p.is_dma:
                    continue
                if p.eng == "tensor" and o.eng == "tensor" and not o.is_dma:
                    continue
                p.signal = True
        cnt = {e: 0 for e in COMPUTE}
        dcnt = {e: 0 for e in engs}
        dval = {e: [0] * NDSEM for e in engs}
        for o in self.ops:
            if o.is_dma:
                i = dcnt[o.eng] % NDSEM
                dcnt[o.eng] += 1
                o.dsem = dsems[o.eng][i]
                o.dprev = dval[o.eng][i]
                dval[o.eng][i] += 16
                o.dval = dval[o.eng][i]
            elif o.signal:
                cnt[o.eng] += 1
                o.semval = cnt[o.eng]
        self.maxsem = dict(cnt)
        block = stack.enter_context(nc.Block())
        byeng = {e: [o for o in self.ops if o.eng == e] for e in engs}

        def run(engname, eobj):
            waited = {}

            def wait(sem, val):
                key = id(sem)
                if waited.get(key, 0) >= val:
                    return
                waited[key] = val
                eobj.wait_ge(sem, val)

            for o in byeng[engname]:
                for d in sorted(o.deps):
                    p = self.ops[d]
                    if p.is_dma:
                        wait(p.dsem, p.dval)
                    else:
                        if p.eng == "tensor" and engname == "tensor" and not o.is_dma:
                            continue
                        wait(sems[p.eng], p.semval)
                if o.is_dma:
                    if o.dprev > 0:
                        wait(o.dsem, o.dprev)
                    inst = o.fn(eobj)
                    inst.then_inc(o.dsem, 16)
                else:
                    inst = o.fn(eobj)
                    if o.signal:
                        inst.then_inc(sems[engname], 1)

        @block.tensor
        def _(e):
            run("tensor", e)

        @block.vector
        def _(e):
            run("vector", e)

        @block.scalar
        def _(e):
            run("scalar", e)

        @block.gpsimd
        def _(e):
            run("gpsimd", e)

        @block.sync
        def _(e):
            run("sync", e)

    def mm(self, out, lhsT, rhs, start, stop, r, w):
        return self.add("tensor", lambda e: e.matmul(out, lhsT, rhs, start=start, stop=stop), r, w)

    def tr(self, out, in_, ident, r, w):
        return self.add("tensor", lambda e: e.transpose(out, in_, ident), r, w)

    def act(self, out, in_, func, r, w, bias=None, scale=None, accum=None):
        kw = {}
        if bias is not None:
            kw["bias"] = bias
        if scale is not None:
            kw["scale"] = scale
        if accum is not None:
            kw["accum_out"] = accum
        return self.add("scalar", lambda e: e.activation(out, in_, func, **kw), r, w)

    def ts(self, eng, out, in0, s1, s2, op0, op1, r, w):
        if op1 is None:
            return self.add(eng, lambda e: e.tensor_scalar(out, in0, s1, None, op0), r, w)
        return self.add(eng, lambda e: e.tensor_scalar(out, in0, s1, s2, op0, op1), r, w)

    def tt(self, eng, out, in0, in1, op, r, w):
        return self.add(eng, lambda e: e.tensor_tensor(out, in0, in1, op), r, w)

    def stt(self, out, in0, scalar, in1, op0, op1, r, w):
        return self.add("vector", lambda e: e.scalar_tensor_tensor(out, in0, scalar, in1, op0, op1), r, w)

    def copy(self, eng, out, in_, r, w):
        if eng == "scalar":
            return self.add(eng, lambda e: e.copy(out, in_), r, w)
        return self.add(eng, lambda e: e.tensor_copy(out, in_), r, w)

    def dma(self, eng, out, in_, r, w, **kw):
        return self.add(eng, lambda e: e.dma_start(out, in_, **kw), r, w, dma=True)

    def memset(self, eng, ap, val, w):
        return self.add(eng, lambda e: e.memset(ap, val), (), w)


class Arena:
    def __init__(self, P, tensor, words):
        self.P = P
        self.t = tensor
        self.words = words
        self.off = 0
        self.names = []

    def reset(self):
        self.P.phase_barrier(None)
        self.off = 0

    def alloc(self, name, shape, dtype):
        n = int(np.prod(shape[1:]))
        if dtype == BF16:
            assert n % 2 == 0
            w = n // 2
        else:
            w = n
        assert self.off + w <= self.words, (name, self.off, w, self.words)
        ap = self.t[0:shape[0], self.off:self.off + w]
        if dtype != F32:
            ap = ap.bitcast(dtype)
        self.off += w
        if len(shape) > 2:
            names = " ".join("a%d" % i for i in range(len(shape) - 1))
            kw = {"a%d" % i: shape[i + 1] for i in range(len(shape) - 1)}
            ap = ap.rearrange("p (%s) -> p %s" % (names, names), **kw)
        self.P.fresh(name)
        return ap

from contextlib import ExitStack
import math
import ml_dtypes
from concourse.bass_utils import run_bass_kernel_spmd

S = 4096
D = 1024
NT = 32
ARENA = 49152
TWO_PI = 2.0 * math.pi
MAGIC = 12582912.0
PI_LO = 3.1415925
NFFT = 8192


def build(nc, dbg=False, stages=("A", "B0", "B1", "B2", "C", "D", "E", "F"), depth=2):
    def din(name, shape, dt=F32):
        return nc.dram_tensor(name, list(shape), dt, kind="ExternalInput").ap()

    skind = "ExternalOutput" if dbg else "Internal"

    def dscr(name, shape, dt):
        return nc.dram_tensor(name, list(shape), dt, kind=skind).ap()

    I = {}
    I["x"] = din("x", [S, D])
    I["w_in"] = din("w_in", [2, D, 6144])
    I["b_in"] = din("b_in", [2, 128, 48])
    I["b_v"] = din("b_v", [2, 128, 512])
    I["norm_mix"] = din("norm_mix", [2, 128, D])
    I["norm_ffn"] = din("norm_ffn", [2, 128, D])
    I["norm_final"] = din("norm_final", [128, D])
    I["cosT"] = din("cosT", [128, S])
    I["sinT"] = din("sinT", [128, S])
    I["hy_cw"] = din("hy_cw", [2, 128, 36])
    I["hy_cb"] = din("hy_cb", [2, 128, 12])
    I["zT"] = din("zT", [33, S])
    I["hy_w1"] = din("hy_w1", [2, 33, 64])
    I["hy_w2"] = din("hy_w2", [2, 64, 64])
    I["hy_w3"] = din("hy_w3", [2, 64, 2048])
    I["hy_cols"] = din("hy_cols", [2, 64, 4])
    I["decay"] = din("decay", [S, 512])
    I["decayb"] = din("decayb", [S, 512])
    I["hy_bias"] = din("hy_bias", [2, 2, 128, 512])
    I["ctab"] = din("ctab", [16, 128, 16 * 128], BF16)
    I["stab"] = din("stab", [16, 128, 16 * 128], BF16)
    I["twid"] = din("twid", [128, 17 * 3])
    I["alt"] = din("alt", [128, 2], BF16)
    I["altrow"] = din("altrow", [1, 128], BF16)
    I["lamv"] = din("lamv", [2, 128, 256])
    I["subln"] = din("subln", [2, 128, 128])
    I["w_up_hy"] = din("w_up_hy", [2, 512, D])
    I["w_up_da"] = din("w_up_da", [2, 512, D])
    I["w_out"] = din("w_out", [2, D, D])
    I["w_router"] = din("w_router", [2, D, 16])
    I["b_router"] = din("b_router", [2, 128, 16])
    I["w_e_gate"] = din("w_e_gate", [2, 16, D, 2048])
    I["w_e_up"] = din("w_e_up", [2, 16, D, 2048])
    I["w_e_down"] = din("w_e_down", [2, 16, 2048, D])
    I["iota"] = din("iota", [128, 512])
    I["jp"] = din("jp", [128, 64], BF16)
    I["ident_bf"] = din("ident_bf", [128, 128], BF16)
    I["ident_f"] = din("ident_f", [128, 128])
    I["ustrict"] = din("ustrict", [128, 128], BF16)
    I["ones_bf"] = din("ones_bf", [128, 128], BF16)
    out = nc.dram_tensor("out", [S, D], F32, kind="ExternalOutput").ap()

    xres = dscr("xres", [S, D], F32)
    uT = dscr("uT", [1536, S], BF16)
    qkT = dscr("qkT", [1024, S], BF16)
    gT = dscr("gT", [2048, S], BF16)
    Vtm = dscr("Vtm", [S, 512], BF16)
    Hsp = dscr("Hsp", [2, 4, 17 * 128, 512], F32)
    hsd = dscr("hsd", [2, S, 512], BF16)
    yhtm = dscr("yhtm", [S, 512], BF16)
    ctm = dscr("ctm", [3, S, 512], BF16)
    yhT = dscr("yhT", [512, S], BF16)
    ydT = dscr("ydT", [512, S], BF16)
    n2d = dscr("n2d", [S, D], BF16)
    dbgaff = dscr("dbgaff", [128, 32 * 16], F32)
    dbgpos = dscr("dbgpos", [128, 16 * 32], F32)

    st = ExitStack()
    with st:
        arena_t = st.enter_context(nc.sbuf_tensor("arena", [128, ARENA], F32))
        ident = st.enter_context(nc.sbuf_tensor("ident", [128, 128], BF16))
        identf = st.enter_context(nc.sbuf_tensor("identf", [128, 128], F32))
        ustr = st.enter_context(nc.sbuf_tensor("ustr", [128, 128], BF16))
        ones = st.enter_context(nc.sbuf_tensor("ones", [128, 128], BF16))
        alt = st.enter_context(nc.sbuf_tensor("altc", [128, 2], BF16))
        altrow = st.enter_context(nc.sbuf_tensor("altr", [1, 128], BF16))
        posm = st.enter_context(nc.sbuf_tensor("posm", [128, 16, 32], F32))
        affhl = st.enter_context(nc.sbuf_tensor("affhl", [128, 32, 16, 4], BF16))
        lamt = st.enter_context(nc.sbuf_tensor("lamt", [128, 8], F32))
        ps = [st.enter_context(nc.psum_tensor("ps%d" % i, [128, 512], F32)) for i in range(8)]
        PK = ["K:ps%d" % i for i in range(8)]
        P = Prog(nc)
        A = Arena(P, arena_t, ARENA)

        def psb(i):
            return ps[i][:].bitcast(BF16)

        P.dma("sync", ident[:], I["ident_bf"], [], ["K:ident"])
        P.dma("sync", identf[:], I["ident_f"], [], ["K:identf"])
        P.dma("sync", ustr[:], I["ustrict"], [], ["K:ustr"])
        P.dma("sync", ones[:], I["ones_bf"], [], ["K:ones"])
        P.dma("sync", alt[:], I["alt"], [], ["K:alt"])
        P.dma("sync", altrow[:], I["altrow"], [], ["K:altrow"])
        for q in range(4):
            P.dma("sync", xres[q * 1024:(q + 1) * 1024, :], I["x"][q * 1024:(q + 1) * 1024, :], [],
                  [("D:xres", j) for j in range(q * 8, q * 8 + 8)])

        def rms_tile(xt_ap, kx, junk, stt3, ks, n, eps):
            P.act(junk, xt_ap, AF.Square, [kx], [ks + "j", ks], accum=stt3[:, 0:1])
            P.ts("vector", stt3[:, 1:2], stt3[:, 0:1], 1.0 / n, eps, ALU.mult, ALU.add, [ks], [ks])
            P.act(stt3[:, 1:2], stt3[:, 1:2], AF.Sqrt, [ks], [ks])
            P.add("vector", lambda e: e.reciprocal(stt3[:, 2:3], stt3[:, 1:2]), [ks], [ks])

        wcnt = [0]

        def load_w(dst, dkey, src, stg, stgk):
            i = wcnt[0] % len(stg)
            wcnt[0] += 1
            P.dma("sync", stg[i], src, [], [stgk[i]])
            ceng = ("scalar", "vector", "scalar")[wcnt[0] % 3]
            P.copy(ceng, dst, stg[i], [stgk[i]], [dkey])

        def phase_A(l):
            A.reset()
            nT = A.alloc("nT", [128, 8, S], BF16)
            cosT = A.alloc("cosT", [128, S], F32)
            sinT = A.alloc("sinT", [128, S], F32)
            gt = A.alloc("gt", [128, D], F32)
            bcol = A.alloc("bcol", [128, 48], F32)
            bv = A.alloc("bv", [128, 512], F32)
            xt = [A.alloc("xt%d" % i, [128, D], F32) for i in range(2)]
            junk = A.alloc("junk", [128, D], BF16)
            xn = [A.alloc("xn%d" % i, [128, D], BF16) for i in range(2)]
            s3 = [A.alloc("s3%d" % i, [128, 4], F32) for i in range(2)]
            wst = [A.alloc("wst%d" % i, [128, 8, 256], F32) for i in range(2)]
            wstk = ["wst0", "wst1"]
            wt = [A.alloc("wt%d" % i, [128, 8, 512], BF16) for i in range(2)]
            stg = [A.alloc("stg%d" % i, [128, 512], BF16) for i in range(3)]
            tq = [A.alloc("tq%d" % i, [128, 512], F32) for i in range(2)]
            P.dma("sync", gt, I["norm_mix"][l], [], ["gt"])
            P.dma("sync", bcol, I["b_in"][l], [], ["bcol"])
            P.dma("sync", bv, I["b_v"][l], [], ["bv"])
            P.dma("sync", cosT, I["cosT"], [], ["cosT"])
            P.dma("sync", sinT, I["sinT"], [], ["sinT"])
            for j in range(NT):
                b = j % 2
                P.dma("sync", xt[b], xres[j * 128:(j + 1) * 128, :], [("D:xres", j)], ["xt%d" % b])
                rms_tile(xt[b], "xt%d" % b, junk, s3[b], "s3%d" % b, D, 1e-6)
                P.stt(xn[b], xt[b], s3[b][:, 2:3], gt, ALU.mult, ALU.mult, ["xt%d" % b, "s3%d" % b, "gt"], ["xn%d" % b])
                pb = psb(6 + b)
                for c in range(8):
                    P.tr(pb[:, c * 128:(c + 1) * 128], xn[b][:, c * 128:(c + 1) * 128], ident[:],
                         ["xn%d" % b, "K:ident"], [PK[6 + b]])
                P.copy("scalar", nT[:, :, j * 128:(j + 1) * 128], pb.rearrange("p (c t) -> p c t", c=8),
                       [PK[6 + b]], [("nT", j // 4)])
            wsrc = I["w_in"][l].rearrange("(c p) f -> p c f", p=128)
            gcnt = [0]

            def load_group(g):
                b = gcnt[0] % 2
                gcnt[0] += 1
                for h in range(2):
                    load_w(wt[b][:, :, h * 256:(h + 1) * 256], ("wt%d" % b, h),
                           wsrc[:, :, g * 512 + h * 256: g * 512 + (h + 1) * 256], wst, wstk)
                return b

            def wkeys(b):
                return [("wt%d" % b, 0), ("wt%d" % b, 1)]

            bankc = [0]
            stc = [0]

            def proj_fm(b, fc, tg):
                bk = bankc[0] % 4
                bankc[0] += 1
                for kc in range(8):
                    P.mm(ps[bk][:], wt[b][:, kc, fc * 128:(fc + 1) * 128], nT[:, kc, tg * 512:(tg + 1) * 512],
                         kc == 0, kc == 7, wkeys(b) + [("nT", tg)], [PK[bk]])
                return bk

            for g in [0, 1, 2, 7, 8, 9, 10]:
                b = load_group(g)
                for fc in range(4):
                    ch = g * 4 + fc
                    for tg in range(8):
                        bk = proj_fm(b, fc, tg)
                        si = stc[0] % 3
                        stc[0] += 1
                        func = AF.Identity if g < 3 else AF.Sigmoid
                        P.act(stg[si], ps[bk][:], func, [PK[bk], "bcol"], ["stg%d" % si], bias=bcol[:, ch:ch + 1])
                        if g < 3:
                            dst = uT[ch * 128:(ch + 1) * 128, tg * 512:(tg + 1) * 512]
                            dk = ("D:uT", ch, tg)
                        else:
                            gc = ch - 28
                            dst = gT[gc * 128:(gc + 1) * 128, tg * 512:(tg + 1) * 512]
                            dk = ("D:gT", gc, tg)
                        P.dma("gpsimd", dst, stg[si], ["stg%d" % si], [dk])
            for (ga, gb2, rbase) in [(3, 5, 0), (4, 6, 4)]:
                ba = load_group(ga)
                bb = load_group(gb2)
                for fc in range(4):
                    cha = ga * 4 + fc
                    chb = gb2 * 4 + fc
                    for tg in range(8):
                        bk1 = proj_fm(ba, fc, tg)
                        bk2 = proj_fm(bb, fc, tg)
                        sl = slice(tg * 512, (tg + 1) * 512)
                        P.stt(tq[0], ps[bk1][:], bcol[:, cha:cha + 1], cosT[:, sl], ALU.add, ALU.mult,
                              [PK[bk1], "bcol", "cosT"], ["tq0"])
                        P.stt(tq[1], ps[bk2][:], bcol[:, chb:chb + 1], sinT[:, sl], ALU.add, ALU.mult,
                              [PK[bk2], "bcol", "sinT"], ["tq1"])
                        si = stc[0] % 3
                        stc[0] += 1
                        P.tt("vector", stg[si], tq[0], tq[1], ALU.add, ["tq0", "tq1"], ["stg%d" % si])
                        rc = rbase + fc
                        P.dma("gpsimd", qkT[rc * 128:(rc + 1) * 128, sl], stg[si], ["stg%d" % si], [("D:qkT", rc, tg)])
            b = load_group(11)
            for j in range(NT):
                bk = bankc[0] % 4
                bankc[0] += 1
                for kc in range(8):
                    P.mm(ps[bk][:], nT[:, kc, j * 128:(j + 1) * 128], wt[b][:, kc, :], kc == 0, kc == 7,
                         wkeys(b) + [("nT", j // 4)], [PK[bk]])
                si = stc[0] % 3
                stc[0] += 1
                P.tt("vector", stg[si], ps[bk][:], bv, ALU.add, [PK[bk], "bv"], ["stg%d" % si])
                P.dma("gpsimd", Vtm[j * 128:(j + 1) * 128, :], stg[si], ["stg%d" % si], [("D:Vtm", j)])

        def sin_layer(dst, lhsT, rhs_fn, bcolap, fcolap, kr, tmp, M):
            for tg in range(8):
                bk = tg % 2
                P.mm(ps[bk][0:M, :], lhsT, rhs_fn(tg), True, True, kr, [PK[bk]])
                a, b_, c = tmp
                P.ts("vector", a, ps[bk][0:M, :], bcolap, fcolap, ALU.add, ALU.mult, [PK[bk], "fcols"], ["sa"])
                P.ts("vector", b_, a, 1.0 / TWO_PI, MAGIC, ALU.mult, ALU.add, ["sa"], ["sb"])
                P.ts("vector", b_, b_, MAGIC, None, ALU.subtract, None, ["sb"], ["sb"])
                P.stt(c, b_, -TWO_PI, a, ALU.mult, ALU.add, ["sa", "sb"], ["sc"])
                P.ts("vector", c, c, PI_LO, -PI_LO, ALU.min, ALU.max, ["sc"], ["sc"])
                P.act(dst[:, tg * 512:(tg + 1) * 512], c, AF.Sin, ["sc"], [dst_key[0]])

        dst_key = [None]

        def table_loads(cb, sb, b, idx):
            P.dma("sync", cb[b], I["ctab"][idx].rearrange("p (u k) -> p u k", k=128), [], ["cb%d" % b])
            P.dma("sync", sb[b], I["stab"][idx].rearrange("p (u k) -> p u k", k=128), [], ["sb%d" % b])

        def eo_view(dram2d, v):
            return dram2d.rearrange("(uc p two) c -> two p uc c", p=128, two=2)[v]

        def smul(out, in_, col, r, w):
            P.ts("vector", out, in_, col, None, ALU.mult, None, r, w)

        def fwd_chunk(kc, ze, zo, zk, cb, sb, T, tw, need=("X1r", "X1i", "X2r", "X2i")):
            b = kc % 2
            M = 128 if kc < 16 else 1
            B0_ = 4 * b
            want_r = ("X1r" in need) or ("X2r" in need)
            want_i = ("X1i" in need) or ("X2i" in need)
            if kc < 16:
                table_loads(cb, sb, b, kc)
                for (bank, tab, tk, zz) in ((B0_, cb, "cb", ze), (B0_ + 1, sb, "sb", ze), (B0_ + 2, cb, "cb", zo), (B0_ + 3, sb, "sb", zo)):
                    if (bank == B0_ and not want_r) or (bank == B0_ + 1 and not want_i):
                        continue
                    for uc in range(16):
                        P.mm(ps[bank][:], tab[b][:, uc, :], zz[:, uc, :], uc == 0, uc == 15, ["%s%d" % (tk, b)] + zk, [PK[bank]])
            else:
                for (bank, zz) in ((B0_, ze), (B0_ + 2, zo)):
                    if (bank == B0_ and not want_r) or (bank == B0_ + 2 and not want_i):
                        continue
                    for uc in range(16):
                        P.mm(ps[bank][0:1, :], alt[:, 0:1], zz[:, uc, :], uc == 0, uc == 15, ["K:alt"] + zk, [PK[bank]])
            Er, Ei, Or, Oi = (ps[B0_ + q][0:M, :] for q in range(4))
            kE, kEi, kO, kOi = (PK[B0_ + q] for q in range(4))
            twr = tw[0:M, kc * 3:kc * 3 + 1]
            tws = tw[0:M, kc * 3 + 1:kc * 3 + 2]
            twn = tw[0:M, kc * 3 + 2:kc * 3 + 3]
            wor, woi, t1, t2 = T["wor"][0:M, :], T["woi"][0:M, :], T["t1"][0:M, :], T["t2"][0:M, :]
            if kc < 16:
                if want_r:
                    smul(t1, Oi, tws, [kOi, "tw"], ["t1"])
                    P.stt(wor, Or, twr, t1, ALU.mult, ALU.add, [kO, "tw", "t1"], ["wor"])
                    P.tt("vector", T["x1r"][0:M, :], Er, wor, ALU.add, [kE, "wor"], ["x1r"])
                    P.tt("vector", T["x2r"][0:M, :], Er, wor, ALU.subtract, [kE, "wor"], ["x2r"])
                if want_i:
                    smul(t2, Or, twn, [kO, "tw"], ["t2"])
                    P.stt(woi, Oi, twr, t2, ALU.mult, ALU.add, [kOi, "tw", "t2"], ["woi"])
                    P.tt("vector", T["x1i"][0:M, :], Ei, woi, ALU.add, [kEi, "woi"], ["x1i"])
                    P.tt("vector", T["x2i"][0:M, :], woi, Ei, ALU.subtract, ["woi", kEi], ["x2i"])
            else:
                if want_r:
                    P.copy("vector", T["x1r"][0:1, :], Er, [kE], ["x1r"])
                    P.copy("vector", T["x2r"][0:1, :], Er, [kE], ["x2r"])
                if want_i:
                    P.ts("vector", T["x1i"][0:1, :], Or, -1.0, None, ALU.mult, None, [kO], ["x1i"])
                    P.ts("vector", T["x2i"][0:1, :], Or, -1.0, None, ALU.mult, None, [kO], ["x2i"])
            return M

        def phase_B0(l):
            A.reset()
            zT = A.alloc("zT", [33, S], F32)
            w1 = A.alloc("w1", [33, 64], F32)
            w2 = A.alloc("w2", [64, 64], F32)
            w3 = A.alloc("w3", [64, 2048], F32)
            fcols = A.alloc("fcols", [64, 4], F32)
            h1 = A.alloc("h1T", [64, S], F32)
            h2 = A.alloc("h2T", [64, S], F32)
            tmp = [A.alloc(n, [64, 512], F32) for n in ("sa", "sb", "sc")]
            dec = [A.alloc("dec%d" % i, [128, 512], F32) for i in range(2)]
            decb = [A.alloc("decb%d" % i, [128, 512], F32) for i in range(2)]
            hf = A.alloc("hf", [128, 512], F32)
            hb = A.alloc("hb", [128, 512], F32)
            hso = [A.alloc("hso%d" % i, [128, 512], BF16) for i in range(2)]
            hdo = [A.alloc("hdo%d" % i, [128, 512], BF16) for i in range(2)]
            ze = A.alloc("ze", [128, 16, 512], BF16)
            zo = A.alloc("zo", [128, 16, 512], BF16)
            cb = [A.alloc("cb%d" % i, [128, 16, 128], BF16) for i in range(2)]
            sb = [A.alloc("sb%d" % i, [128, 16, 128], BF16) for i in range(2)]
            tw = A.alloc("tw", [128, 51], F32)
            T = {n: A.alloc(n, [128, 512], F32) for n in ("wor", "woi", "t1", "t2", "x1r", "x1i", "x2r", "x2i")}
            P.dma("sync", zT, I["zT"], [], ["zT"])
            P.dma("sync", w1, I["hy_w1"][l], [], ["w1"])
            P.dma("sync", w2, I["hy_w2"][l], [], ["w2"])
            P.dma("sync", w3, I["hy_w3"][l], [], ["w3"])
            P.dma("sync", fcols, I["hy_cols"][l], [], ["fcols"])
            P.dma("sync", tw, I["twid"], [], ["tw"])
            dst_key[0] = "h1T"
            sin_layer(h1, w1, lambda tg: zT[:, tg * 512:(tg + 1) * 512], fcols[:, 0:1], fcols[:, 1:2],
                      ["w1", "zT"], tmp, 64)
            dst_key[0] = "h2T"
            sin_layer(h2, w2, lambda tg: h1[:, tg * 512:(tg + 1) * 512], fcols[:, 2:3], fcols[:, 3:4],
                      ["w2", "h1T"], tmp, 64)
            for o in range(2):
                for jt in range(NT):
                    b = jt % 2
                    P.dma("sync", dec[b], I["decay"][jt * 128:(jt + 1) * 128, :], [], ["dec%d" % b])
                    P.dma("sync", decb[b], I["decayb"][jt * 128:(jt + 1) * 128, :], [], ["decb%d" % b])
                    P.mm(ps[4][:], h2[:, jt * 128:(jt + 1) * 128], w3[:, o * 1024:o * 1024 + 512], True, True,
                         ["h2T", "w3"], [PK[4]])
                    P.mm(ps[5][:], h2[:, jt * 128:(jt + 1) * 128], w3[:, o * 1024 + 512:o * 1024 + 1024], True, True,
                         ["h2T", "w3"], [PK[5]])
                    P.tt("vector", hf, ps[4][:], dec[b], ALU.mult, [PK[4], "dec%d" % b], ["hf"])
                    P.tt("vector", hb, ps[5][:], decb[b], ALU.mult, [PK[5], "decb%d" % b], ["hb"])
                    P.tt("vector", hso[b], hf, hb, ALU.add, ["hf", "hb"], ["hso%d" % b])
                    P.tt("vector", hdo[b], hf, hb, ALU.subtract, ["hf", "hb"], ["hdo%d" % b])
                    P.dma("gpsimd", hsd[0, jt * 128:(jt + 1) * 128, :], hso[b], ["hso%d" % b], [("D:hsd", 0, jt)])
                    P.dma("gpsimd", hsd[1, jt * 128:(jt + 1) * 128, :], hdo[b], ["hdo%d" % b], [("D:hsd", 1, jt)])
                for sig in range(2):
                    hk_ = [("D:hsd", sig, jt) for jt in range(NT)]
                    P.dma("sync", ze, eo_view(hsd[sig], 0), hk_, ["ze"])
                    P.dma("sync", zo, eo_view(hsd[sig], 1), hk_, ["zo"])
                    for kc in range(17):
                        M = fwd_chunk(kc, ze, zo, ["ze", "zo"], cb, sb, T, tw,
                                      need=("X1r", "X2r") if sig == 0 else ("X1i", "X2i"))
                        if sig == 0:
                            P.dma("gpsimd", Hsp[o, 0, kc * 128:kc * 128 + M, :], T["x1r"][0:M, :], ["x1r"], [("D:Hsp", o, 0, kc)])
                            P.dma("gpsimd", Hsp[o, 2, kc * 128:kc * 128 + M, :], T["x2r"][0:M, :], ["x2r"], [("D:Hsp", o, 2, kc)])
                        else:
                            P.dma("gpsimd", Hsp[o, 1, kc * 128:kc * 128 + M, :], T["x1i"][0:M, :], ["x1i"], [("D:Hsp", o, 1, kc)])
                            P.dma("gpsimd", Hsp[o, 3, kc * 128:kc * 128 + M, :], T["x2i"][0:M, :], ["x2i"], [("D:Hsp", o, 3, kc)])

        def phase_B1(l):
            A.reset()
            cw = A.alloc("cw", [128, 36], F32)
            cbi = A.alloc("cbi", [128, 12], F32)
            ub = [A.alloc("ub%d" % i, [128, S], BF16) for i in range(2)]
            acc = [A.alloc("acc%d" % i, [128, S], F32) for i in range(2)]
            ucb = [A.alloc("ucb%d" % i, [128, S], BF16) for i in range(2)]
            ttm = [A.alloc("ttm%d" % i, [128, 32, 128], BF16) for i in range(2)]
            P.dma("sync", cw, I["hy_cw"][l], [], ["cw"])
            P.dma("sync", cbi, I["hy_cb"][l], [], ["cbi"])
            for ch in range(12):
                b = ch % 2
                P.dma("sync", ub[b], uT[ch * 128:(ch + 1) * 128, :], [("D:uT", ch, tg) for tg in range(8)], ["ub%d" % b])
                P.act(acc[b], ub[b], AF.Identity, ["ub%d" % b, "cw", "cbi"], ["acc%d" % b],
                      bias=cbi[:, ch:ch + 1], scale=cw[:, ch * 3 + 1:ch * 3 + 2])
                P.stt(acc[b][:, 1:S], ub[b][:, 0:S - 1], cw[:, ch * 3:ch * 3 + 1], acc[b][:, 1:S], ALU.mult, ALU.add,
                      ["ub%d" % b, "cw", "acc%d" % b], ["acc%d" % b])
                P.stt(ucb[b][:, 0:S - 1], ub[b][:, 1:S], cw[:, ch * 3 + 2:ch * 3 + 3], acc[b][:, 0:S - 1], ALU.mult, ALU.add,
                      ["ub%d" % b, "cw", "acc%d" % b], ["ucb%d" % b])
                P.copy("vector", ucb[b][:, S - 1:S], acc[b][:, S - 1:S], ["acc%d" % b, "ucb%d" % b], ["ucb%d" % b])
                for t8 in range(4):
                    bk = 4 + (t8 % 2)
                    pb = psb(bk)
                    for i in range(8):
                        tc = t8 * 8 + i
                        P.tr(pb[:, i * 128:(i + 1) * 128], ucb[b][:, tc * 128:(tc + 1) * 128], ident[:],
                             ["ucb%d" % b, "K:ident"], [PK[bk]])
                    P.copy("scalar" if t8 % 2 == 0 else "vector", ttm[b][:, t8 * 8:(t8 + 1) * 8, :],
                           pb.rearrange("p (c t) -> p c t", c=8), [PK[bk]], ["ttm%d" % b])
                which = ch // 4
                cc = ch % 4
                dst = ctm[which].rearrange("(tc p) c -> p tc c", p=128)[:, :, cc * 128:(cc + 1) * 128]
                P.dma("gpsimd", dst, ttm[b], ["ttm%d" % b], [("D:ctm", which, cc)])

        def phase_B2(l):
            A.reset()
            ze = [A.alloc("ze%d" % i, [128, 16, 512], BF16) for i in range(2)]
            zo = [A.alloc("zo%d" % i, [128, 16, 512], BF16) for i in range(2)]
            AA = {n: A.alloc(n, [128, 16, 512], BF16) for n in ("a0r", "a0i", "a1r", "a1i")}
            any_ = A.alloc("any", [1, 2, 512], BF16)
            cb = [A.alloc("cb%d" % i, [128, 16, 128], BF16) for i in range(2)]
            sb = [A.alloc("sb%d" % i, [128, 16, 128], BF16) for i in range(2)]
            tw = A.alloc("tw", [128, 51], F32)
            T = {n: A.alloc(n, [128, 512], F32) for n in ("wor", "woi", "t1", "t2", "x1r", "x1i", "x2r", "x2i",
                                                            "y1r", "y1i", "y2r", "y2i", "u1", "u2")}
            Hh = [A.alloc("hh%d" % i, [128, 512], F32) for i in range(4)]
            dbt = A.alloc("dbt", [128, 512], F32)
            gte = [A.alloc("gte%d" % i, [128, 512], BF16) for i in range(2)]
            zot = [A.alloc("zot%d" % i, [128, 512], BF16) for i in range(2)]
            P.dma("sync", tw, I["twid"], [], ["tw"])
            ck = [("D:ctm", 0, cc) for cc in range(4)]
            P.dma("sync", ze[0], eo_view(ctm[0], 0), ck, ["ze0"])
            P.dma("sync", zo[0], eo_view(ctm[0], 1), ck, ["zo0"])
            for o in range(2):
                zin = (ze[o], zo[o])
                zk = ["ze%d" % o, "zo%d" % o]
                P.dma("sync", dbt, I["hy_bias"][l, o], [], ["dbt"])
                for kc in range(17):
                    M = fwd_chunk(kc, zin[0], zin[1], zk, cb, sb, T, tw)
                    for q in range(4):
                        P.dma("sync", Hh[q][0:M, :], Hsp[o, q, kc * 128:kc * 128 + M, :], [("D:Hsp", o, q, kc)], ["hh%d" % q])
                    R = lambda n: T[n][0:M, :]
                    H1r, H1i, H2r, H2i = (Hh[q][0:M, :] for q in range(4))
                    for (xr, xi, hr, hi, yr, yi, e1, e2) in (("x1r", "x1i", H1r, H1i, "y1r", "y1i", "vector", "vector"),
                                                             ("x2r", "x2i", H2r, H2i, "y2r", "y2i", "vector", "vector")):
                        hq = ["hh0", "hh1"] if yr == "y1r" else ["hh2", "hh3"]
                        P.tt(e1, R("u1"), R(xr), hr, ALU.mult, [xr] + hq, ["u1"])
                        P.tt(e1, R("u2"), R(xi), hi, ALU.mult, [xi] + hq, ["u2"])
                        P.tt(e1, R(yr), R("u1"), R("u2"), ALU.subtract, ["u1", "u2"], [yr])
                        P.tt(e2, R("t1"), R(xr), hi, ALU.mult, [xr] + hq, ["t1"])
                        P.tt(e2, R("t2"), R(xi), hr, ALU.mult, [xi] + hq, ["t2"])
                        P.tt(e2, R(yi), R("t1"), R("t2"), ALU.add, ["t1", "t2"], [yi])
                    twr = tw[0:M, kc * 3:kc * 3 + 1]
                    tws = tw[0:M, kc * 3 + 1:kc * 3 + 2]
                    twn = tw[0:M, kc * 3 + 2:kc * 3 + 3]
                    if kc < 16:
                        P.tt("vector", AA["a0r"][:, kc, :], R("y1r"), R("y2r"), ALU.add, ["y1r", "y2r"], [("a0r", kc)])
                        P.tt("vector", AA["a0i"][:, kc, :], R("y1i"), R("y2i"), ALU.subtract, ["y1i", "y2i"], [("a0i", kc)])
                        P.tt("vector", R("u1"), R("y1r"), R("y2r"), ALU.subtract, ["y1r", "y2r"], ["u1"])
                        P.tt("vector", R("u2"), R("y1i"), R("y2i"), ALU.add, ["y1i", "y2i"], ["u2"])
                        smul(R("t1"), R("u2"), twn, ["u2", "tw"], ["t1"])
                        P.stt(AA["a1r"][:, kc, :], R("u1"), twr, R("t1"), ALU.mult, ALU.add, ["u1", "tw", "t1"], [("a1r", kc)])
                        smul(R("t2"), R("u1"), tws, ["u1", "tw"], ["t2"])
                        P.stt(AA["a1i"][:, kc, :], R("u2"), twr, R("t2"), ALU.mult, ALU.add, ["u2", "tw", "t2"], [("a1i", kc)])
                        if kc == 0:
                            for n in ("a0r", "a1r"):
                                P.ts("vector", AA[n][0:1, 0, :], AA[n][0:1, 0, :], 0.5, None, ALU.mult, None, [(n, 0)], [(n, 0)])
                    else:
                        P.copy("vector", any_[0:1, 0, :], R("y1r"), ["y1r"], ["any"])
                        P.ts("vector", any_[0:1, 1, :], R("y1i"), -1.0, None, ALU.mult, None, ["y1i", "any"], ["any"])
                akeys = {n: [(n, kc) for kc in range(16)] for n in AA}
                for v in range(2):
                    ar, ai = ("a0r", "a0i") if v == 0 else ("a1r", "a1i")
                    zv = zin[v]
                    for uc in range(16):
                        b = uc % 2
                        table_loads(cb, sb, b, uc)
                        gsrc = ctm[1 + o].rearrange("(uc p two) c -> two p uc c", p=128, two=2)[v][:, uc, :]
                        P.dma("sync", gte[b], gsrc, [("D:ctm", 1 + o, cc) for cc in range(4)], ["gte%d" % b])
                        bk = 4 + b
                        for kc in range(16):
                            P.mm(ps[bk][:], cb[b][:, kc, :], AA[ar][:, kc, :], kc == 0, False, ["cb%d" % b] + akeys[ar], [PK[bk]])
                        for kc in range(16):
                            P.mm(ps[bk][:], sb[b][:, kc, :], AA[ai][:, kc, :], False, False, ["sb%d" % b] + akeys[ai], [PK[bk]])
                        P.mm(ps[bk][:], altrow[0:1, :], any_[0:1, v, :], False, True, ["K:altrow", "any"], [PK[bk]])
                        P.tt("vector", T["u1"], dbt, zv[:, uc, :], ALU.mult, ["dbt", zk[v]], ["u1"])
                        P.stt(T["u2"], ps[bk][:], 2.0 / NFFT, T["u1"], ALU.mult, ALU.add, [PK[bk], "u1"], ["u2"])
                        if o == 0:
                            dstz = (ze[1], zo[1])[v]
                            P.tt("vector", dstz[:, uc, :], T["u2"], gte[b], ALU.mult, ["u2", "gte%d" % b], [("ze1", "zo1")[v]])
                        else:
                            P.tt("vector", zot[b], T["u2"], gte[b], ALU.mult, ["u2", "gte%d" % b], ["zot%d" % b])
                            dsty = yhtm.rearrange("(uc p two) c -> two p uc c", p=128, two=2)[v][:, uc, :]
                            P.dma("gpsimd", dsty, zot[b], ["zot%d" % b], [("D:yhtm", v, uc)])
            ytk = [("D:yhtm", v, uc) for v in range(2) for uc in range(16)]
            for tc in range(32):
                b = tc % 2
                P.dma("sync", gte[b], yhtm[tc * 128:(tc + 1) * 128, :], ytk, ["gte%d" % b])
                pb = psb(6 + b)
                for cc in range(4):
                    P.tr(pb[:, cc * 128:(cc + 1) * 128], gte[b][:, cc * 128:(cc + 1) * 128], ident[:],
                         ["gte%d" % b, "K:ident"], [PK[6 + b]])
                ysv = zot[b].rearrange("p (c t) -> p c t", c=4)
                P.copy("scalar", ysv, pb[:, 0:512].rearrange("p (c t) -> p c t", c=4), [PK[6 + b]], ["zot%d" % b])
                dst = yhT.rearrange("(cc p) t -> p cc t", p=128)[:, :, tc * 128:(tc + 1) * 128]
                P.dma("gpsimd", dst, ysv, ["zot%d" % b], [("D:yhT", tc)])

        def phase_C(l):
            A.reset()
            lam_init = 0.8 - 0.6 * math.exp(-0.3 * l)
            lv = A.alloc("lv", [128, 256], F32)
            lj = A.alloc("lj", [128, 64], F32)
            gs = A.alloc("gs", [128, 128], F32)
            QT = [A.alloc("QT%d" % i, [128, S], BF16) for i in range(2)]
            KT = [A.alloc("KT%d" % i, [128, S], BF16) for i in range(2)]
            Vh = [A.alloc("Vh%d" % i, [128, 32, 130], BF16) for i in range(2)]
            Et = [A.alloc("Et%d" % i, [128, 512], BF16) for i in range(3)]
            oc = [[A.alloc("oc%d_%d" % (c, q), [128, 128], F32) for q in range(4)] for c in range(2)]
            rc = A.alloc("rc", [128, 8], F32)
            od = A.alloc("od", [128, 128], F32)
            oj = A.alloc("oj", [128, 128], BF16)
            s3 = A.alloc("s3a", [128, 4], F32)
            yb = A.alloc("yb", [128, 128], BF16)
            ydst = [A.alloc("ydst%d" % i, [128, 512], BF16) for i in range(2)]
            P.dma("sync", lv, I["lamv"][l], [], ["lv"])
            P.dma("sync", gs, I["subln"][l], [], ["gs"])
            P.tt("vector", lj, lv[:, 0:64], lv[:, 64:128], ALU.mult, ["lv"], ["lj"])
            P.add("vector", lambda e: e.tensor_reduce(lamt[:, 0:1], lj, AX.X, ALU.add), ["lj"], ["K:lamt"])
            P.tt("vector", lj, lv[:, 128:192], lv[:, 192:256], ALU.mult, ["lv", "lj"], ["lj"])
            P.add("vector", lambda e: e.tensor_reduce(lamt[:, 1:2], lj, AX.X, ALU.add), ["lj", "K:lamt"], ["K:lamt"])
            P.act(lamt[:, 3:5], lamt[:, 0:2], AF.Exp, ["K:lamt"], ["K:lamt"])
            P.tt("vector", lamt[:, 5:6], lamt[:, 4:5], lamt[:, 3:4], ALU.subtract, ["K:lamt"], ["K:lamt"])
            P.ts("vector", lamt[:, 2:3], lamt[:, 5:6], -lam_init, None, ALU.add, None, ["K:lamt"], ["K:lamt"])
            P.ts("vector", gs, gs, 1.0 - lam_init, None, ALU.mult, None, ["gs"], ["gs"])
            scale = 64 ** -0.5
            ecnt = [0]
            for h in range(4):
                hb_ = h % 2
                P.dma("sync", QT[hb_], qkT[h * 128:(h + 1) * 128, :], [("D:qkT", h, tg) for tg in range(8)], ["QT%d" % hb_])
                P.dma("sync", KT[hb_], qkT[512 + h * 128:512 + (h + 1) * 128, :], [("D:qkT", 4 + h, tg) for tg in range(8)],
                      ["KT%d" % hb_])
                P.dma("sync", Vh[hb_][:, :, 0:128], Vtm.rearrange("(kt p) c -> p kt c", p=128)[:, :, h * 128:(h + 1) * 128],
                      [("D:Vtm", j) for j in range(NT)], [("Vh%d" % hb_, 0)])
                P.memset("gpsimd", Vh[hb_][:, :, 128:129], 1.0, [("Vh%d" % hb_, 1)])
                vk = [("Vh%d" % hb_, 0), ("Vh%d" % hb_, 1)]
                for qg in range(8):
                    qs_ = slice(qg * 512, (qg + 1) * 512)
                    for c in range(2):
                        pr = slice(c * 64, (c + 1) * 64)
                        def score(kt):
                            P.mm(ps[kt % 2][:], KT[hb_][pr, kt * 128:(kt + 1) * 128], QT[hb_][pr, qs_], True, True,
                                 ["KT%d" % hb_, "QT%d" % hb_], [PK[kt % 2]])
                        score(0)
                        for kt in range(32):
                            sb_ = kt % 2
                            if kt + 1 < 32:
                                score(kt + 1)
                            ei = ecnt[0] % 3
                            ecnt[0] += 1
                            P.act(Et[ei], ps[sb_][:], AF.Exp, [PK[sb_]], ["Et%d" % ei], scale=scale)
                            for q4 in range(4):
                                P.mm(ps[2 + q4][:, 0:129], Et[ei][:, q4 * 128:(q4 + 1) * 128], Vh[hb_][:, kt, 0:129],
                                     kt == 0, kt == 31, ["Et%d" % ei] + vk, [PK[2 + q4]])
                        for q4 in range(4):
                            P.add("vector", (lambda q4=q4, c=c: lambda e: e.reciprocal(rc[:, c * 4 + q4:c * 4 + q4 + 1],
                                                                                       ps[2 + q4][:, 128:129]))(),
                                  [PK[2 + q4]], [("rc", c, q4)])
                            P.ts("vector", oc[c][q4], ps[2 + q4][:, 0:128], rc[:, c * 4 + q4:c * 4 + q4 + 1], None, ALU.mult, None,
                                 [PK[2 + q4], ("rc", c, q4)], ["oc%d_%d" % (c, q4)])
                    yi = qg % 2
                    pb = psb(6)
                    for q4 in range(4):
                        P.stt(od, oc[1][q4], lamt[:, 2:3], oc[0][q4], ALU.mult, ALU.add,
                              ["oc1_%d" % q4, "oc0_%d" % q4, "K:lamt"], ["od"])
                        rms_tile(od, "od", oj, s3, "s3a", 128, 1e-5)
                        P.stt(yb, od, s3[:, 2:3], gs, ALU.mult, ALU.mult, ["od", "s3a", "gs"], ["yb"])
                        P.tr(pb[:, q4 * 128:(q4 + 1) * 128], yb, ident[:], ["yb", "K:ident"], [PK[6]])
                    P.copy("scalar", ydst[yi], pb[:, 0:512], [PK[6]], ["ydst%d" % yi])
                    P.dma("gpsimd", ydT[h * 128:(h + 1) * 128, qs_], ydst[yi], ["ydst%d" % yi], [("D:ydT", h, qg)])

        def phase_D(l):
            A.reset()
            wuh = A.alloc("wuh", [128, 4, D], BF16)
            wua = A.alloc("wua", [128, 4, D], BF16)
            wo = A.alloc("wo", [128, 8, D], BF16)
            wst = [A.alloc("wst%d" % i, [128, 4, 512], F32) for i in range(2)]
            wstk = ["wst0", "wst1"]
            yh = [A.alloc("yh%d" % i, [128, 4, 512], BF16) for i in range(2)]
            yd = [A.alloc("yd%d" % i, [128, 4, 512], BF16) for i in range(2)]
            gg = [A.alloc("gg%d" % i, [128, 16, 512], BF16) for i in range(2)]
            mT = [A.alloc("mT%d" % i, [128, 8, 512], BF16) for i in range(2)]
            ta = A.alloc("ta", [128, 512], F32)
            tb = A.alloc("tb", [128, 512], F32)
            xt = [A.alloc("xt%d" % i, [128, D], F32) for i in range(2)]
            xo = [A.alloc("xo%d" % i, [128, D], F32) for i in range(2)]
            for (dst, nm, src) in ((wuh, "wuh", I["w_up_hy"][l]), (wua, "wua", I["w_up_da"][l])):
                sv = src.rearrange("(c p) f -> p c f", p=128)
                for h in range(2):
                    load_w(dst[:, :, h * 512:(h + 1) * 512], (nm, h), sv[:, :, h * 512:(h + 1) * 512], wst, wstk)
            sv = I["w_out"][l].rearrange("(c p) f -> p c f", p=128)
            for c2 in range(2):
                for h in range(2):
                    load_w(wo[:, c2 * 4:(c2 + 1) * 4, h * 512:(h + 1) * 512], ("wo", c2, h),
                           sv[:, c2 * 4:(c2 + 1) * 4, h * 512:(h + 1) * 512], wst, wstk)
            wok = [("wo", c2, h) for c2 in range(2) for h in range(2)]
            xcnt = [0]
            for tg in range(8):
                b = tg % 2
                sl = slice(tg * 512, (tg + 1) * 512)
                P.dma("sync", yh[b], yhT.rearrange("(cc p) t -> p cc t", p=128)[:, :, sl],
                      [("D:yhT", tc) for tc in range(tg * 4, tg * 4 + 4)], ["yh%d" % b])
                P.dma("sync", yd[b], ydT.rearrange("(cc p) t -> p cc t", p=128)[:, :, sl],
                      [("D:ydT", h, tg) for h in range(4)], ["yd%d" % b])
                P.dma("sync", gg[b], gT.rearrange("(cc p) t -> p cc t", p=128)[:, :, sl],
                      [("D:gT", gc, tg) for gc in range(16)], ["gg%d" % b])
                for dm in range(8):
                    for cc in range(4):
                        P.mm(ps[0][:], wuh[:, cc, dm * 128:(dm + 1) * 128], yh[b][:, cc, :], cc == 0, cc == 3,
                             [("wuh", 0), ("wuh", 1), "yh%d" % b], [PK[0]])
                    for cc in range(4):
                        P.mm(ps[1][:], wua[:, cc, dm * 128:(dm + 1) * 128], yd[b][:, cc, :], cc == 0, cc == 3,
                             [("wua", 0), ("wua", 1), "yd%d" % b], [PK[1]])
                    P.tt("vector", ta, ps[0][:], gg[b][:, dm, :], ALU.mult, [PK[0], "gg%d" % b], ["ta"])
                    P.tt("vector", tb, ps[1][:], gg[b][:, 8 + dm, :], ALU.mult, [PK[1], "gg%d" % b], ["tb"])
                    P.tt("vector", mT[b][:, dm, :], ta, tb, ALU.add, ["ta", "tb"], [("mT%d" % b, dm)])
                mk = [("mT%d" % b, dm) for dm in range(8)]
                for tt_ in range(4):
                    j = tg * 4 + tt_
                    xb = xcnt[0] % 2
                    xcnt[0] += 1
                    P.dma("sync", xt[xb], xres[j * 128:(j + 1) * 128, :], [("D:xres", j)], ["xt%d" % xb])
                    for og in range(2):
                        bk = 2 + og
                        for dm in range(8):
                            P.mm(ps[bk][:], mT[b][:, dm, tt_ * 128:(tt_ + 1) * 128], wo[:, dm, og * 512:(og + 1) * 512],
                                 dm == 0, dm == 7, mk + wok, [PK[bk]])
                        P.tt("vector", xo[xb][:, og * 512:(og + 1) * 512], ps[bk][:], xt[xb][:, og * 512:(og + 1) * 512], ALU.add,
                             [PK[bk], "xt%d" % xb], [("xo%d" % xb, og)])
                    P.dma("gpsimd", xres[j * 128:(j + 1) * 128, :], xo[xb], [("xo%d" % xb, 0), ("xo%d" % xb, 1)], [("D:xres", j)])

        def phase_E1(l):
            A.reset()
            gt = A.alloc("gt", [128, D], F32)
            wr = A.alloc("wr", [128, 8, 16], F32)
            br = A.alloc("br", [128, 16], F32)
            xt = [A.alloc("xt%d" % i, [128, D], F32) for i in range(2)]
            junk = A.alloc("junk", [128, D], BF16)
            xn = [A.alloc("xn%d" % i, [128, D], F32) for i in range(2)]
            nb = [A.alloc("nb%d" % i, [128, D], BF16) for i in range(2)]
            xT = [A.alloc("xT%d" % i, [128, 8, 128], F32) for i in range(2)]
            s3 = [A.alloc("s3%d" % i, [128, 4], F32) for i in range(2)]
            lg = A.alloc("lg", [128, 32, 16], F32)
            mx = A.alloc("mx", [128, 32], F32)
            ex = A.alloc("ex", [128, 32, 16], F32)
            aff = A.alloc("aff", [128, 32, 16], F32)
            lo = A.alloc("lo", [128, 16], F32)
            mid = A.alloc("mid", [128, 16], F32)
            cmp_ = A.alloc("cmp", [128, 32, 16], F32)
            cnt = A.alloc("cnt", [128, 16], F32)
            onesm = A.alloc("onesm", [128, 128], F32)
            ge = A.alloc("ge", [128, 16], F32)
            mask = A.alloc("mask", [128, 16, 32], F32)
            maskb = A.alloc("maskb", [128, 16, 32], BF16)
            csum = A.alloc("csum", [128, 16, 32], F32)
            onesf = A.alloc("onesf", [128, 512], F32)
            base = A.alloc("base", [128, 16], F32)
            mcum = A.alloc("mcum", [128, 16, 32], F32)
            mcumb = A.alloc("mcumb", [128, 16, 32], BF16)
            ptmp = A.alloc("ptmp", [128, 16, 32], F32)
            ahi = A.alloc("ahi", [128, 32, 16], BF16)
            P.dma("sync", gt, I["norm_ffn"][l], [], ["gt"])
            P.dma("sync", wr, I["w_router"][l].rearrange("(c p) e -> p c e", p=128), [], ["wr"])
            P.dma("sync", br, I["b_router"][l], [], ["br"])
            for j in range(NT):
                b = j % 2
                P.dma("sync", xt[b], xres[j * 128:(j + 1) * 128, :], [("D:xres", j)], ["xt%d" % b])
                rms_tile(xt[b], "xt%d" % b, junk, s3[b], "s3%d" % b, D, 1e-6)
                P.stt(xn[b], xt[b], s3[b][:, 2:3], gt, ALU.mult, ALU.mult, ["xt%d" % b, "s3%d" % b, "gt"], ["xn%d" % b])
                P.copy("scalar", nb[b], xn[b], ["xn%d" % b], ["nb%d" % b])
                P.dma("gpsimd", n2d[j * 128:(j + 1) * 128, :], nb[b], ["nb%d" % b], [("D:n2d", j)])
                for c in range(8):
                    bk = 4 + 2 * b + c // 4
                    P.tr(ps[bk][:, (c % 4) * 128:(c % 4 + 1) * 128], xn[b][:, c * 128:(c + 1) * 128], identf[:],
                         ["xn%d" % b, "K:identf"], [PK[bk]])
                P.copy("scalar", xT[b][:, 0:4, :], ps[4 + 2 * b][:].rearrange("p (c t) -> p c t", c=4), [PK[4 + 2 * b]],
                       [("xT%d" % b, 0)])
                P.copy("vector", xT[b][:, 4:8, :], ps[5 + 2 * b][:].rearrange("p (c t) -> p c t", c=4), [PK[5 + 2 * b]],
                       [("xT%d" % b, 1)])
                for c in range(8):
                    P.mm(ps[b][:, 0:16], xT[b][:, c, :], wr[:, c, :], c == 0, c == 7,
                         [("xT%d" % b, 0), ("xT%d" % b, 1), "wr"], [PK[b]])
                P.tt("vector", lg[:, j, :], ps[b][:, 0:16], br, ALU.add, [PK[b], "br"], ["lg"])
            P.add("vector", lambda e: e.tensor_reduce(mx, lg, AX.X, ALU.max), ["lg"], ["mx"])
            P.tt("vector", ex, lg, mx.unsqueeze(2).broadcast_to([128, 32, 16]), ALU.subtract, ["lg", "mx"], ["ex"])
            P.act(ex, ex, AF.Exp, ["ex"], ["ex"])
            P.add("vector", lambda e: e.tensor_reduce(mx, ex, AX.X, ALU.add), ["ex"], ["mx"])
            P.add("vector", lambda e: e.reciprocal(mx, mx), ["mx"], ["mx"])
            P.tt("vector", aff, ex, mx.unsqueeze(2).broadcast_to([128, 32, 16]), ALU.mult, ["ex", "mx"], ["aff"])
            P.memset("vector", lo, 0.0, ["lo"])
            P.memset("vector", onesf, 1.0, ["onesf"])
            P.memset("vector", onesm, 1.0, ["onesm"])
            for it in range(28):
                hstep = 0.5 ** (it + 1)
                P.ts("vector", mid, lo, hstep, None, ALU.add, None, ["lo"], ["mid"])
                P.tt("vector", cmp_, aff, mid.unsqueeze(1).broadcast_to([128, 32, 16]), ALU.is_ge, ["aff", "mid"], ["cmp"])
                P.add("vector", lambda e: e.tensor_reduce(cnt, cmp_.rearrange("p j e -> p e j"), AX.X, ALU.add), ["cmp"], ["cnt"])
                P.mm(ps[2][:, 0:16], onesm, cnt, True, True, ["onesm", "cnt"], [PK[2]])
                P.ts("vector", ge, ps[2][:, 0:16], 511.5, hstep, ALU.is_ge, ALU.mult, [PK[2]], ["ge"])
                P.tt("vector", lo, lo, ge, ALU.add, ["lo", "ge"], ["lo"])
            P.tt("vector", mask, aff.rearrange("p j e -> p e j"), lo.unsqueeze(2).broadcast_to([128, 16, 32]), ALU.is_ge,
                 ["aff", "lo"], ["mask"])
            P.copy("vector", maskb, mask, ["mask"], ["maskb"])
            mflat = mask.rearrange("p e j -> p (e j)")
            cflat = csum.rearrange("p e j -> p (e j)")
            P.add("vector", lambda e: e.tensor_tensor_scan(cflat, onesf, mflat, 0.0, ALU.mult, ALU.add), ["mask", "onesf"], ["csum"])
            P.memset("vector", base[:, 0:1], 0.0, [("base", 0)])
            P.copy("vector", base[:, 1:16], csum[:, 0:15, 31], ["csum"], [("base", 1)])
            P.tt("vector", mcum, csum, mask, ALU.subtract, ["csum", "mask"], ["mcum"])
            P.tt("vector", mcum, mcum, base.unsqueeze(2).broadcast_to([128, 16, 32]), ALU.subtract,
                 ["mcum", ("base", 0), ("base", 1)], ["mcum"])
            P.copy("vector", mcumb, mcum, ["mcum"], ["mcumb"])
            P.mm(ps[3][:], ustr[:], maskb.rearrange("p e j -> p (e j)"), True, False, ["K:ustr", "maskb"], [PK[3]])
            P.mm(ps[3][:], ones[:], mcumb.rearrange("p e j -> p (e j)"), False, True, ["K:ones", "mcumb"], [PK[3]])
            pf = ptmp.rearrange("p e j -> p (e j)")
            P.ts("vector", pf, ps[3][:], 1.0, None, ALU.add, None, [PK[3]], ["ptmp"])
            P.tt("vector", pf, pf, mflat, ALU.mult, ["ptmp", "mask"], ["ptmp"])
            P.ts("vector", posm[:].rearrange("p e j -> p (e j)"), pf, -1.0, None, ALU.add, None, ["ptmp"], ["K:posm"])
            jpt = A.alloc("jpt", [128, 32, 2], BF16)
            P.dma("sync", jpt, I["jp"].rearrange("p (j two) -> p j two", two=2), [], ["jpt"])
            P.copy("vector", affhl[:, :, :, 2:4], jpt.unsqueeze(2).broadcast_to([128, 32, 16, 2]), ["jpt", "K:affhl"], ["K:affhl"])
            P.copy("vector", ahi, aff, ["aff"], ["ahi"])
            P.copy("vector", affhl[:, :, :, 0], ahi, ["ahi"], ["K:affhl"])
            P.tt("vector", affhl[:, :, :, 1], aff, ahi, ALU.subtract, ["aff", "ahi", "K:affhl"], ["K:affhl"])
            if dbg:
                P.dma("gpsimd", dbgaff, aff.rearrange("p j e -> p (j e)"), ["aff"], ["D:dbgaff"])
                P.dma("gpsimd", dbgpos, posm[:].rearrange("p e j -> p (e j)"), ["K:posm"], ["D:dbgpos"])

        def phase_E2(l):
            A.reset()
            Sel = A.alloc("Sel", [128, 32, 512], BF16)
            xgt = A.alloc("xgt", [128, 4, D], BF16)
            xg = A.alloc("xg", [128, 8, 512], BF16)
            hT = A.alloc("hT", [128, 16, 512], BF16)
            yy = [A.alloc("yy%d" % i, [128, 4, D], F32) for i in range(2)]
            wsl = [A.alloc("wsl%d" % i, [128, 8, 512], BF16) for i in range(4)]
            wst = [A.alloc("wst%d" % i, [128, 8, 512], F32) for i in range(3)]
            wstk = ["wst0", "wst1", "wst2"]
            iot = A.alloc("iot", [128, 512], F32)
            g16 = A.alloc("g16", [128, 16], F32)
            gsl = [A.alloc("gsl%d" % i, [128, 4], F32) for i in range(2)]
            idxf = A.alloc("idxf", [128, 4], F32)
            idxi = [A.alloc("idxi%d" % i, [128, 4], I32) for i in range(2)]
            sg = [A.alloc("sg%d" % i, [128, 512], F32) for i in range(2)]
            P.dma("sync", iot, I["iota"], [], ["iot"])
            n2k = [("D:n2d", j) for j in range(NT)]
            xk = [("D:xres", j) for j in range(NT)]
            wc = [0]

            def wload(src3):
                i = wc[0] % 4
                wc[0] += 1
                load_w(wsl[i], "wsl%d" % i, src3, wst, wstk)
                return wsl[i], "wsl%d" % i

            selk = [("Sel", q) for q in range(4)]
            xgk = [("xg", q) for q in range(4)]
            hk = [("hT", f) for f in range(16)]

            def prep1(e_):
                eb = e_ % 2
                for j in range(32):
                    P.ts("vector", Sel[:, j, :], iot, posm[:, e_, j:j + 1], None, ALU.is_equal, None,
                         ["iot", "K:posm"], [("Sel", j // 8)])
                for sc in range(4):
                    for j in range(32):
                        P.mm(ps[7][:, sc * 4:sc * 4 + 4], Sel[:, j, sc * 128:(sc + 1) * 128], affhl[:, j, e_, :], j == 0, j == 31,
                             selk + ["K:affhl"], [PK[7]])
                P.copy("vector", g16, ps[7][:, 0:16], [PK[7]], ["g16"])
                g4 = g16.rearrange("p (s f) -> p s f", f=4)
                P.tt("vector", gsl[eb], g4[:, :, 0], g4[:, :, 1], ALU.add, ["g16"], ["gsl%d" % eb])
                P.stt(idxf, g4[:, :, 2], 128.0, g4[:, :, 3], ALU.mult, ALU.add, ["g16"], ["idxf"])
                P.copy("vector", idxi[eb], idxf, ["idxf"], ["idxi%d" % eb])
                for sc in range(4):
                    P.add("gpsimd", (lambda sc=sc, eb=eb: lambda e: e.indirect_dma_start(
                        out=xgt[:, sc, :], out_offset=None, in_=n2d,
                        in_offset=bass.IndirectOffsetOnAxis(ap=idxi[eb][:, sc:sc + 1], axis=0)))(),
                        ["idxi%d" % eb] + n2k, [("xgt", sc)], dma=True)

            def prep2(e_):
                pb = psb(3)
                for dc2 in range(4):
                    for h in range(2):
                        dc = dc2 * 2 + h
                        for sc in range(4):
                            P.tr(pb[:, h * 512 + sc * 128:h * 512 + (sc + 1) * 128], xgt[:, sc, dc * 128:(dc + 1) * 128], ident[:],
                                 [("xgt", sc), "K:ident"], [PK[3]])
                    P.copy("scalar" if dc2 % 2 == 0 else "vector", xg[:, dc2 * 2:dc2 * 2 + 2, :],
                           pb.rearrange("p (h s) -> p h s", h=2), [PK[3]], [("xg", dc2)])

            def ffn_up(e_):
                gsrc = I["w_e_gate"][l, e_].rearrange("(c p) f -> p c f", p=128)
                usrc = I["w_e_up"][l, e_].rearrange("(c p) f -> p c f", p=128)
                for fg in range(4):
                    wg, wgk = wload(gsrc[:, :, fg * 512:(fg + 1) * 512])
                    wu, wuk = wload(usrc[:, :, fg * 512:(fg + 1) * 512])
                    for fc in range(4):
                        for dc in range(8):
                            P.mm(ps[4][:], wg[:, dc, fc * 128:(fc + 1) * 128], xg[:, dc, :], dc == 0, dc == 7, [wgk] + xgk, [PK[4]])
                        for dc in range(8):
                            P.mm(ps[5][:], wu[:, dc, fc * 128:(fc + 1) * 128], xg[:, dc, :], dc == 0, dc == 7, [wuk] + xgk, [PK[5]])
                        si = fc % 2
                        P.act(sg[si], ps[4][:], AF.Silu, [PK[4]], ["sg%d" % si])
                        P.tt("vector", hT[:, fg * 4 + fc, :], sg[si], ps[5][:], ALU.mult, ["sg%d" % si, PK[5]], [("hT", fg * 4 + fc)])

            def ffn_down(e_):
                eb = e_ % 2
                dsrc = I["w_e_down"][l, e_].rearrange("(c p) d -> p c d", p=128)
                dbanks = [0, 1, 2, 6]
                for dg in range(2):
                    for fh in range(2):
                        wd, wdk = wload(dsrc[:, fh * 8:(fh + 1) * 8, dg * 512:(dg + 1) * 512])
                        for sc in range(4):
                            for f8 in range(8):
                                fc = fh * 8 + f8
                                P.mm(ps[dbanks[sc]][:], hT[:, fc, sc * 128:(sc + 1) * 128], wd[:, f8, :], fc == 0, fc == 15,
                                     hk + [wdk], [PK[dbanks[sc]]])
                    for sc in range(4):
                        P.ts("vector", yy[eb][:, sc, dg * 512:(dg + 1) * 512], ps[dbanks[sc]][:],
                             gsl[eb][:, sc:sc + 1], None, ALU.mult, None, [PK[dbanks[sc]], "gsl%d" % eb], [("yy%d" % eb, sc, dg)])

            def scatter(e_):
                eb = e_ % 2
                for sc in range(4):
                    P.add("gpsimd", (lambda sc=sc, eb=eb: lambda e: e.indirect_dma_start(
                        out=xres, out_offset=bass.IndirectOffsetOnAxis(ap=idxi[eb][:, sc:sc + 1], axis=0),
                        in_=yy[eb][:, sc, :], in_offset=None, compute_op=ALU.add))(),
                        ["idxi%d" % eb, ("yy%d" % eb, sc, 0), ("yy%d" % eb, sc, 1)] + xk, xk, dma=True)

            prep1(0)
            prep2(0)
            for e_ in range(16):
                ffn_up(e_)
                if e_ + 1 < 16:
                    prep1(e_ + 1)
                ffn_down(e_)
                if e_ + 1 < 16:
                    prep2(e_ + 1)
                scatter(e_)

        def phase_F():
            A.reset()
            gt = A.alloc("gt", [128, D], F32)
            xt = [A.alloc("xt%d" % i, [128, D], F32) for i in range(2)]
            junk = A.alloc("junk", [128, D], BF16)
            xo = [A.alloc("xo%d" % i, [128, D], F32) for i in range(2)]
            s3 = [A.alloc("s3%d" % i, [128, 4], F32) for i in range(2)]
            P.dma("sync", gt, I["norm_final"], [], ["gt"])
            for j in range(NT):
                b = j % 2
                P.dma("sync", xt[b], xres[j * 128:(j + 1) * 128, :], [("D:xres", j)], ["xt%d" % b])
                rms_tile(xt[b], "xt%d" % b, junk, s3[b], "s3%d" % b, D, 1e-6)
                P.stt(xo[b], xt[b], s3[b][:, 2:3], gt, ALU.mult, ALU.mult, ["xt%d" % b, "s3%d" % b, "gt"], ["xo%d" % b])
                P.dma("gpsimd", out[j * 128:(j + 1) * 128, :], xo[b], ["xo%d" % b], [("D:out", j)])

        for l in range(depth):
            if "A" in stages:
                phase_A(l)
            if "B0" in stages:
                phase_B0(l)
            if "B1" in stages:
                phase_B1(l)
            if "B2" in stages:
                phase_B2(l)
            if "C" in stages:
                phase_C(l)
            if "D" in stages:
                phase_D(l)
            if "E" in stages:
                phase_E1(l)
                phase_E2(l)
        if "F" in stages:
            phase_F()
        fin = [k for k in P.state if (k if isinstance(k, str) else k[0]).startswith("D:")]
        P.add("sync", lambda e: e.nop(), fin, [])
        P.add("gpsimd", lambda e: e.nop(), fin, [])
        P.emit(st)
        print("ops", len(P.ops), "maxsem", P.maxsem, flush=True)
    return nc


_CONST = {}


def _consts():
    if _CONST:
        return _CONST
    bf = ml_dtypes.bfloat16
    half = 32
    inv = (10000.0 ** (-np.arange(half, dtype=np.float32) * 2.0 / 64)).astype(np.float32)
    pos = np.arange(S, dtype=np.float32)
    ang = pos[:, None] * inv[None, :]
    cos = np.cos(ang).astype(np.float32).T
    sin = np.sin(ang).astype(np.float32).T
    cosT = np.zeros((128, S), np.float32)
    sinT = np.zeros((128, S), np.float32)
    for p in range(128):
        i = p % 32
        cosT[p] = cos[i]
        sinT[p] = -sin[i] if (p % 64) < 32 else sin[i]
    _CONST["cosT"] = cosT
    _CONST["sinT"] = sinT
    L = S
    t = np.linspace(0.0, 1.0, L, dtype=np.float32)
    bands = 16
    w = (2.0 * np.float32(math.pi) * np.arange(L, dtype=np.float32) / L).astype(np.float32)
    fr = np.linspace(1e-4, bands - 1, bands, dtype=np.float32)
    a = w[:, None] * fr[None, :]
    z = np.concatenate([t[:, None], np.cos(a), -np.sin(a)], axis=-1).astype(np.float32)
    _CONST["zT"] = np.ascontiguousarray(z.T)
    mind = math.log(1e-2) / 0.3
    maxd = math.log(1e-2) / 1.5
    deltas = np.abs(np.linspace(mind, maxd, 512, dtype=np.float32))
    decay = np.exp(-t[:, None] * deltas[None, :]).astype(np.float32)
    _CONST["decay"] = decay
    db = decay.copy()
    db[0] = 0.0
    _CONST["decayb"] = db
    n = np.arange(2048, dtype=np.int64)
    prod = (n[:, None] * n[None, :]) % 4096
    angd = prod.astype(np.float64) * (2.0 * math.pi / 4096)
    for nm, fn in (("ctab", np.cos), ("stab", lambda v: -np.sin(v))):
        m = fn(angd).astype(np.float32)
        m = m.reshape(16, 128, 16, 128)
        m = np.ascontiguousarray(m.transpose(2, 1, 0, 3)).reshape(16, 128, 16 * 128)
        _CONST[nm] = m.astype(bf)
    kk = np.arange(17 * 128, dtype=np.float64)
    ph = kk * (2.0 * math.pi / NFFT)
    tw = np.stack([np.cos(ph), np.sin(ph), -np.sin(ph)], axis=-1).astype(np.float32)
    _CONST["twid"] = np.ascontiguousarray(tw.reshape(17, 128, 3).transpose(1, 0, 2)).reshape(128, 51)
    altv = np.where(np.arange(128) % 2 == 0, 1.0, -1.0).astype(np.float32)
    _CONST["alt"] = np.stack([altv, altv], axis=1).astype(bf)
    _CONST["altrow"] = altv[None, :].astype(bf)
    _CONST["iota"] = np.broadcast_to(np.arange(512, dtype=np.float32)[None, :], (128, 512)).copy()
    jp = np.zeros((128, 32, 2), np.float32)
    jp[:, :, 0] = np.arange(32, dtype=np.float32)[None, :]
    jp[:, :, 1] = np.arange(128, dtype=np.float32)[:, None]
    _CONST["jp"] = jp.reshape(128, 64).astype(bf)
    _CONST["ident_bf"] = np.eye(128, dtype=np.float32).astype(bf)
    _CONST["ident_f"] = np.eye(128, dtype=np.float32)
    _CONST["ustrict"] = np.triu(np.ones((128, 128), np.float32), 1).astype(bf)
    _CONST["ones_bf"] = np.ones((128, 128), np.float32).astype(bf)
    return _CONST


def _rep(v, n=128):
    return np.ascontiguousarray(np.broadcast_to(v[..., None, :], v.shape[:-1] + (n, v.shape[-1])))


def prep_shared(inp):
    c = dict(_consts())
    w_in = np.asarray(inp["w_in"])
    b_in = np.asarray(inp["b_in"])
    sw = np.concatenate([np.arange(h * 64 + 32, h * 64 + 64).tolist() + np.arange(h * 64, h * 64 + 32).tolist()
                         for h in range(8)]).astype(np.int64)
    u0, q0, k0, v0, gh0, ga0 = 0, 1536, 2048, 2560, 3072, 4096
    cols = np.concatenate([np.arange(u0, u0 + 1536), np.arange(q0, q0 + 512), np.arange(k0, k0 + 512),
                           q0 + sw, k0 + sw, np.arange(gh0, gh0 + 1024), np.arange(ga0, ga0 + 1024),
                           np.arange(v0, v0 + 512)])
    c["w_in"] = np.ascontiguousarray(w_in[:, :, cols])
    bext = b_in[:, cols]
    c["b_in"] = np.ascontiguousarray(bext.reshape(2, 48, 128).transpose(0, 2, 1))
    c["b_v"] = _rep(b_in[:, v0:v0 + 512])
    c["norm_mix"] = _rep(np.asarray(inp["norm_mix"]))
    c["norm_ffn"] = _rep(np.asarray(inp["norm_ffn"]))
    c["norm_final"] = _rep(np.asarray(inp["norm_final"]))
    cw = np.asarray(inp["hy_conv_w"])
    c["hy_cw"] = np.ascontiguousarray(cw.reshape(2, 3, 12, 128).transpose(0, 3, 2, 1)).reshape(2, 128, 36)
    c["hy_cb"] = np.ascontiguousarray(np.asarray(inp["hy_conv_b"]).reshape(2, 12, 128).transpose(0, 2, 1))
    c["hy_w1"] = np.asarray(inp["hy_ffn_w1"])
    c["hy_w2"] = np.asarray(inp["hy_ffn_w2"])
    c["hy_w3"] = np.asarray(inp["hy_ffn_w3"])
    c["hy_cols"] = np.ascontiguousarray(np.stack([np.asarray(inp["hy_ffn_b1"]), np.asarray(inp["hy_ffn_f1"]),
                                                  np.asarray(inp["hy_ffn_b2"]), np.asarray(inp["hy_ffn_f2"])], axis=-1))
    c["hy_bias"] = _rep(np.asarray(inp["hy_bias"]))
    lam = np.concatenate([np.asarray(inp["lambda_q1"]), np.asarray(inp["lambda_k1"]),
                          np.asarray(inp["lambda_q2"]), np.asarray(inp["lambda_k2"])], axis=-1)
    c["lamv"] = _rep(lam)
    c["subln"] = _rep(np.asarray(inp["subln_g"]))
    c["w_up_hy"] = np.asarray(inp["w_up_hyena"])
    c["w_up_da"] = np.asarray(inp["w_up_attn"])
    c["w_out"] = np.asarray(inp["w_out"])
    c["w_router"] = np.asarray(inp["w_router"])
    c["b_router"] = _rep(np.asarray(inp["b_router"]))
    c["w_e_gate"] = np.asarray(inp["w_e_gate"])
    c["w_e_up"] = np.asarray(inp["w_e_up"])
    c["w_e_down"] = np.asarray(inp["w_e_down"])
    return {k: np.ascontiguousarray(v) for k, v in c.items()}


def kernel(**inputs):
    x = np.asarray(inputs["x"], dtype=np.float32)
    shared = prep_shared(inputs)
    nc = build(bass.Bass("TRN2", target_bir_lowering=False))
    in_maps = []
    for core in range(8):
        m = dict(shared)
        m["x"] = np.ascontiguousarray(x[core % 4])
        in_maps.append(m)
    res = run_bass_kernel_spmd(nc, in_maps, core_ids=list(range(8)))
    return np.stack([np.asarray(res.results[b]["out"], dtype=np.float32) for b in range(4)], axis=0)
```

```python
import numpy as np
import concourse.bass as bass
import concourse.mybir as mybir

F32 = mybir.dt.float32
BF16 = mybir.dt.bfloat16
I32 = mybir.dt.int32
AF = mybir.ActivationFunctionType
ALU = mybir.AluOpType
AX = mybir.AxisListType

COMPUTE = ("tensor", "vector", "scalar", "gpsimd")
NDSEM = 16


class Op:
    __slots__ = ("id", "eng", "fn", "deps", "signal", "semval", "is_dma", "dsem", "dval", "dprev")


class Prog:
    def __init__(self, nc):
        self.nc = nc
        self.ops = []
        self.state = {}

    def _prune(self, ids):
        best = {}
        out = set()
        for i in ids:
            o = self.ops[i]
            if o.is_dma:
                out.add(i)
            else:
                if o.eng not in best or best[o.eng] < i:
                    best[o.eng] = i
        out.update(best.values())
        return out

    def add(self, eng, fn, r=(), w=(), dma=False):
        o = Op()
        o.id = len(self.ops)
        o.eng = eng
        o.fn = fn
        o.is_dma = dma
        o.signal = dma
        deps = set()
        for k in tuple(r) + tuple(w):
            if k not in self.state:
                nm = k if isinstance(k, str) else k[0]
                if not (nm.startswith("D:") or nm.startswith("K:")):
                    self.state[k] = [None, list(getattr(self, "_pend", []))]
            st = self.state.get(k)
            if st is not None and st[0] is not None:
                deps.add(st[0])
        for k in w:
            st = self.state.get(k)
            if st is not None:
                deps.update(st[1])
        o.deps = self._prune(deps)
        self.ops.append(o)
        for k in r:
            st = self.state.setdefault(k, [None, []])
            st[1].append(o.id)
            if len(st[1]) > 24:
                st[1] = list(self._prune(st[1]))
        for k in w:
            self.state[k] = [o.id, []]
        return o

    def phase_barrier(self, newkeys, oldprefix=None):
        pend = set()
        for k, st in list(self.state.items()):
            nm = k if isinstance(k, str) else k[0]
            if nm.startswith("D:") or nm.startswith("K:"):
                continue
            if st[0] is not None:
                pend.add(st[0])
            pend.update(st[1])
            del self.state[k]
        pend = list(self._prune(pend))
        self._pend = pend

    def fresh(self, key):
        self.state[key] = [None, list(getattr(self, "_pend", []))]

    def emit(self, stack):
        nc = self.nc
        engs = ["tensor", "vector", "scalar", "gpsimd", "sync"]
        sems = {e: stack.enter_context(nc.semaphore("s_" + e)) for e in COMPUTE}
        dsems = {e: [stack.enter_context(nc.semaphore("d_%s%d" % (e, i))) for i in range(NDSEM)]
                 for e in engs}
        for o in self.ops:
            for d in o.deps:
                p = self.ops[d]
                if p.is_dma:
                    continue
                if p.eng == "tensor" and o.eng == "tensor" and not o.is_dma:
                    continue
                p.signal = True
        cnt = {e: 0 for e in COMPUTE}
        dcnt = {e: 0 for e in engs}
        dval = {e: [0] * NDSEM for e in engs}
        for o in self.ops:
            if o.is_dma:
                i = dcnt[o.eng] % NDSEM
                dcnt[o.eng] += 1
                o.dsem = dsems[o.eng][i]
                o.dprev = dval[o.eng][i]
                dval[o.eng][i] += 16
                o.dval = dval[o.eng][i]
            elif o.signal:
                cnt[o.eng] += 1
                o.semval = cnt[o.eng]
        self.maxsem = dict(cnt)
        block = stack.enter_context(nc.Block())
        byeng = {e: [o for o in self.ops if o.eng == e] for e in engs}

        def run(engname, eobj):
            waited = {}

            def wait(sem, val):
                key = id(sem)
                if waited.get(key, 0) >= val:
                    return
                waited[key] = val
                eobj.wait_ge(sem, val)

            for o in byeng[engname]:
                for d in sorted(o.deps):
                    p = self.ops[d]
                    if p.is_dma:
                        wait(p.dsem, p.dval)
                    else:
                        if p.eng == "tensor" and engname == "tensor" and not o.is_dma:
                            continue
                        wait(sems[p.eng], p.semval)
                if o.is_dma:
                    if o.dprev > 0:
                        wait(o.dsem, o.dprev)
                    inst = o.fn(eobj)
                    inst.then_inc(o.dsem, 16)
                else:
                    inst = o.fn(eobj)
                    if o.signal:
                        inst.then_inc(sems[engname], 1)

        @block.tensor
        def _(e):
            run("tensor", e)

        @block.vector
        def _(e):
            run("vector", e)

        @block.scalar
        def _(e):
            run("scalar", e)

        @block.gpsimd
        def _(e):
            run("gpsimd", e)

        @block.sync
        def _(e):
            run("sync", e)

    def mm(self, out, lhsT, rhs, start, stop, r, w):
        return self.add("tensor", lambda e: e.matmul(out, lhsT, rhs, start=start, stop=stop), r, w)

    def tr(self, out, in_, ident, r, w):
        return self.add("tensor", lambda e: e.transpose(out, in_, ident), r, w)

    def act(self, out, in_, func, r, w, bias=None, scale=None, accum=None):
        kw = {}
        if bias is not None:
            kw["bias"] = bias
        if scale is not None:
            kw["scale"] = scale
        if accum is not None:
            kw["accum_out"] = accum
        return self.add("scalar", lambda e: e.activation(out, in_, func, **kw), r, w)

    def ts(self, eng, out, in0, s1, s2, op0, op1, r, w):
        if op1 is None:
            return self.add(eng, lambda e: e.tensor_scalar(out, in0, s1, None, op0), r, w)
        return self.add(eng, lambda e: e.tensor_scalar(out, in0, s1, s2, op0, op1), r, w)

    def tt(self, eng, out, in0, in1, op, r, w):
        return self.add(eng, lambda e: e.tensor_tensor(out, in0, in1, op), r, w)

    def stt(self, out, in0, scalar, in1, op0, op1, r, w):
        return self.add("vector", lambda e: e.scalar_tensor_tensor(out, in0, scalar, in1, op0, op1), r, w)

    def copy(self, eng, out, in_, r, w):
        if eng == "scalar":
            return self.add(eng, lambda e: e.copy(out, in_), r, w)
        return self.add(eng, lambda e: e.tensor_copy(out, in_), r, w)

    def dma(self, eng, out, in_, r, w, **kw):
        return self.add(eng, lambda e: e.dma_start(out, in_, **kw), r, w, dma=True)

    def memset(self, eng, ap, val, w):
        return self.add(eng, lambda e: e.memset(ap, val), (), w)


class Arena:
    def __init__(self, P, tensor, words):
        self.P = P
        self.t = tensor
        self.words = words
        self.off = 0
        self.names = []

    def reset(self):
        self.P.phase_barrier(None)
        self.off = 0

    def alloc(self, name, shape, dtype):
        n = int(np.prod(shape[1:]))
        if dtype == BF16:
            assert n % 2 == 0
            w = n // 2
        else:
            w = n
        assert self.off + w <= self.words, (name, self.off, w, self.words)
        ap = self.t[0:shape[0], self.off:self.off + w]
        if dtype != F32:
            ap = ap.bitcast(dtype)
        self.off += w
        if len(shape) > 2:
            names = " ".join("a%d" % i for i in range(len(shape) - 1))
            kw = {"a%d" % i: shape[i + 1] for i in range(len(shape) - 1)}
            ap = ap.rearrange("p (%s) -> p %s" % (names, names), **kw)
        self.P.fresh(name)
        return ap

from contextlib import ExitStack
import math
import ml_dtypes
from concourse.bass_utils import run_bass_kernel_spmd

S = 4096
D = 1024
NT = 32
ARENA = 49152
TWO_PI = 2.0 * math.pi
MAGIC = 12582912.0
PI_LO = 3.1415925
NFFT = 8192


def build(nc, dbg=False, stages=("A", "B0", "B1", "B2", "C", "D", "E", "F"), depth=2):
    def din(name, shape, dt=F32):
        return nc.dram_tensor(name, list(shape), dt, kind="ExternalInput").ap()

    skind = "ExternalOutput" if dbg else "Internal"

    def dscr(name, shape, dt):
        return nc.dram_tensor(name, list(shape), dt, kind=skind).ap()

    I = {}
    I["x"] = din("x", [S, D])
    I["w_in"] = din("w_in", [2, D, 6144])
    I["b_in"] = din("b_in", [2, 128, 48])
    I["b_v"] = din("b_v", [2, 128, 512])
    I["norm_mix"] = din("norm_mix", [2, 128, D])
    I["norm_ffn"] = din("norm_ffn", [2, 128, D])
    I["norm_final"] = din("norm_final", [128, D])
    I["cosT"] = din("cosT", [128, S])
    I["sinT"] = din("sinT", [128, S])
    I["hy_cw"] = din("hy_cw", [2, 128, 36])
    I["hy_cb"] = din("hy_cb", [2, 128, 12])
    I["zT"] = din("zT", [33, S])
    I["hy_w1"] = din("hy_w1", [2, 33, 64])
    I["hy_w2"] = din("hy_w2", [2, 64, 64])
    I["hy_w3"] = din("hy_w3", [2, 64, 2048])
    I["hy_cols"] = din("hy_cols", [2, 64, 4])
    I["decay"] = din("decay", [S, 512])
    I["decayb"] = din("decayb", [S, 512])
    I["hy_bias"] = din("hy_bias", [2, 2, 128, 512])
    I["ctab"] = din("ctab", [16, 128, 16 * 128], BF16)
    I["stab"] = din("stab", [16, 128, 16 * 128], BF16)
    I["twid"] = din("twid", [128, 17 * 3])
    I["alt"] = din("alt", [128, 2], BF16)
    I["altrow"] = din("altrow", [1, 128], BF16)
    I["lamv"] = din("lamv", [2, 128, 256])
    I["subln"] = din("subln", [2, 128, 128])
    I["w_up_hy"] = din("w_up_hy", [2, 512, D])
    I["w_up_da"] = din("w_up_da", [2, 512, D])
    I["w_out"] = din("w_out", [2, D, D])
    I["w_router"] = din("w_router", [2, D, 16])
    I["b_router"] = din("b_router", [2, 128, 16])
    I["w_e_gate"] = din("w_e_gate", [2, 16, D, 2048])
    I["w_e_up"] = din("w_e_up", [2, 16, D, 2048])
    I["w_e_down"] = din("w_e_down", [2, 16, 2048, D])
    I["iota"] = din("iota", [128, 512])
    I["jp"] = din("jp", [128, 64], BF16)
    I["ident_bf"] = din("ident_bf", [128, 128], BF16)
    I["ident_f"] = din("ident_f", [128, 128])
    I["ustrict"] = din("ustrict", [128, 128], BF16)
    I["ones_bf"] = din("ones_bf", [128, 128], BF16)
    out = nc.dram_tensor("out", [S, D], F32, kind="ExternalOutput").ap()

    xres = dscr("xres", [S, D], F32)
    uT = dscr("uT", [1536, S], BF16)
    qkT = dscr("qkT", [1024, S], BF16)
    gT = dscr("gT", [2048, S], BF16)
    Vtm = dscr("Vtm", [S, 512], BF16)
    Hsp = dscr("Hsp", [2, 4, 17 * 128, 512], F32)
    hsd = dscr("hsd", [2, S, 512], BF16)
    yhtm = dscr("yhtm", [S, 512], BF16)
    ctm = dscr("ctm", [3, S, 512], BF16)
    yhT = dscr("yhT", [512, S], BF16)
    ydT = dscr("ydT", [512, S], BF16)
    n2d = dscr("n2d", [S, D], BF16)
    dbgaff = dscr("dbgaff", [128, 32 * 16], F32)
    dbgpos = dscr("dbgpos", [128, 16 * 32], F32)

    st = ExitStack()
    with st:
        arena_t = st.enter_context(nc.sbuf_tensor("arena", [128, ARENA], F32))
        ident = st.enter_context(nc.sbuf_tensor("ident", [128, 128], BF16))
        identf = st.enter_context(nc.sbuf_tensor("identf", [128, 128], F32))
        ustr = st.enter_context(nc.sbuf_tensor("ustr", [128, 128], BF16))
        ones = st.enter_context(nc.sbuf_tensor("ones", [128, 128], BF16))
        alt = st.enter_context(nc.sbuf_tensor("altc", [128, 2], BF16))
        altrow = st.enter_context(nc.sbuf_tensor("altr", [1, 128], BF16))
        posm = st.enter_context(nc.sbuf_tensor("posm", [128, 16, 32], F32))
        affhl = st.enter_context(nc.sbuf_tensor("affhl", [128, 32, 16, 4], BF16))
        lamt = st.enter_context(nc.sbuf_tensor("lamt", [128, 8], F32))
        ps = [st.enter_context(nc.psum_tensor("ps%d" % i, [128, 512], F32)) for i in range(8)]
        PK = ["K:ps%d" % i for i in range(8)]
        P = Prog(nc)
        A = Arena(P, arena_t, ARENA)

        def psb(i):
            return ps[i][:].bitcast(BF16)

        P.dma("sync", ident[:], I["ident_bf"], [], ["K:ident"])
        P.dma("sync", identf[:], I["ident_f"], [], ["K:identf"])
        P.dma("sync", ustr[:], I["ustrict"], [], ["K:ustr"])
        P.dma("sync", ones[:], I["ones_bf"], [], ["K:ones"])
        P.dma("sync", alt[:], I["alt"], [], ["K:alt"])
        P.dma("sync", altrow[:], I["altrow"], [], ["K:altrow"])
        for q in range(4):
            P.dma("sync", xres[q * 1024:(q + 1) * 1024, :], I["x"][q * 1024:(q + 1) * 1024, :], [],
                  [("D:xres", j) for j in range(q * 8, q * 8 + 8)])

        def rms_tile(xt_ap, kx, junk, stt3, ks, n, eps):
            P.act(junk, xt_ap, AF.Square, [kx], [ks + "j", ks], accum=stt3[:, 0:1])
            P.ts("vector", stt3[:, 1:2], stt3[:, 0:1], 1.0 / n, eps, ALU.mult, ALU.add, [ks], [ks])
            P.act(stt3[:, 1:2], stt3[:, 1:2], AF.Sqrt, [ks], [ks])
            P.add("vector", lambda e: e.reciprocal(stt3[:, 2:3], stt3[:, 1:2]), [ks], [ks])

        wcnt = [0]

        def load_w(dst, dkey, src, stg, stgk):
            i = wcnt[0] % len(stg)
            wcnt[0] += 1
            P.dma("sync", stg[i], src, [], [stgk[i]])
            ceng = ("scalar", "vector", "scalar")[wcnt[0] % 3]
            P.copy(ceng, dst, stg[i], [stgk[i]], [dkey])

        def phase_A(l):
            A.reset()
            nT = A.alloc("nT", [128, 8, S], BF16)
            cosT = A.alloc("cosT", [128, S], F32)
            sinT = A.alloc("sinT", [128, S], F32)
            gt = A.alloc("gt", [128, D], F32)
            bcol = A.alloc("bcol", [128, 48], F32)
            bv = A.alloc("bv", [128, 512], F32)
            xt = [A.alloc("xt%d" % i, [128, D], F32) for i in range(2)]
            junk = A.alloc("junk", [128, D], BF16)
            xn = [A.alloc("xn%d" % i, [128, D], BF16) for i in range(2)]
            s3 = [A.alloc("s3%d" % i, [128, 4], F32) for i in range(2)]
            wst = [A.alloc("wst%d" % i, [128, 8, 256], F32) for i in range(2)]
            wstk = ["wst0", "wst1"]
            wt = [A.alloc("wt%d" % i, [128, 8, 512], BF16) for i in range(2)]
            stg = [A.alloc("stg%d" % i, [128, 512], BF16) for i in range(3)]
            tq = [A.alloc("tq%d" % i, [128, 512], F32) for i in range(2)]
            P.dma("sync", gt, I["norm_mix"][l], [], ["gt"])
            P.dma("sync", bcol, I["b_in"][l], [], ["bcol"])
            P.dma("sync", bv, I["b_v"][l], [], ["bv"])
            P.dma("sync", cosT, I["cosT"], [], ["cosT"])
            P.dma("sync", sinT, I["sinT"], [], ["sinT"])
            for j in range(NT):
                b = j % 2
                P.dma("sync", xt[b], xres[j * 128:(j + 1) * 128, :], [("D:xres", j)], ["xt%d" % b])
                rms_tile(xt[b], "xt%d" % b, junk, s3[b], "s3%d" % b, D, 1e-6)
                P.stt(xn[b], xt[b], s3[b][:, 2:3], gt, ALU.mult, ALU.mult, ["xt%d" % b, "s3%d" % b, "gt"], ["xn%d" % b])
                pb = psb(6 + b)
                for c in range(8):
                    P.tr(pb[:, c * 128:(c + 1) * 128], xn[b][:, c * 128:(c + 1) * 128], ident[:],
                         ["xn%d" % b, "K:ident"], [PK[6 + b]])
                P.copy("scalar", nT[:, :, j * 128:(j + 1) * 128], pb.rearrange("p (c t) -> p c t", c=8),
                       [PK[6 + b]], [("nT", j // 4)])
            wsrc = I["w_in"][l].rearrange("(c p) f -> p c f", p=128)
            gcnt = [0]

            def load_group(g):
                b = gcnt[0] % 2
                gcnt[0] += 1
                for h in range(2):
                    load_w(wt[b][:, :, h * 256:(h + 1) * 256], ("wt%d" % b, h),
                           wsrc[:, :, g * 512 + h * 256: g * 512 + (h + 1) * 256], wst, wstk)
                return b

            def wkeys(b):
                return [("wt%d" % b, 0), ("wt%d" % b, 1)]

            bankc = [0]
            stc = [0]

            def proj_fm(b, fc, tg):
                bk = bankc[0] % 4
                bankc[0] += 1
                for kc in range(8):
                    P.mm(ps[bk][:], wt[b][:, kc, fc * 128:(fc + 1) * 128], nT[:, kc, tg * 512:(tg + 1) * 512],
                         kc == 0, kc == 7, wkeys(b) + [("nT", tg)], [PK[bk]])
                return bk

            for g in [0, 1, 2, 7, 8, 9, 10]:
                b = load_group(g)
                for fc in range(4):
                    ch = g * 4 + fc
                    for tg in range(8):
                        bk = proj_fm(b, fc, tg)
                        si = stc[0] % 3
                        stc[0] += 1
                        func = AF.Identity if g < 3 else AF.Sigmoid
                        P.act(stg[si], ps[bk][:], func, [PK[bk], "bcol"], ["stg%d" % si], bias=bcol[:, ch:ch + 1])
                        if g < 3:
                            dst = uT[ch * 128:(ch + 1) * 128, tg * 512:(tg + 1) * 512]
                            dk = ("D:uT", ch, tg)
                        else:
                            gc = ch - 28
                            dst = gT[gc * 128:(gc + 1) * 128, tg * 512:(tg + 1) * 512]
                            dk = ("D:gT", gc, tg)
                        P.dma("gpsimd", dst, stg[si], ["stg%d" % si], [dk])
            for (ga, gb2, rbase) in [(3, 5, 0), (4, 6, 4)]:
                ba = load_group(ga)
                bb = load_group(gb2)
                for fc in range(4):
                    cha = ga * 4 + fc
                    chb = gb2 * 4 + fc
                    for tg in range(8):
                        bk1 = proj_fm(ba, fc, tg)
                        bk2 = proj_fm(bb, fc, tg)
                        sl = slice(tg * 512, (tg + 1) * 512)
                        P.stt(tq[0], ps[bk1][:], bcol[:, cha:cha + 1], cosT[:, sl], ALU.add, ALU.mult,
                              [PK[bk1], "bcol", "cosT"], ["tq0"])
                        P.stt(tq[1], ps[bk2][:], bcol[:, chb:chb + 1], sinT[:, sl], ALU.add, ALU.mult,
                              [PK[bk2], "bcol", "sinT"], ["tq1"])
                        si = stc[0] % 3
                        stc[0] += 1
                        P.tt("vector", stg[si], tq[0], tq[1], ALU.add, ["tq0", "tq1"], ["stg%d" % si])
                        rc = rbase + fc
                        P.dma("gpsimd", qkT[rc * 128:(rc + 1) * 128, sl], stg[si], ["stg%d" % si], [("D:qkT", rc, tg)])
            b = load_group(11)
            for j in range(NT):
                bk = bankc[0] % 4
                bankc[0] += 1
                for kc in range(8):
                    P.mm(ps[bk][:], nT[:, kc, j * 128:(j + 1) * 128], wt[b][:, kc, :], kc == 0, kc == 7,
                         wkeys(b) + [("nT", j // 4)], [PK[bk]])
                si = stc[0] % 3
                stc[0] += 1
                P.tt("vector", stg[si], ps[bk][:], bv, ALU.add, [PK[bk], "bv"], ["stg%d" % si])
                P.dma("gpsimd", Vtm[j * 128:(j + 1) * 128, :], stg[si], ["stg%d" % si], [("D:Vtm", j)])

        def sin_layer(dst, lhsT, rhs_fn, bcolap, fcolap, kr, tmp, M):
            for tg in range(8):
                bk = tg % 2
                P.mm(ps[bk][0:M, :], lhsT, rhs_fn(tg), True, True, kr, [PK[bk]])
                a, b_, c = tmp
                P.ts("vector", a, ps[bk][0:M, :], bcolap, fcolap, ALU.add, ALU.mult, [PK[bk], "fcols"], ["sa"])
                P.ts("vector", b_, a, 1.0 / TWO_PI, MAGIC, ALU.mult, ALU.add, ["sa"], ["sb"])
                P.ts("vector", b_, b_, MAGIC, None, ALU.subtract, None, ["sb"], ["sb"])
                P.stt(c, b_, -TWO_PI, a, ALU.mult, ALU.add, ["sa", "sb"], ["sc"])
                P.ts("vector", c, c, PI_LO, -PI_LO, ALU.min, ALU.max, ["sc"], ["sc"])
                P.act(dst[:, tg * 512:(tg + 1) * 512], c, AF.Sin, ["sc"], [dst_key[0]])

        dst_key = [None]

        def table_loads(cb, sb, b, idx):
            P.dma("sync", cb[b], I["ctab"][idx].rearrange("p (u k) -> p u k", k=128), [], ["cb%d" % b])
            P.dma("sync", sb[b], I["stab"][idx].rearrange("p (u k) -> p u k", k=128), [], ["sb%d" % b])

        def eo_view(dram2d, v):
            return dram2d.rearrange("(uc p two) c -> two p uc c", p=128, two=2)[v]

        def smul(out, in_, col, r, w):
            P.ts("vector", out, in_, col, None, ALU.mult, None, r, w)

        def fwd_chunk(kc, ze, zo, zk, cb, sb, T, tw, need=("X1r", "X1i", "X2r", "X2i")):
            b = kc % 2
            M = 128 if kc < 16 else 1
            B0_ = 4 * b
            want_r = ("X1r" in need) or ("X2r" in need)
            want_i = ("X1i" in need) or ("X2i" in need)
            if kc < 16:
                table_loads(cb, sb, b, kc)
                for (bank, tab, tk, zz) in ((B0_, cb, "cb", ze), (B0_ + 1, sb, "sb", ze), (B0_ + 2, cb, "cb", zo), (B0_ + 3, sb, "sb", zo)):
                    if (bank == B0_ and not want_r) or (bank == B0_ + 1 and not want_i):
                        continue
                    for uc in range(16):
                        P.mm(ps[bank][:], tab[b][:, uc, :], zz[:, uc, :], uc == 0, uc == 15, ["%s%d" % (tk, b)] + zk, [PK[bank]])
            else:
                for (bank, zz) in ((B0_, ze), (B0_ + 2, zo)):
                    if (bank == B0_ and not want_r) or (bank == B0_ + 2 and not want_i):
                        continue
                    for uc in range(16):
                        P.mm(ps[bank][0:1, :], alt[:, 0:1], zz[:, uc, :], uc == 0, uc == 15, ["K:alt"] + zk, [PK[bank]])
            Er, Ei, Or, Oi = (ps[B0_ + q][0:M, :] for q in range(4))
            kE, kEi, kO, kOi = (PK[B0_ + q] for q in range(4))
            twr = tw[0:M, kc * 3:kc * 3 + 1]
            tws = tw[0:M, kc * 3 + 1:kc * 3 + 2]
            twn = tw[0:M, kc * 3 + 2:kc * 3 + 3]
            wor, woi, t1, t2 = T["wor"][0:M, :], T["woi"][0:M, :], T["t1"][0:M, :], T["t2"][0:M, :]
            if kc < 16:
                if want_r:
                    smul(t1, Oi, tws, [kOi, "tw"], ["t1"])
                    P.stt(wor, Or, twr, t1, ALU.mult, ALU.add, [kO, "tw", "t1"], ["wor"])
                    P.tt("vector", T["x1r"][0:M, :], Er, wor, ALU.add, [kE, "wor"], ["x1r"])
                    P.tt("vector", T["x2r"][0:M, :], Er, wor, ALU.subtract, [kE, "wor"], ["x2r"])
                if want_i:
                    smul(t2, Or, twn, [kO, "tw"], ["t2"])
                    P.stt(woi, Oi, twr, t2, ALU.mult, ALU.add, [kOi, "tw", "t2"], ["woi"])
                    P.tt("vector", T["x1i"][0:M, :], Ei, woi, ALU.add, [kEi, "woi"], ["x1i"])
                    P.tt("vector", T["x2i"][0:M, :], woi, Ei, ALU.subtract, ["woi", kEi], ["x2i"])
            else:
                if want_r:
                    P.copy("vector", T["x1r"][0:1, :], Er, [kE], ["x1r"])
                    P.copy("vector", T["x2r"][0:1, :], Er, [kE], ["x2r"])
                if want_i:
                    P.ts("vector", T["x1i"][0:1, :], Or, -1.0, None, ALU.mult, None, [kO], ["x1i"])
                    P.ts("vector", T["x2i"][0:1, :], Or, -1.0, None, ALU.mult, None, [kO], ["x2i"])
            return M

        def phase_B0(l):
            A.reset()
            zT = A.alloc("zT", [33, S], F32)
            w1 = A.alloc("w1", [33, 64], F32)
            w2 = A.alloc("w2", [64, 64], F32)
            w3 = A.alloc("w3", [64, 2048], F32)
            fcols = A.alloc("fcols", [64, 4], F32)
            h1 = A.alloc("h1T", [64, S], F32)
            h2 = A.alloc("h2T", [64, S], F32)
            tmp = [A.alloc(n, [64, 512], F32) for n in ("sa", "sb", "sc")]
            dec = [A.alloc("dec%d" % i, [128, 512], F32) for i in range(2)]
            decb = [A.alloc("decb%d" % i, [128, 512], F32) for i in range(2)]
            hf = A.alloc("hf", [128, 512], F32)
            hb = A.alloc("hb", [128, 512], F32)
            hso = [A.alloc("hso%d" % i, [128, 512], BF16) for i in range(2)]
            hdo = [A.alloc("hdo%d" % i, [128, 512], BF16) for i in range(2)]
            ze = A.alloc("ze", [128, 16, 512], BF16)
            zo = A.alloc("zo", [128, 16, 512], BF16)
            cb = [A.alloc("cb%d" % i, [128, 16, 128], BF16) for i in range(2)]
            sb = [A.alloc("sb%d" % i, [128, 16, 128], BF16) for i in range(2)]
            tw = A.alloc("tw", [128, 51], F32)
            T = {n: A.alloc(n, [128, 512], F32) for n in ("wor", "woi", "t1", "t2", "x1r", "x1i", "x2r", "x2i")}
            P.dma("sync", zT, I["zT"], [], ["zT"])
            P.dma("sync", w1, I["hy_w1"][l], [], ["w1"])
            P.dma("sync", w2, I["hy_w2"][l], [], ["w2"])
            P.dma("sync", w3, I["hy_w3"][l], [], ["w3"])
            P.dma("sync", fcols, I["hy_cols"][l], [], ["fcols"])
            P.dma("sync", tw, I["twid"], [], ["tw"])
            dst_key[0] = "h1T"
            sin_layer(h1, w1, lambda tg: zT[:, tg * 512:(tg + 1) * 512], fcols[:, 0:1], fcols[:, 1:2],
                      ["w1", "zT"], tmp, 64)
            dst_key[0] = "h2T"
            sin_layer(h2, w2, lambda tg: h1[:, tg * 512:(tg + 1) * 512], fcols[:, 2:3], fcols[:, 3:4],
                      ["w2", "h1T"], tmp, 64)
            for o in range(2):
                for jt in range(NT):
                    b = jt % 2
                    P.dma("sync", dec[b], I["decay"][jt * 128:(jt + 1) * 128, :], [], ["dec%d" % b])
                    P.dma("sync", decb[b], I["decayb"][jt * 128:(jt + 1) * 128, :], [], ["decb%d" % b])
                    P.mm(ps[4][:], h2[:, jt * 128:(jt + 1) * 128], w3[:, o * 1024:o * 1024 + 512], True, True,
                         ["h2T", "w3"], [PK[4]])
                    P.mm(ps[5][:], h2[:, jt * 128:(jt + 1) * 128], w3[:, o * 1024 + 512:o * 1024 + 1024], True, True,
                         ["h2T", "w3"], [PK[5]])
                    P.tt("vector", hf, ps[4][:], dec[b], ALU.mult, [PK[4], "dec%d" % b], ["hf"])
                    P.tt("vector", hb, ps[5][:], decb[b], ALU.mult, [PK[5], "decb%d" % b], ["hb"])
                    P.tt("vector", hso[b], hf, hb, ALU.add, ["hf", "hb"], ["hso%d" % b])
                    P.tt("vector", hdo[b], hf, hb, ALU.subtract, ["hf", "hb"], ["hdo%d" % b])
                    P.dma("gpsimd", hsd[0, jt * 128:(jt + 1) * 128, :], hso[b], ["hso%d" % b], [("D:hsd", 0, jt)])
                    P.dma("gpsimd", hsd[1, jt * 128:(jt + 1) * 128, :], hdo[b], ["hdo%d" % b], [("D:hsd", 1, jt)])
                for sig in range(2):
                    hk_ = [("D:hsd", sig, jt) for jt in range(NT)]
                    P.dma("sync", ze, eo_view(hsd[sig], 0), hk_, ["ze"])
                    P.dma("sync", zo, eo_view(hsd[sig], 1), hk_, ["zo"])
                    for kc in range(17):
                        M = fwd_chunk(kc, ze, zo, ["ze", "zo"], cb, sb, T, tw,
                                      need=("X1r", "X2r") if sig == 0 else ("X1i", "X2i"))
                        if sig == 0:
                            P.dma("gpsimd", Hsp[o, 0, kc * 128:kc * 128 + M, :], T["x1r"][0:M, :], ["x1r"], [("D:Hsp", o, 0, kc)])
                            P.dma("gpsimd", Hsp[o, 2, kc * 128:kc * 128 + M, :], T["x2r"][0:M, :], ["x2r"], [("D:Hsp", o, 2, kc)])
                        else:
                            P.dma("gpsimd", Hsp[o, 1, kc * 128:kc * 128 + M, :], T["x1i"][0:M, :], ["x1i"], [("D:Hsp", o, 1, kc)])
                            P.dma("gpsimd", Hsp[o, 3, kc * 128:kc * 128 + M, :], T["x2i"][0:M, :], ["x2i"], [("D:Hsp", o, 3, kc)])

        def phase_B1(l):
            A.reset()
            cw = A.alloc("cw", [128, 36], F32)
            cbi = A.alloc("cbi", [128, 12], F32)
            ub = [A.alloc("ub%d" % i, [128, S], BF16) for i in range(2)]
            acc = [A.alloc("acc%d" % i, [128, S], F32) for i in range(2)]
            ucb = [A.alloc("ucb%d" % i, [128, S], BF16) for i in range(2)]
            ttm = [A.alloc("ttm%d" % i, [128, 32, 128], BF16) for i in range(2)]
            P.dma("sync", cw, I["hy_cw"][l], [], ["cw"])
            P.dma("sync", cbi, I["hy_cb"][l], [], ["cbi"])
            for ch in range(12):
                b = ch % 2
                P.dma("sync", ub[b], uT[ch * 128:(ch + 1) * 128, :], [("D:uT", ch, tg) for tg in range(8)], ["ub%d" % b])
                P.act(acc[b], ub[b], AF.Identity, ["ub%d" % b, "cw", "cbi"], ["acc%d" % b],
                      bias=cbi[:, ch:ch + 1], scale=cw[:, ch * 3 + 1:ch * 3 + 2])
                P.stt(acc[b][:, 1:S], ub[b][:, 0:S - 1], cw[:, ch * 3:ch * 3 + 1], acc[b][:, 1:S], ALU.mult, ALU.add,
                      ["ub%d" % b, "cw", "acc%d" % b], ["acc%d" % b])
                P.stt(ucb[b][:, 0:S - 1], ub[b][:, 1:S], cw[:, ch * 3 + 2:ch * 3 + 3], acc[b][:, 0:S - 1], ALU.mult, ALU.add,
                      ["ub%d" % b, "cw", "acc%d" % b], ["ucb%d" % b])
                P.copy("vector", ucb[b][:, S - 1:S], acc[b][:, S - 1:S], ["acc%d" % b, "ucb%d" % b], ["ucb%d" % b])
                for t8 in range(4):
                    bk = 4 + (t8 % 2)
                    pb = psb(bk)
                    for i in range(8):
                        tc = t8 * 8 + i
                        P.tr(pb[:, i * 128:(i + 1) * 128], ucb[b][:, tc * 128:(tc + 1) * 128], ident[:],
                             ["ucb%d" % b, "K:ident"], [PK[bk]])
                    P.copy("scalar" if t8 % 2 == 0 else "vector", ttm[b][:, t8 * 8:(t8 + 1) * 8, :],
                           pb.rearrange("p (c t) -> p c t", c=8), [PK[bk]], ["ttm%d" % b])
                which = ch // 4
                cc = ch % 4
                dst = ctm[which].rearrange("(tc p) c -> p tc c", p=128)[:, :, cc * 128:(cc + 1) * 128]
                P.dma("gpsimd", dst, ttm[b], ["ttm%d" % b], [("D:ctm", which, cc)])

        def phase_B2(l):
            A.reset()
            ze = [A.alloc("ze%d" % i, [128, 16, 512], BF16) for i in range(2)]
            zo = [A.alloc("zo%d" % i, [128, 16, 512], BF16) for i in range(2)]
            AA = {n: A.alloc(n, [128, 16, 512], BF16) for n in ("a0r", "a0i", "a1r", "a1i")}
            any_ = A.alloc("any", [1, 2, 512], BF16)
            cb = [A.alloc("cb%d" % i, [128, 16, 128], BF16) for i in range(2)]
            sb = [A.alloc("sb%d" % i, [128, 16, 128], BF16) for i in range(2)]
            tw = A.alloc("tw", [128, 51], F32)
            T = {n: A.alloc(n, [128, 512], F32) for n in ("wor", "woi", "t1", "t2", "x1r", "x1i", "x2r", "x2i",
                                                            "y1r", "y1i", "y2r", "y2i", "u1", "u2")}
            Hh = [A.alloc("hh%d" % i, [128, 512], F32) for i in range(4)]
            dbt = A.alloc("dbt", [128, 512], F32)
            gte = [A.alloc("gte%d" % i, [128, 512], BF16) for i in range(2)]
            zot = [A.alloc("zot%d" % i, [128, 512], BF16) for i in range(2)]
            P.dma("sync", tw, I["twid"], [], ["tw"])
            ck = [("D:ctm", 0, cc) for cc in range(4)]
            P.dma("sync", ze[0], eo_view(ctm[0], 0), ck, ["ze0"])
            P.dma("sync", zo[0], eo_view(ctm[0], 1), ck, ["zo0"])
            for o in range(2):
                zin = (ze[o], zo[o])
                zk = ["ze%d" % o, "zo%d" % o]
                P.dma("sync", dbt, I["hy_bias"][l, o], [], ["dbt"])
                for kc in range(17):
                    M = fwd_chunk(kc, zin[0], zin[1], zk, cb, sb, T, tw)
                    for q in range(4):
                        P.dma("sync", Hh[q][0:M, :], Hsp[o, q, kc * 128:kc * 128 + M, :], [("D:Hsp", o, q, kc)], ["hh%d" % q])
                    R = lambda n: T[n][0:M, :]
                    H1r, H1i, H2r, H2i = (Hh[q][0:M, :] for q in range(4))
                    for (xr, xi, hr, hi, yr, yi, e1, e2) in (("x1r", "x1i", H1r, H1i, "y1r", "y1i", "vector", "vector"),
                                                             ("x2r", "x2i", H2r, H2i, "y2r", "y2i", "vector", "vector")):
                        hq = ["hh0", "hh1"] if yr == "y1r" else ["hh2", "hh3"]
                        P.tt(e1, R("u1"), R(xr), hr, ALU.mult, [xr] + hq, ["u1"])
                        P.tt(e1, R("u2"), R(xi), hi, ALU.mult, [xi] + hq, ["u2"])
                        P.tt(e1, R(yr), R("u1"), R("u2"), ALU.subtract, ["u1", "u2"], [yr])
                        P.tt(e2, R("t1"), R(xr), hi, ALU.mult, [xr] + hq, ["t1"])
                        P.tt(e2, R("t2"), R(xi), hr, ALU.mult, [xi] + hq, ["t2"])
                        P.tt(e2, R(yi), R("t1"), R("t2"), ALU.add, ["t1", "t2"], [yi])
                    twr = tw[0:M, kc * 3:kc * 3 + 1]
                    tws = tw[0:M, kc * 3 + 1:kc * 3 + 2]
                    twn = tw[0:M, kc * 3 + 2:kc * 3 + 3]
                    if kc < 16:
                        P.tt("vector", AA["a0r"][:, kc, :], R("y1r"), R("y2r"), ALU.add, ["y1r", "y2r"], [("a0r", kc)])
                        P.tt("vector", AA["a0i"][:, kc, :], R("y1i"), R("y2i"), ALU.subtract, ["y1i", "y2i"], [("a0i", kc)])
                        P.tt("vector", R("u1"), R("y1r"), R("y2r"), ALU.subtract, ["y1r", "y2r"], ["u1"])
                        P.tt("vector", R("u2"), R("y1i"), R("y2i"), ALU.add, ["y1i", "y2i"], ["u2"])
                        smul(R("t1"), R("u2"), twn, ["u2", "tw"], ["t1"])
                        P.stt(AA["a1r"][:, kc, :], R("u1"), twr, R("t1"), ALU.mult, ALU.add, ["u1", "tw", "t1"], [("a1r", kc)])
                        smul(R("t2"), R("u1"), tws, ["u1", "tw"], ["t2"])
                        P.stt(AA["a1i"][:, kc, :], R("u2"), twr, R("t2"), ALU.mult, ALU.add, ["u2", "tw", "t2"], [("a1i", kc)])
                        if kc == 0:
                            for n in ("a0r", "a1r"):
                                P.ts("vector", AA[n][0:1, 0, :], AA[n][0:1, 0, :], 0.5, None, ALU.mult, None, [(n, 0)], [(n, 0)])
                    else:
                        P.copy("vector", any_[0:1, 0, :], R("y1r"), ["y1r"], ["any"])
                        P.ts("vector", any_[0:1, 1, :], R("y1i"), -1.0, None, ALU.mult, None, ["y1i", "any"], ["any"])
                akeys = {n: [(n, kc) for kc in range(16)] for n in AA}
                for v in range(2):
                    ar, ai = ("a0r", "a0i") if v == 0 else ("a1r", "a1i")
                    zv = zin[v]
                    for uc in range(16):
                        b = uc % 2
                        table_loads(cb, sb, b, uc)
                        gsrc = ctm[1 + o].rearrange("(uc p two) c -> two p uc c", p=128, two=2)[v][:, uc, :]
                        P.dma("sync", gte[b], gsrc, [("D:ctm", 1 + o, cc) for cc in range(4)], ["gte%d" % b])
                        bk = 4 + b
                        for kc in range(16):
                            P.mm(ps[bk][:], cb[b][:, kc, :], AA[ar][:, kc, :], kc == 0, False, ["cb%d" % b] + akeys[ar], [PK[bk]])
                        for kc in range(16):
                            P.mm(ps[bk][:], sb[b][:, kc, :], AA[ai][:, kc, :], False, False, ["sb%d" % b] + akeys[ai], [PK[bk]])
                        P.mm(ps[bk][:], altrow[0:1, :], any_[0:1, v, :], False, True, ["K:altrow", "any"], [PK[bk]])
                        P.tt("vector", T["u1"], dbt, zv[:, uc, :], ALU.mult, ["dbt", zk[v]], ["u1"])
                        P.stt(T["u2"], ps[bk][:], 2.0 / NFFT, T["u1"], ALU.mult, ALU.add, [PK[bk], "u1"], ["u2"])
                        if o == 0:
                            dstz = (ze[1], zo[1])[v]
                            P.tt("vector", dstz[:, uc, :], T["u2"], gte[b], ALU.mult, ["u2", "gte%d" % b], [("ze1", "zo1")[v]])
                        else:
                            P.tt("vector", zot[b], T["u2"], gte[b], ALU.mult, ["u2", "gte%d" % b], ["zot%d" % b])
                            dsty = yhtm.rearrange("(uc p two) c -> two p uc c", p=128, two=2)[v][:, uc, :]
                            P.dma("gpsimd", dsty, zot[b], ["zot%d" % b], [("D:yhtm", v, uc)])
            ytk = [("D:yhtm", v, uc) for v in range(2) for uc in range(16)]
            for tc in range(32):
                b = tc % 2
                P.dma("sync", gte[b], yhtm[tc * 128:(tc + 1) * 128, :], ytk, ["gte%d" % b])
                pb = psb(6 + b)
                for cc in range(4):
                    P.tr(pb[:, cc * 128:(cc + 1) * 128], gte[b][:, cc * 128:(cc + 1) * 128], ident[:],
                         ["gte%d" % b, "K:ident"], [PK[6 + b]])
                ysv = zot[b].rearrange("p (c t) -> p c t", c=4)
                P.copy("scalar", ysv, pb[:, 0:512].rearrange("p (c t) -> p c t", c=4), [PK[6 + b]], ["zot%d" % b])
                dst = yhT.rearrange("(cc p) t -> p cc t", p=128)[:, :, tc * 128:(tc + 1) * 128]
                P.dma("gpsimd", dst, ysv, ["zot%d" % b], [("D:yhT", tc)])

        def phase_C(l):
            A.reset()
            lam_init = 0.8 - 0.6 * math.exp(-0.3 * l)
            lv = A.alloc("lv", [128, 256], F32)
            lj = A.alloc("lj", [128, 64], F32)
            gs = A.alloc("gs", [128, 128], F32)
            QT = [A.alloc("QT%d" % i, [128, S], BF16) for i in range(2)]
            KT = [A.alloc("KT%d" % i, [128, S], BF16) for i in range(2)]
            Vh = [A.alloc("Vh%d" % i, [128, 32, 130], BF16) for i in range(2)]
            Et = [A.alloc("Et%d" % i, [128, 512], BF16) for i in range(3)]
            oc = [[A.alloc("oc%d_%d" % (c, q), [128, 128], F32) for q in range(4)] for c in range(2)]
            rc = A.alloc("rc", [128, 8], F32)
            od = A.alloc("od", [128, 128], F32)
            oj = A.alloc("oj", [128, 128], BF16)
            s3 = A.alloc("s3a", [128, 4], F32)
            yb = A.alloc("yb", [128, 128], BF16)
            ydst = [A.alloc("ydst%d" % i, [128, 512], BF16) for i in range(2)]
            P.dma("sync", lv, I["lamv"][l], [], ["lv"])
            P.dma("sync", gs, I["subln"][l], [], ["gs"])
            P.tt("vector", lj, lv[:, 0:64], lv[:, 64:128], ALU.mult, ["lv"], ["lj"])
            P.add("vector", lambda e: e.tensor_reduce(lamt[:, 0:1], lj, AX.X, ALU.add), ["lj"], ["K:lamt"])
            P.tt("vector", lj, lv[:, 128:192], lv[:, 192:256], ALU.mult, ["lv", "lj"], ["lj"])
            P.add("vector", lambda e: e.tensor_reduce(lamt[:, 1:2], lj, AX.X, ALU.add), ["lj", "K:lamt"], ["K:lamt"])
            P.act(lamt[:, 3:5], lamt[:, 0:2], AF.Exp, ["K:lamt"], ["K:lamt"])
            P.tt("vector", lamt[:, 5:6], lamt[:, 4:5], lamt[:, 3:4], ALU.subtract, ["K:lamt"], ["K:lamt"])
            P.ts("vector", lamt[:, 2:3], lamt[:, 5:6], -lam_init, None, ALU.add, None, ["K:lamt"], ["K:lamt"])
            P.ts("vector", gs, gs, 1.0 - lam_init, None, ALU.mult, None, ["gs"], ["gs"])
            scale = 64 ** -0.5
            ecnt = [0]
            for h in range(4):
                hb_ = h % 2
                P.dma("sync", QT[hb_], qkT[h * 128:(h + 1) * 128, :], [("D:qkT", h, tg) for tg in range(8)], ["QT%d" % hb_])
                P.dma("sync", KT[hb_], qkT[512 + h * 128:512 + (h + 1) * 128, :], [("D:qkT", 4 + h, tg) for tg in range(8)],
                      ["KT%d" % hb_])
                P.dma("sync", Vh[hb_][:, :, 0:128], Vtm.rearrange("(kt p) c -> p kt c", p=128)[:, :, h * 128:(h + 1) * 128],
                      [("D:Vtm", j) for j in range(NT)], [("Vh%d" % hb_, 0)])
                P.memset("gpsimd", Vh[hb_][:, :, 128:129], 1.0, [("Vh%d" % hb_, 1)])
                vk = [("Vh%d" % hb_, 0), ("Vh%d" % hb_, 1)]
                for qg in range(8):
                    qs_ = slice(qg * 512, (qg + 1) * 512)
                    for c in range(2):
                        pr = slice(c * 64, (c + 1) * 64)
                        def score(kt):
                            P.mm(ps[kt % 2][:], KT[hb_][pr, kt * 128:(kt + 1) * 128], QT[hb_][pr, qs_], True, True,
                                 ["KT%d" % hb_, "QT%d" % hb_], [PK[kt % 2]])
                        score(0)
                        for kt in range(32):
                            sb_ = kt % 2
                            if kt + 1 < 32:
                                score(kt + 1)
                            ei = ecnt[0] % 3
                            ecnt[0] += 1
                            P.act(Et[ei], ps[sb_][:], AF.Exp, [PK[sb_]], ["Et%d" % ei], scale=scale)
                            for q4 in range(4):
                                P.mm(ps[2 + q4][:, 0:129], Et[ei][:, q4 * 128:(q4 + 1) * 128], Vh[hb_][:, kt, 0:129],
                                     kt == 0, kt == 31, ["Et%d" % ei] + vk, [PK[2 + q4]])
                        for q4 in range(4):
                            P.add("vector", (lambda q4=q4, c=c: lambda e: e.reciprocal(rc[:, c * 4 + q4:c * 4 + q4 + 1],
                                                                                       ps[2 + q4][:, 128:129]))(),
                                  [PK[2 + q4]], [("rc", c, q4)])
                            P.ts("vector", oc[c][q4], ps[2 + q4][:, 0:128], rc[:, c * 4 + q4:c * 4 + q4 + 1], None, ALU.mult, None,
                                 [PK[2 + q4], ("rc", c, q4)], ["oc%d_%d" % (c, q4)])
                    yi = qg % 2
                    pb = psb(6)
                    for q4 in range(4):
                        P.stt(od, oc[1][q4], lamt[:, 2:3], oc[0][q4], ALU.mult, ALU.add,
                              ["oc1_%d" % q4, "oc0_%d" % q4, "K:lamt"], ["od"])
                        rms_tile(od, "od", oj, s3, "s3a", 128, 1e-5)
                        P.stt(yb, od, s3[:, 2:3], gs, ALU.mult, ALU.mult, ["od", "s3a", "gs"], ["yb"])
                        P.tr(pb[:, q4 * 128:(q4 + 1) * 128], yb, ident[:], ["yb", "K:ident"], [PK[6]])
                    P.copy("scalar", ydst[yi], pb[:, 0:512], [PK[6]], ["ydst%d" % yi])
                    P.dma("gpsimd", ydT[h * 128:(h + 1) * 128, qs_], ydst[yi], ["ydst%d" % yi], [("D:ydT", h, qg)])

        def phase_D(l):
            A.reset()
            wuh = A.alloc("wuh", [128, 4, D], BF16)
            wua = A.alloc("wua", [128, 4, D], BF16)
            wo = A.alloc("wo", [128, 8, D], BF16)
            wst = [A.alloc("wst%d" % i, [128, 4, 512], F32) for i in range(2)]
            wstk = ["wst0", "wst1"]
            yh = [A.alloc("yh%d" % i, [128, 4, 512], BF16) for i in range(2)]
            yd = [A.alloc("yd%d" % i, [128, 4, 512], BF16) for i in range(2)]
            gg = [A.alloc("gg%d" % i, [128, 16, 512], BF16) for i in range(2)]
            mT = [A.alloc("mT%d" % i, [128, 8, 512], BF16) for i in range(2)]
            ta = A.alloc("ta", [128, 512], F32)
            tb = A.alloc("tb", [128, 512], F32)
            xt = [A.alloc("xt%d" % i, [128, D], F32) for i in range(2)]
            xo = [A.alloc("xo%d" % i, [128, D], F32) for i in range(2)]
            for (dst, nm, src) in ((wuh, "wuh", I["w_up_hy"][l]), (wua, "wua", I["w_up_da"][l])):
                sv = src.rearrange("(c p) f -> p c f", p=128)
                for h in range(2):
                    load_w(dst[:, :, h * 512:(h + 1) * 512], (nm, h), sv[:, :, h * 512:(h + 1) * 512], wst, wstk)
            sv = I["w_out"][l].rearrange("(c p) f -> p c f", p=128)
            for c2 in range(2):
                for h in range(2):
                    load_w(wo[:, c2 * 4:(c2 + 1) * 4, h * 512:(h + 1) * 512], ("wo", c2, h),
                           sv[:, c2 * 4:(c2 + 1) * 4, h * 512:(h + 1) * 512], wst, wstk)
            wok = [("wo", c2, h) for c2 in range(2) for h in range(2)]
            xcnt = [0]
            for tg in range(8):
                b = tg % 2
                sl = slice(tg * 512, (tg + 1) * 512)
                P.dma("sync", yh[b], yhT.rearrange("(cc p) t -> p cc t", p=128)[:, :, sl],
                      [("D:yhT", tc) for tc in range(tg * 4, tg * 4 + 4)], ["yh%d" % b])
                P.dma("sync", yd[b], ydT.rearrange("(cc p) t -> p cc t", p=128)[:, :, sl],
                      [("D:ydT", h, tg) for h in range(4)], ["yd%d" % b])
                P.dma("sync", gg[b], gT.rearrange("(cc p) t -> p cc t", p=128)[:, :, sl],
                      [("D:gT", gc, tg) for gc in range(16)], ["gg%d" % b])
                for dm in range(8):
                    for cc in range(4):
                        P.mm(ps[0][:], wuh[:, cc, dm * 128:(dm + 1) * 128], yh[b][:, cc, :], cc == 0, cc == 3,
                             [("wuh", 0), ("wuh", 1), "yh%d" % b], [PK[0]])
                    for cc in range(4):
                        P.mm(ps[1][:], wua[:, cc, dm * 128:(dm + 1) * 128], yd[b][:, cc, :], cc == 0, cc == 3,
                             [("wua", 0), ("wua", 1), "yd%d" % b], [PK[1]])
                    P.tt("vector", ta, ps[0][:], gg[b][:, dm, :], ALU.mult, [PK[0], "gg%d" % b], ["ta"])
                    P.tt("vector", tb, ps[1][:], gg[b][:, 8 + dm, :], ALU.mult, [PK[1], "gg%d" % b], ["tb"])
                    P.tt("vector", mT[b][:, dm, :], ta, tb, ALU.add, ["ta", "tb"], [("mT%d" % b, dm)])
                mk = [("mT%d" % b, dm) for dm in range(8)]
                for tt_ in range(4):
                    j = tg * 4 + tt_
                    xb = xcnt[0] % 2
                    xcnt[0] += 1
                    P.dma("sync", xt[xb], xres[j * 128:(j + 1) * 128, :], [("D:xres", j)], ["xt%d" % xb])
                    for og in range(2):
                        bk = 2 + og
                        for dm in range(8):
                            P.mm(ps[bk][:], mT[b][:, dm, tt_ * 128:(tt_ + 1) * 128], wo[:, dm, og * 512:(og + 1) * 512],
                                 dm == 0, dm == 7, mk + wok, [PK[bk]])
                        P.tt("vector", xo[xb][:, og * 512:(og + 1) * 512], ps[bk][:], xt[xb][:, og * 512:(og + 1) * 512], ALU.add,
                             [PK[bk], "xt%d" % xb], [("xo%d" % xb, og)])
                    P.dma("gpsimd", xres[j * 128:(j + 1) * 128, :], xo[xb], [("xo%d" % xb, 0), ("xo%d" % xb, 1)], [("D:xres", j)])

        def phase_E1(l):
            A.reset()
            gt = A.alloc("gt", [128, D], F32)
            wr = A.alloc("wr", [128, 8, 16], F32)
            br = A.alloc("br", [128, 16], F32)
            xt = [A.alloc("xt%d" % i, [128, D], F32) for i in range(2)]
            junk = A.alloc("junk", [128, D], BF16)
            xn = [A.alloc("xn%d" % i, [128, D], F32) for i in range(2)]
            nb = [A.alloc("nb%d" % i, [128, D], BF16) for i in range(2)]
            xT = [A.alloc("xT%d" % i, [128, 8, 128], F32) for i in range(2)]
            s3 = [A.alloc("s3%d" % i, [128, 4], F32) for i in range(2)]
            lg = A.alloc("lg", [128, 32, 16], F32)
            mx = A.alloc("mx", [128, 32], F32)
            ex = A.alloc("ex", [128, 32, 16], F32)
            aff = A.alloc("aff", [128, 32, 16], F32)
            lo = A.alloc("lo", [128, 16], F32)
            mid = A.alloc("mid", [128, 16], F32)
            cmp_ = A.alloc("cmp", [128, 32, 16], F32)
            cnt = A.alloc("cnt", [128, 16], F32)
            onesm = A.alloc("onesm", [128, 128], F32)
            ge = A.alloc("ge", [128, 16], F32)
            mask = A.alloc("mask", [128, 16, 32], F32)
            maskb = A.alloc("maskb", [128, 16, 32], BF16)
            csum = A.alloc("csum", [128, 16, 32], F32)
            onesf = A.alloc("onesf", [128, 512], F32)
            base = A.alloc("base", [128, 16], F32)
            mcum = A.alloc("mcum", [128, 16, 32], F32)
            mcumb = A.alloc("mcumb", [128, 16, 32], BF16)
            ptmp = A.alloc("ptmp", [128, 16, 32], F32)
            ahi = A.alloc("ahi", [128, 32, 16], BF16)
            P.dma("sync", gt, I["norm_ffn"][l], [], ["gt"])
            P.dma("sync", wr, I["w_router"][l].rearrange("(c p) e -> p c e", p=128), [], ["wr"])
            P.dma("sync", br, I["b_router"][l], [], ["br"])
            for j in range(NT):
                b = j % 2
                P.dma("sync", xt[b], xres[j * 128:(j + 1) * 128, :], [("D:xres", j)], ["xt%d" % b])
                rms_tile(xt[b], "xt%d" % b, junk, s3[b], "s3%d" % b, D, 1e-6)
                P.stt(xn[b], xt[b], s3[b][:, 2:3], gt, ALU.mult, ALU.mult, ["xt%d" % b, "s3%d" % b, "gt"], ["xn%d" % b])
                P.copy("scalar", nb[b], xn[b], ["xn%d" % b], ["nb%d" % b])
                P.dma("gpsimd", n2d[j * 128:(j + 1) * 128, :], nb[b], ["nb%d" % b], [("D:n2d", j)])
                for c in range(8):
                    bk = 4 + 2 * b + c // 4
                    P.tr(ps[bk][:, (c % 4) * 128:(c % 4 + 1) * 128], xn[b][:, c * 128:(c + 1) * 128], identf[:],
                         ["xn%d" % b, "K:identf"], [PK[bk]])
                P.copy("scalar", xT[b][:, 0:4, :], ps[4 + 2 * b][:].rearrange("p (c t) -> p c t", c=4), [PK[4 + 2 * b]],
                       [("xT%d" % b, 0)])
                P.copy("vector", xT[b][:, 4:8, :], ps[5 + 2 * b][:].rearrange("p (c t) -> p c t", c=4), [PK[5 + 2 * b]],
                       [("xT%d" % b, 1)])
                for c in range(8):
                    P.mm(ps[b][:, 0:16], xT[b][:, c, :], wr[:, c, :], c == 0, c == 7,
                         [("xT%d" % b, 0), ("xT%d" % b, 1), "wr"], [PK[b]])
                P.tt("vector", lg[:, j, :], ps[b][:, 0:16], br, ALU.add, [PK[b], "br"], ["lg"])
            P.add("vector", lambda e: e.tensor_reduce(mx, lg, AX.X, ALU.max), ["lg"], ["mx"])
            P.tt("vector", ex, lg, mx.unsqueeze(2).broadcast_to([128, 32, 16]), ALU.subtract, ["lg", "mx"], ["ex"])
            P.act(ex, ex, AF.Exp, ["ex"], ["ex"])
            P.add("vector", lambda e: e.tensor_reduce(mx, ex, AX.X, ALU.add), ["ex"], ["mx"])
            P.add("vector", lambda e: e.reciprocal(mx, mx), ["mx"], ["mx"])
            P.tt("vector", aff, ex, mx.unsqueeze(2).broadcast_to([128, 32, 16]), ALU.mult, ["ex", "mx"], ["aff"])
            P.memset("vector", lo, 0.0, ["lo"])
            P.memset("vector", onesf, 1.0, ["onesf"])
            P.memset("vector", onesm, 1.0, ["onesm"])
            for it in range(28):
                hstep = 0.5 ** (it + 1)
                P.ts("vector", mid, lo, hstep, None, ALU.add, None, ["lo"], ["mid"])
                P.tt("vector", cmp_, aff, mid.unsqueeze(1).broadcast_to([128, 32, 16]), ALU.is_ge, ["aff", "mid"], ["cmp"])
                P.add("vector", lambda e: e.tensor_reduce(cnt, cmp_.rearrange("p j e -> p e j"), AX.X, ALU.add), ["cmp"], ["cnt"])
                P.mm(ps[2][:, 0:16], onesm, cnt, True, True, ["onesm", "cnt"], [PK[2]])
                P.ts("vector", ge, ps[2][:, 0:16], 511.5, hstep, ALU.is_ge, ALU.mult, [PK[2]], ["ge"])
                P.tt("vector", lo, lo, ge, ALU.add, ["lo", "ge"], ["lo"])
            P.tt("vector", mask, aff.rearrange("p j e -> p e j"), lo.unsqueeze(2).broadcast_to([128, 16, 32]), ALU.is_ge,
                 ["aff", "lo"], ["mask"])
            P.copy("vector", maskb, mask, ["mask"], ["maskb"])
            mflat = mask.rearrange("p e j -> p (e j)")
            cflat = csum.rearrange("p e j -> p (e j)")
            P.add("vector", lambda e: e.tensor_tensor_scan(cflat, onesf, mflat, 0.0, ALU.mult, ALU.add), ["mask", "onesf"], ["csum"])
            P.memset("vector", base[:, 0:1], 0.0, [("base", 0)])
            P.copy("vector", base[:, 1:16], csum[:, 0:15, 31], ["csum"], [("base", 1)])
            P.tt("vector", mcum, csum, mask, ALU.subtract, ["csum", "mask"], ["mcum"])
            P.tt("vector", mcum, mcum, base.unsqueeze(2).broadcast_to([128, 16, 32]), ALU.subtract,
                 ["mcum", ("base", 0), ("base", 1)], ["mcum"])
            P.copy("vector", mcumb, mcum, ["mcum"], ["mcumb"])
            P.mm(ps[3][:], ustr[:], maskb.rearrange("p e j -> p (e j)"), True, False, ["K:ustr", "maskb"], [PK[3]])
            P.mm(ps[3][:], ones[:], mcumb.rearrange("p e j -> p (e j)"), False, True, ["K:ones", "mcumb"], [PK[3]])
            pf = ptmp.rearrange("p e j -> p (e j)")
            P.ts("vector", pf, ps[3][:], 1.0, None, ALU.add, None, [PK[3]], ["ptmp"])
            P.tt("vector", pf, pf, mflat, ALU.mult, ["ptmp", "mask"], ["ptmp"])
            P.ts("vector", posm[:].rearrange("p e j -> p (e j)"), pf, -1.0, None, ALU.add, None, ["ptmp"], ["K:posm"])
            jpt = A.alloc("jpt", [128, 32, 2], BF16)
            P.dma("sync", jpt, I["jp"].rearrange("p (j two) -> p j two", two=2), [], ["jpt"])
            P.copy("vector", affhl[:, :, :, 2:4], jpt.unsqueeze(2).broadcast_to([128, 32, 16, 2]), ["jpt", "K:affhl"], ["K:affhl"])
            P.copy("vector", ahi, aff, ["aff"], ["ahi"])
            P.copy("vector", affhl[:, :, :, 0], ahi, ["ahi"], ["K:affhl"])
            P.tt("vector", affhl[:, :, :, 1], aff, ahi, ALU.subtract, ["aff", "ahi", "K:affhl"], ["K:affhl"])
            if dbg:
                P.dma("gpsimd", dbgaff, aff.rearrange("p j e -> p (j e)"), ["aff"], ["D:dbgaff"])
                P.dma("gpsimd", dbgpos, posm[:].rearrange("p e j -> p (e j)"), ["K:posm"], ["D:dbgpos"])

        def phase_E2(l):
            A.reset()
            Sel = A.alloc("Sel", [128, 32, 512], BF16)
            xgt = A.alloc("xgt", [128, 4, D], BF16)
            xg = A.alloc("xg", [128, 8, 512], BF16)
            hT = A.alloc("hT", [128, 16, 512], BF16)
            yy = [A.alloc("yy%d" % i, [128, 4, D], F32) for i in range(2)]
            wsl = [A.alloc("wsl%d" % i, [128, 8, 512], BF16) for i in range(4)]
            wst = [A.alloc("wst%d" % i, [128, 8, 512], F32) for i in range(3)]
            wstk = ["wst0", "wst1", "wst2"]
            iot = A.alloc("iot", [128, 512], F32)
            g16 = A.alloc("g16", [128, 16], F32)
            gsl = [A.alloc("gsl%d" % i, [128, 4], F32) for i in range(2)]
            idxf = A.alloc("idxf", [128, 4], F32)
            idxi = [A.alloc("idxi%d" % i, [128, 4], I32) for i in range(2)]
            sg = [A.alloc("sg%d" % i, [128, 512], F32) for i in range(2)]
            P.dma("sync", iot, I["iota"], [], ["iot"])
            n2k = [("D:n2d", j) for j in range(NT)]
            xk = [("D:xres", j) for j in range(NT)]
            wc = [0]

            def wload(src3):
                i = wc[0] % 4
                wc[0] += 1
                load_w(wsl[i], "wsl%d" % i, src3, wst, wstk)
                return wsl[i], "wsl%d" % i

            selk = [("Sel", q) for q in range(4)]
            xgk = [("xg", q) for q in range(4)]
            hk = [("hT", f) for f in range(16)]

            def prep1(e_):
                eb = e_ % 2
                for j in range(32):
                    P.ts("vector", Sel[:, j, :], iot, posm[:, e_, j:j + 1], None, ALU.is_equal, None,
                         ["iot", "K:posm"], [("Sel", j // 8)])
                for sc in range(4):
                    for j in range(32):
                        P.mm(ps[7][:, sc * 4:sc * 4 + 4], Sel[:, j, sc * 128:(sc + 1) * 128], affhl[:, j, e_, :], j == 0, j == 31,
                             selk + ["K:affhl"], [PK[7]])
                P.copy("vector", g16, ps[7][:, 0:16], [PK[7]], ["g16"])
                g4 = g16.rearrange("p (s f) -> p s f", f=4)
                P.tt("vector", gsl[eb], g4[:, :, 0], g4[:, :, 1], ALU.add, ["g16"], ["gsl%d" % eb])
                P.stt(idxf, g4[:, :, 2], 128.0, g4[:, :, 3], ALU.mult, ALU.add, ["g16"], ["idxf"])
                P.copy("vector", idxi[eb], idxf, ["idxf"], ["idxi%d" % eb])
                for sc in range(4):
                    P.add("gpsimd", (lambda sc=sc, eb=eb: lambda e: e.indirect_dma_start(
                        out=xgt[:, sc, :], out_offset=None, in_=n2d,
                        in_offset=bass.IndirectOffsetOnAxis(ap=idxi[eb][:, sc:sc + 1], axis=0)))(),
                        ["idxi%d" % eb] + n2k, [("xgt", sc)], dma=True)

            def prep2(e_):
                pb = psb(3)
                for dc2 in range(4):
                    for h in range(2):
                        dc = dc2 * 2 + h
                        for sc in range(4):
                            P.tr(pb[:, h * 512 + sc * 128:h * 512 + (sc + 1) * 128], xgt[:, sc, dc * 128:(dc + 1) * 128], ident[:],
                                 [("xgt", sc), "K:ident"], [PK[3]])
                    P.copy("scalar" if dc2 % 2 == 0 else "vector", xg[:, dc2 * 2:dc2 * 2 + 2, :],
                           pb.rearrange("p (h s) -> p h s", h=2), [PK[3]], [("xg", dc2)])

            def ffn_up(e_):
                gsrc = I["w_e_gate"][l, e_].rearrange("(c p) f -> p c f", p=128)
                usrc = I["w_e_up"][l, e_].rearrange("(c p) f -> p c f", p=128)
                for fg in range(4):
                    wg, wgk = wload(gsrc[:, :, fg * 512:(fg + 1) * 512])
                    wu, wuk = wload(usrc[:, :, fg * 512:(fg + 1) * 512])
                    for fc in range(4):
                        for dc in range(8):
                            P.mm(ps[4][:], wg[:, dc, fc * 128:(fc + 1) * 128], xg[:, dc, :], dc == 0, dc == 7, [wgk] + xgk, [PK[4]])
                        for dc in range(8):
                            P.mm(ps[5][:], wu[:, dc, fc * 128:(fc + 1) * 128], xg[:, dc, :], dc == 0, dc == 7, [wuk] + xgk, [PK[5]])
                        si = fc % 2
                        P.act(sg[si], ps[4][:], AF.Silu, [PK[4]], ["sg%d" % si])
                        P.tt("vector", hT[:, fg * 4 + fc, :], sg[si], ps[5][:], ALU.mult, ["sg%d" % si, PK[5]], [("hT", fg * 4 + fc)])

            def ffn_down(e_):
                eb = e_ % 2
                dsrc = I["w_e_down"][l, e_].rearrange("(c p) d -> p c d", p=128)
                dbanks = [0, 1, 2, 6]
                for dg in range(2):
                    for fh in range(2):
                        wd, wdk = wload(dsrc[:, fh * 8:(fh + 1) * 8, dg * 512:(dg + 1) * 512])
                        for sc in range(4):
                            for f8 in range(8):
                                fc = fh * 8 + f8
                                P.mm(ps[dbanks[sc]][:], hT[:, fc, sc * 128:(sc + 1) * 128], wd[:, f8, :], fc == 0, fc == 15,
                                     hk + [wdk], [PK[dbanks[sc]]])
                    for sc in range(4):
                        P.ts("vector", yy[eb][:, sc, dg * 512:(dg + 1) * 512], ps[dbanks[sc]][:],
                             gsl[eb][:, sc:sc + 1], None, ALU.mult, None, [PK[dbanks[sc]], "gsl%d" % eb], [("yy%d" % eb, sc, dg)])

            def scatter(e_):
                eb = e_ % 2
                for sc in range(4):
                    P.add("gpsimd", (lambda sc=sc, eb=eb: lambda e: e.indirect_dma_start(
                        out=xres, out_offset=bass.IndirectOffsetOnAxis(ap=idxi[eb][:, sc:sc + 1], axis=0),
                        in_=yy[eb][:, sc, :], in_offset=None, compute_op=ALU.add))(),
                        ["idxi%d" % eb, ("yy%d" % eb, sc, 0), ("yy%d" % eb, sc, 1)] + xk, xk, dma=True)

            prep1(0)
            prep2(0)
            for e_ in range(16):
                ffn_up(e_)
                if e_ + 1 < 16:
                    prep1(e_ + 1)
                ffn_down(e_)
                if e_ + 1 < 16:
                    prep2(e_ + 1)
                scatter(e_)

        def phase_F():
            A.reset()
            gt = A.alloc("gt", [128, D], F32)
            xt = [A.alloc("xt%d" % i, [128, D], F32) for i in range(2)]
            junk = A.alloc("junk", [128, D], BF16)
            xo = [A.alloc("xo%d" % i, [128, D], F32) for i in range(2)]
            s3 = [A.alloc("s3%d" % i, [128, 4], F32) for i in range(2)]
            P.dma("sync", gt, I["norm_final"], [], ["gt"])
            for j in range(NT):
                b = j % 2
                P.dma("sync", xt[b], xres[j * 128:(j + 1) * 128, :], [("D:xres", j)], ["xt%d" % b])
                rms_tile(xt[b], "xt%d" % b, junk, s3[b], "s3%d" % b, D, 1e-6)
                P.stt(xo[b], xt[b], s3[b][:, 2:3], gt, ALU.mult, ALU.mult, ["xt%d" % b, "s3%d" % b, "gt"], ["xo%d" % b])
                P.dma("gpsimd", out[j * 128:(j + 1) * 128, :], xo[b], ["xo%d" % b], [("D:out", j)])

        for l in range(depth):
            if "A" in stages:
                phase_A(l)
            if "B0" in stages:
                phase_B0(l)
            if "B1" in stages:
                phase_B1(l)
            if "B2" in stages:
                phase_B2(l)
            if "C" in stages:
                phase_C(l)
            if "D" in stages:
                phase_D(l)
            if "E" in stages:
                phase_E1(l)
                phase_E2(l)
        if "F" in stages:
            phase_F()
        fin = [k for k in P.state if (k if isinstance(k, str) else k[0]).startswith("D:")]
        P.add("sync", lambda e: e.nop(), fin, [])
        P.add("gpsimd", lambda e: e.nop(), fin, [])
        P.emit(st)
        print("ops", len(P.ops), "maxsem", P.maxsem, flush=True)
    return nc


_CONST = {}


def _consts():
    if _CONST:
        return _CONST
    bf = ml_dtypes.bfloat16
    half = 32
    inv = (10000.0 ** (-np.arange(half, dtype=np.float32) * 2.0 / 64)).astype(np.float32)
    pos = np.arange(S, dtype=np.float32)
    ang = pos[:, None] * inv[None, :]
    cos = np.cos(ang).astype(np.float32).T
    sin = np.sin(ang).astype(np.float32).T
    cosT = np.zeros((128, S), np.float32)
    sinT = np.zeros((128, S), np.float32)
    for p in range(128):
        i = p % 32
        cosT[p] = cos[i]
        sinT[p] = -sin[i] if (p % 64) < 32 else sin[i]
    _CONST["cosT"] = cosT
    _CONST["sinT"] = sinT
    L = S
    t = np.linspace(0.0, 1.0, L, dtype=np.float32)
    bands = 16
    w = (2.0 * np.float32(math.pi) * np.arange(L, dtype=np.float32) / L).astype(np.float32)
    fr = np.linspace(1e-4, bands - 1, bands, dtype=np.float32)
    a = w[:, None] * fr[None, :]
    z = np.concatenate([t[:, None], np.cos(a), -np.sin(a)], axis=-1).astype(np.float32)
    _CONST["zT"] = np.ascontiguousarray(z.T)
    mind = math.log(1e-2) / 0.3
    maxd = math.log(1e-2) / 1.5
    deltas = np.abs(np.linspace(mind, maxd, 512, dtype=np.float32))
    decay = np.exp(-t[:, None] * deltas[None, :]).astype(np.float32)
    _CONST["decay"] = decay
    db = decay.copy()
    db[0] = 0.0
    _CONST["decayb"] = db
    n = np.arange(2048, dtype=np.int64)
    prod = (n[:, None] * n[None, :]) % 4096
    angd = prod.astype(np.float64) * (2.0 * math.pi / 4096)
    for nm, fn in (("ctab", np.cos), ("stab", lambda v: -np.sin(v))):
        m = fn(angd).astype(np.float32)
        m = m.reshape(16, 128, 16, 128)
        m = np.ascontiguousarray(m.transpose(2, 1, 0, 3)).reshape(16, 128, 16 * 128)
        _CONST[nm] = m.astype(bf)
    kk = np.arange(17 * 128, dtype=np.float64)
    ph = kk * (2.0 * math.pi / NFFT)
    tw = np.stack([np.cos(ph), np.sin(ph), -np.sin(ph)], axis=-1).astype(np.float32)
    _CONST["twid"] = np.ascontiguousarray(tw.reshape(17, 128, 3).transpose(1, 0, 2)).reshape(128, 51)
    altv = np.where(np.arange(128) % 2 == 0, 1.0, -1.0).astype(np.float32)
    _CONST["alt"] = np.stack([altv, altv], axis=1).astype(bf)
    _CONST["altrow"] = altv[None, :].astype(bf)
    _CONST["iota"] = np.broadcast_to(np.arange(512, dtype=np.float32)[None, :], (128, 512)).copy()
    jp = np.zeros((128, 32, 2), np.float32)
    jp[:, :, 0] = np.arange(32, dtype=np.float32)[None, :]
    jp[:, :, 1] = np.arange(128, dtype=np.float32)[:, None]
    _CONST["jp"] = jp.reshape(128, 64).astype(bf)
    _CONST["ident_bf"] = np.eye(128, dtype=np.float32).astype(bf)
    _CONST["ident_f"] = np.eye(128, dtype=np.float32)
    _CONST["ustrict"] = np.triu(np.ones((128, 128), np.float32), 1).astype(bf)
    _CONST["ones_bf"] = np.ones((128, 128), np.float32).astype(bf)
    return _CONST


def _rep(v, n=128):
    return np.ascontiguousarray(np.broadcast_to(v[..., None, :], v.shape[:-1] + (n, v.shape[-1])))


def prep_shared(inp):
    c = dict(_consts())
    w_in = np.asarray(inp["w_in"])
    b_in = np.asarray(inp["b_in"])
    sw = np.concatenate([np.arange(h * 64 + 32, h * 64 + 64).tolist() + np.arange(h * 64, h * 64 + 32).tolist()
                         for h in range(8)]).astype(np.int64)
    u0, q0, k0, v0, gh0, ga0 = 0, 1536, 2048, 2560, 3072, 4096
    cols = np.concatenate([np.arange(u0, u0 + 1536), np.arange(q0, q0 + 512), np.arange(k0, k0 + 512),
                           q0 + sw, k0 + sw, np.arange(gh0, gh0 + 1024), np.arange(ga0, ga0 + 1024),
                           np.arange(v0, v0 + 512)])
    c["w_in"] = np.ascontiguousarray(w_in[:, :, cols])
    bext = b_in[:, cols]
    c["b_in"] = np.ascontiguousarray(bext.reshape(2, 48, 128).transpose(0, 2, 1))
    c["b_v"] = _rep(b_in[:, v0:v0 + 512])
    c["norm_mix"] = _rep(np.asarray(inp["norm_mix"]))
    c["norm_ffn"] = _rep(np.asarray(inp["norm_ffn"]))
    c["norm_final"] = _rep(np.asarray(inp["norm_final"]))
    cw = np.asarray(inp["hy_conv_w"])
    c["hy_cw"] = np.ascontiguousarray(cw.reshape(2, 3, 12, 128).transpose(0, 3, 2, 1)).reshape(2, 128, 36)
    c["hy_cb"] = np.ascontiguousarray(np.asarray(inp["hy_conv_b"]).reshape(2, 12, 128).transpose(0, 2, 1))
    c["hy_w1"] = np.asarray(inp["hy_ffn_w1"])
    c["hy_w2"] = np.asarray(inp["hy_ffn_w2"])
    c["hy_w3"] = np.asarray(inp["hy_ffn_w3"])
    c["hy_cols"] = np.ascontiguousarray(np.stack([np.asarray(inp["hy_ffn_b1"]), np.asarray(inp["hy_ffn_f1"]),
                                                  np.asarray(inp["hy_ffn_b2"]), np.asarray(inp["hy_ffn_f2"])], axis=-1))
    c["hy_bias"] = _rep(np.asarray(inp["hy_bias"]))
    lam = np.concatenate([np.asarray(inp["lambda_q1"]), np.asarray(inp["lambda_k1"]),
                          np.asarray(inp["lambda_q2"]), np.asarray(inp["lambda_k2"])], axis=-1)
    c["lamv"] = _rep(lam)
    c["subln"] = _rep(np.asarray(inp["subln_g"]))
    c["w_up_hy"] = np.asarray(inp["w_up_hyena"])
    c["w_up_da"] = np.asarray(inp["w_up_attn"])
    c["w_out"] = np.asarray(inp["w_out"])
    c["w_router"] = np.asarray(inp["w_router"])
    c["b_router"] = _rep(np.asarray(inp["b_router"]))
    c["w_e_gate"] = np.asarray(inp["w_e_gate"])
    c["w_e_up"] = np.asarray(inp["w_e_up"])
    c["w_e_down"] = np.asarray(inp["w_e_down"])
    return {k: np.ascontiguousarray(v) for k, v in c.items()}


def kernel(**inputs):
    x = np.asarray(inputs["x"], dtype=np.float32)
    shared = prep_shared(inputs)
    nc = build(bass.Bass("TRN2", target_bir_lowering=False))
    in_maps = []
    for core in range(8):
        m = dict(shared)
        m["x"] = np.ascontiguousarray(x[core % 4])
        in_maps.append(m)
    res = run_bass_kernel_spmd(nc, in_maps, core_ids=list(range(8)))
    return np.stack([np.asarray(res.results[b]["out"], dtype=np.float32) for b in range(4)], axis=0)
```
